# Optimizing a Trainium2 kernel written in Bass

```python
import jax, jax.numpy as jnp
from jax import lax
import numpy as np

D_MODEL = 1024
BATCH = 4
SEQ = 8192
DEPTH = 2

GRID_W = 64
CTX_LEN = 256
EPS = 1e-6
N_HEADS = 8
N_KV_HEADS = 2
HEAD_DIM = 64
ATTN_W = N_HEADS * HEAD_DIM
KV_W = N_KV_HEADS * HEAD_DIM
CONV_W = D_MODEL - ATTN_W
CONV_TAPS = 3
Q_BLOCK = 128
ROPE_THETA = 10000.0
AXIS_DIM = HEAD_DIM // 2
IN_SPLITS = (ATTN_W, ATTN_W + KV_W, ATTN_W + 2 * KV_W, ATTN_W + 2 * KV_W + CONV_W, ATTN_W + 2 * KV_W + 2 * CONV_W)
IN_W = ATTN_W + 2 * KV_W + 3 * CONV_W
CHUNK = 128
GM_W = D_MODEL
GM_GROUPS = 8
GM_GC = GM_W // GM_GROUPS
D_FF = 2816
N_EXPERTS = 8
TOP_K = 2
EXPERT_FF = 3584
N_EVEN = (DEPTH + 1) // 2
N_ODD = DEPTH // 2

kernel_name = "hybrid_dit_attn_conv_gmlp_moe"


def _rms_norm(x, g):
    xf = x.astype(jnp.float32)
    y = xf * lax.rsqrt(jnp.mean(xf * xf, axis=-1, keepdims=True) + EPS)
    return (y * g.astype(jnp.float32)).astype(x.dtype)


def _layer_norm(x, g, b):
    xf = x.astype(jnp.float32)
    mu = jnp.mean(xf, axis=-1, keepdims=True)
    var = jnp.mean(jnp.square(xf - mu), axis=-1, keepdims=True)
    y = (xf - mu) * lax.rsqrt(var + EPS)
    return (y * g.astype(jnp.float32) + b.astype(jnp.float32)).astype(x.dtype)


def _modulation(cond, w_mod, b_mod):
    return jnp.split(jax.nn.silu(cond) @ w_mod + b_mod, 6, axis=-1)


def _pre(x, g, shift, scale):
    return _rms_norm(x, g) * (1.0 + scale) + shift


def _axial_rope(n):
    rows = n // GRID_W
    r, col = jnp.meshgrid(jnp.arange(rows, dtype=jnp.float32), jnp.arange(GRID_W, dtype=jnp.float32), indexing="ij")
    inv = 1.0 / (ROPE_THETA ** (jnp.arange(0, AXIS_DIM, 2, dtype=jnp.float32) / AXIS_DIM))
    ang = jnp.concatenate([r.reshape(-1, 1) * inv, col.reshape(-1, 1) * inv], axis=-1)
    return jnp.cos(ang), jnp.sin(ang)


def _apply_rope(x, cos, sin):
    cos = cos[None, :, None, :].astype(x.dtype)
    sin = sin[None, :, None, :].astype(x.dtype)
    x1, x2 = jnp.split(x, 2, axis=-1)
    return jnp.concatenate([x1 * cos - x2 * sin, x2 * cos + x1 * sin], axis=-1)


def _block_attention(q, k, v):
    b, n, h, d = q.shape
    kvh = k.shape[2]
    g = h // kvh
    qb = q.reshape(b, n // Q_BLOCK, Q_BLOCK, kvh, g, d).transpose(1, 0, 2, 3, 4, 5)
    scale = d ** -0.5

    def one_block(qblk):
        s = jnp.einsum("bqkgd,bmkd->bkgqm", qblk, k).astype(jnp.float32) * scale
        p = jax.nn.softmax(s, axis=-1).astype(v.dtype)
        return jnp.einsum("bkgqm,bmkd->bqkgd", p, v)

    o = lax.map(one_block, qb)
    return o.transpose(1, 0, 2, 3, 4, 5).reshape(b, n, h * d)


def _short_conv(z, w_conv):
    zp = jnp.pad(z, ((0, 0), (1, 1), (0, 0)))
    return w_conv[0] * zp[:, :-2] + w_conv[1] * zp[:, 1:-1] + w_conv[2] * zp[:, 2:]


def _context_kv(hc, w_in, g_k):
    b, m, _ = hc.shape
    k, v = jnp.split(hc @ w_in[:, ATTN_W:ATTN_W + 2 * KV_W], 2, axis=-1)
    k = _rms_norm(k.reshape(b, m, N_KV_HEADS, HEAD_DIM), g_k)
    return k, v.reshape(b, m, N_KV_HEADS, HEAD_DIM)


def _attn_conv_mixer(h, w_in, g_q, g_k, w_conv, w_out, rope, ext_kv):
    b, n, _ = h.shape
    q, k, v, gate_b, gate_c, hv = jnp.split(h @ w_in, IN_SPLITS, axis=-1)
    q = _rms_norm(q.reshape(b, n, N_HEADS, HEAD_DIM), g_q)
    k = _rms_norm(k.reshape(b, n, N_KV_HEADS, HEAD_DIM), g_k)
    v = v.reshape(b, n, N_KV_HEADS, HEAD_DIM)
    if rope is not None:
        q = _apply_rope(q, *rope)
        k = jnp.concatenate([ext_kv[0], _apply_rope(k, *rope)], axis=1)
        v = jnp.concatenate([ext_kv[1], v], axis=1)
    attn = _block_attention(q, k, v)
    conv = gate_b * _short_conv(gate_c * hv, w_conv)
    return jnp.concatenate([attn, conv], axis=-1) @ w_out


def _chunk_gmlp(h, w_in, g_v, b_v, w_s, b_s, w_out):
    b, n, _ = h.shape
    u, v = jnp.split(jax.nn.gelu(h @ w_in), 2, axis=-1)
    v = _layer_norm(v, g_v, b_v).reshape(b, n // CHUNK, CHUNK, GM_GROUPS, GM_GC)
    s = jnp.einsum("gpq,bcqgd->bcpgd", w_s, v) + b_s.T[:, :, None]
    return (u * s.reshape(b, n, GM_W)) @ w_out


def _swiglu(h, w_gate, w_up, w_down):
    return (jax.nn.silu(h @ w_gate) * (h @ w_up)) @ w_down


def _moe_swiglu(h, w_router, b_router, w_gate, w_up, w_down):
    logits = (h @ w_router).astype(jnp.float32) + b_router.astype(jnp.float32)
    top_v, top_i = lax.top_k(logits, TOP_K)
    wts = jax.nn.softmax(top_v, axis=-1)
    gates = jnp.sum(jax.nn.one_hot(top_i, N_EXPERTS, dtype=jnp.float32) * wts[..., None], axis=-2).astype(h.dtype)
    out = jnp.zeros_like(h)
    for e in range(N_EXPERTS):
        out = out + gates[..., e:e + 1] * _swiglu(h, w_gate[e], w_up[e], w_down[e])
    return out


def setup_inputs(seed: int = 0) -> dict:
    key = jax.random.key(seed)
    ks = list(jax.random.split(key, 48))
    D = D_MODEL
    ne, no = N_EVEN, N_ODD

    def nrm(shape, scale):
        return jax.random.normal(ks.pop(), shape, jnp.float32) * scale

    def gain(shape):
        return 1.0 + nrm(shape, 0.1)

    return {
        "x": nrm((BATCH, SEQ, D), 1.0),
        "c": nrm((BATCH, D), 1.0),
        "ctx": nrm((BATCH, CTX_LEN, D), 1.0),
        "c_ctx": nrm((D,), 1.0),
        "e_w_mod": nrm((ne, D, 6 * D), 0.5 * D ** -0.5),
        "e_b_mod": nrm((ne, 6 * D), 0.02),
        "e_g_pre_mix": gain((ne, D)),
        "e_g_post_mix": gain((ne, D)),
        "e_w_in": nrm((ne, D, IN_W), D ** -0.5),
        "e_g_q": gain((ne, HEAD_DIM)),
        "e_g_k": gain((ne, HEAD_DIM)),
        "e_w_conv": nrm((ne, CONV_TAPS, CONV_W), 0.5),
        "e_w_out": nrm((ne, ATTN_W + CONV_W, D), (ATTN_W + CONV_W) ** -0.5),
        "e_g_pre_ffn": gain((ne, D)),
        "e_g_post_ffn": gain((ne, D)),
        "e_w_gate": nrm((ne, D, D_FF), D ** -0.5),
        "e_w_up": nrm((ne, D, D_FF), D ** -0.5),
        "e_w_down": nrm((ne, D_FF, D), D_FF ** -0.5),
        "o_w_mod": nrm((no, D, 6 * D), 0.5 * D ** -0.5),
        "o_b_mod": nrm((no, 6 * D), 0.02),
        "o_g_pre_mix": gain((no, D)),
        "o_g_post_mix": gain((no, D)),
        "o_w_in": nrm((no, D, 2 * GM_W), D ** -0.5),
        "o_g_v": gain((no, GM_W)),
        "o_b_v": nrm((no, GM_W), 0.02),
        "o_w_s": nrm((no, GM_GROUPS, CHUNK, CHUNK), 0.5 * CHUNK ** -0.5),
        "o_b_s": gain((no, GM_GROUPS, CHUNK)),
        "o_w_out": nrm((no, GM_W, D), GM_W ** -0.5),
        "o_g_pre_ffn": gain((no, D)),
        "o_g_post_ffn": gain((no, D)),
        "o_w_router": nrm((no, D, N_EXPERTS), D ** -0.5),
        "o_b_router": nrm((no, N_EXPERTS), 0.01),
        "o_w_gate": nrm((no, N_EXPERTS, D, EXPERT_FF), D ** -0.5),
        "o_w_up": nrm((no, N_EXPERTS, D, EXPERT_FF), D ** -0.5),
        "o_w_down": nrm((no, N_EXPERTS, EXPERT_FF, D), EXPERT_FF ** -0.5),
    }


def reference(x, c, ctx, c_ctx,
              e_w_mod, e_b_mod, e_g_pre_mix, e_g_post_mix, e_w_in, e_g_q, e_g_k, e_w_conv, e_w_out,
              e_g_pre_ffn, e_g_post_ffn, e_w_gate, e_w_up, e_w_down,
              o_w_mod, o_b_mod, o_g_pre_mix, o_g_post_mix, o_w_in, o_g_v, o_b_v, o_w_s, o_b_s, o_w_out,
              o_g_pre_ffn, o_g_post_ffn, o_w_router, o_b_router, o_w_gate, o_w_up, o_w_down):
    rope = _axial_rope(x.shape[1])
    for layer in range(DEPTH):
        i = layer // 2
        ctx_read_later = any(l % 2 == 0 for l in range(layer + 1, DEPTH))
        if layer % 2 == 0:
            mx = _modulation(c[:, None, :], e_w_mod[i], e_b_mod[i])
            mc = _modulation(c_ctx, e_w_mod[i], e_b_mod[i])
            hc = _pre(ctx, e_g_pre_mix[i], mc[0], mc[1])
            ctx_kv = _context_kv(hc, e_w_in[i], e_g_k[i])
            hx = _pre(x, e_g_pre_mix[i], mx[0], mx[1])
            y = _attn_conv_mixer(hx, e_w_in[i], e_g_q[i], e_g_k[i], e_w_conv[i], e_w_out[i], rope, ctx_kv)
            x = x + mx[2] * _rms_norm(y, e_g_post_mix[i])
            f = _swiglu(_pre(x, e_g_pre_ffn[i], mx[3], mx[4]), e_w_gate[i], e_w_up[i], e_w_down[i])
            x = x + mx[5] * _rms_norm(f, e_g_post_ffn[i])
            if ctx_read_later:
                yc = _attn_conv_mixer(hc, e_w_in[i], e_g_q[i], e_g_k[i], e_w_conv[i], e_w_out[i], None, None)
                ctx = ctx + mc[2] * _rms_norm(yc, e_g_post_mix[i])
                fc = _swiglu(_pre(ctx, e_g_pre_ffn[i], mc[3], mc[4]), e_w_gate[i], e_w_up[i], e_w_down[i])
                ctx = ctx + mc[5] * _rms_norm(fc, e_g_post_ffn[i])
        else:
            mx = _modulation(c[:, None, :], o_w_mod[i], o_b_mod[i])
            hx = _pre(x, o_g_pre_mix[i], mx[0], mx[1])
            y = _chunk_gmlp(hx, o_w_in[i], o_g_v[i], o_b_v[i], o_w_s[i], o_b_s[i], o_w_out[i])
            x = x + mx[2] * _rms_norm(y, o_g_post_mix[i])
            f = _moe_swiglu(_pre(x, o_g_pre_ffn[i], mx[3], mx[4]), o_w_router[i], o_b_router[i],
                            o_w_gate[i], o_w_up[i], o_w_down[i])
            x = x + mx[5] * _rms_norm(f, o_g_post_ffn[i])
            if ctx_read_later:
                mc = _modulation(c_ctx, o_w_mod[i], o_b_mod[i])
                hc = _pre(ctx, o_g_pre_mix[i], mc[0], mc[1])
                yc = _chunk_gmlp(hc, o_w_in[i], o_g_v[i], o_b_v[i], o_w_s[i], o_b_s[i], o_w_out[i])
                ctx = ctx + mc[2] * _rms_norm(yc, o_g_post_mix[i])
                fc = _moe_swiglu(_pre(ctx, o_g_pre_ffn[i], mc[3], mc[4]), o_w_router[i], o_b_router[i],
                                 o_w_gate[i], o_w_up[i], o_w_down[i])
                ctx = ctx + mc[5] * _rms_norm(fc, o_g_post_ffn[i])
    return x
```

```python
import numpy as np
from contextlib import ExitStack
import concourse.bass as bass
import concourse.mybir as mybir
from concourse.bass_utils import run_bass_kernel_spmd

F32 = mybir.dt.float32
BF16 = mybir.dt.bfloat16
AF = mybir.ActivationFunctionType
ALU = mybir.AluOpType
AX = mybir.AxisListType

D = 1024
SEQ = 8192
NB = 4
CTX = 256
NOWN = 4096
NT_OWN = 32
EPS = 1e-6
IN_W = 2304
D_FF = 2816
E_FF = 3584
NEXP = 8


class Buf:
    __slots__ = ("name", "w", "r")

    def __init__(self, name):
        self.name = name
        self.w = None
        self.r = {}


class Eng:
    def __init__(self, nc, name, eng):
        self.name = name
        self.e = eng
        self.sem = nc.alloc_semaphore("sem_" + name)
        self.cnt = 0
        self.waited = {}

    def wait(self, tok):
        if tok is None:
            return
        key, sem, val, _ = tok
        if self.waited.get(key, 0) >= val:
            return
        self.e.wait_ge(sem, val)
        self.waited[key] = val


class Prog:
    def __init__(self, nc):
        self.nc = nc
        self.PE = Eng(nc, "pe", nc.tensor)
        self.ACT = Eng(nc, "act", nc.scalar)
        self.DVE = Eng(nc, "dve", nc.vector)
        self.POOL = Eng(nc, "pool", nc.gpsimd)
        self.SP = Eng(nc, "sp", nc.sync)
        self.dsem = {}
        for q, n in (("sp", 12), ("pool", 8)):
            self.dsem[q] = [[nc.alloc_semaphore(f"dq_{q}{i}"), 0, f"dq_{q}{i}"] for i in range(n)]
        self.dnext = {"sp": 0, "pool": 0}

    def _deps(self, E, reads, writes):
        for b in reads:
            E.wait(b.w)
        for b in writes:
            if b.w is not None and not (E.name == "pe" and b.w[3] == "pe"):
                E.wait(b.w)
            for t in b.r.values():
                if not (t[3] == E.name and E.name == "pe"):
                    E.wait(t)

    def _record(self, tok, ename, reads, writes):
        for b in reads:
            b.r[ename] = tok
        for b in writes:
            b.w = tok
            b.r = {}

    def op(self, E, fn, reads=(), writes=(), inc=True):
        self._deps(E, reads, writes)
        ins = fn()
        if inc:
            E.cnt += 1
            ins.then_inc(E.sem, 1)
            tok = (E.name, E.sem, E.cnt, E.name)
        else:
            tok = (E.name, E.sem, E.cnt + 1, E.name)
        self._record(tok, E.name, reads, writes)
        return ins

    def dma(self, Q, out, in_, reads=(), writes=(), slow=False):
        q = Q.name
        slot = self.dsem[q][self.dnext[q]]
        self.dnext[q] = (self.dnext[q] + 1) % len(self.dsem[q])
        if slot[1] > 0:
            Q.wait((slot[2], slot[0], slot[1], "dma"))
        self._deps(Q, reads, writes)
        if slow:
            ins = Q.e.dma_start(out=out, in_=in_, allow_slow_non_contiguous=True)
        else:
            ins = Q.e.dma_start(out=out, in_=in_)
        slot[1] += 16
        ins.then_inc(slot[0], 16)
        tok = (slot[2], slot[0], slot[1], "dma")
        for b in reads:
            b.r["dma_" + slot[2]] = tok
        for b in writes:
            b.w = tok
            b.r = {}
        return tok

    def dma_ind(self, out, out_offset, in_, in_offset, reads=(), writes=()):
        Q = self.POOL
        q = "pool"
        slot = self.dsem[q][self.dnext[q]]
        self.dnext[q] = (self.dnext[q] + 1) % len(self.dsem[q])
        if slot[1] > 0:
            Q.wait((slot[2], slot[0], slot[1], "dma"))
        self._deps(Q, reads, writes)
        ins = Q.e.indirect_dma_start(out=out, out_offset=out_offset, in_=in_, in_offset=in_offset)
        slot[1] += 16
        ins.then_inc(slot[0], 16)
        tok = (slot[2], slot[0], slot[1], "dma")
        for b in reads:
            b.r["dma_" + slot[2]] = tok
        for b in writes:
            b.w = tok
            b.r = {}
        return tok

    def predicated(self, regs, thresh, body):
        nc = self.nc
        engs = (self.PE, self.ACT, self.DVE, self.POOL, self.SP)
        cnt0 = {E.name: E.cnt for E in engs}
        waited0 = {E.name: dict(E.waited) for E in engs}
        d0 = {q: [sl[1] for sl in slots] for q, slots in self.dsem.items()}
        with nc.If_cmp(regs, thresh, "IS_GT"):
            body()
        for E in engs:
            E.waited = waited0[E.name]
        with nc.Else():
            for E in engs:
                delta = E.cnt - cnt0[E.name]
                if delta > 0:
                    E.e.drain()
                    E.e.sem_inc(E.sem, delta)
            for q, slots in self.dsem.items():
                Q = self.SP if q == "sp" else self.POOL
                for i, sl in enumerate(slots):
                    delta = sl[1] - d0[q][i]
                    if delta > 0:
                        if d0[q][i] > 0:
                            Q.e.wait_ge(sl[0], d0[q][i])
                        Q.e.sem_inc(sl[0], delta)
        for E in engs:
            E.waited = waited0[E.name]
        if getattr(self, "_dbgreg", False):
            self._nreg = getattr(self, "_nreg", 0) + 1
            for E in engs:
                got = []
                try:
                    while True:
                        got.append(E.e.alloc_register(f"probe_{E.name}_{self._nreg}_{len(got)}"))
                except Exception:
                    pass
                for r in got:
                    E.e.free_register(r)
                if self._nreg <= 3 or self._nreg % 20 == 0:
                    print("region", self._nreg, E.name, "free regs", len(got), flush=True)

    def barrier(self):
        engs = (self.PE, self.ACT, self.DVE, self.POOL, self.SP)
        for E in engs:
            for O in engs:
                if O is not E and O.cnt > 0:
                    E.wait((O.name, O.sem, O.cnt, O.name))
            self.drain_all(E)

    def drain_all(self, E):
        for q in self.dsem.values():
            for slot in q:
                if slot[1] > 0:
                    E.wait((slot[2], slot[0], slot[1], "dma"))


def _build(cfg):
    dbg = cfg.get("dbg", False)
    phases = cfg.get("phases", "0ABCD")
    nblk_a = cfg.get("nblk_a", 8)
    nc = bass.Bass("TRN2", target_bir_lowering=False)
    es = ExitStack()
    P = Prog(nc)
    PE, ACT, DVE, POOL, SP = P.PE, P.ACT, P.DVE, P.POOL, P.SP

    def din(name, shape, dt=F32):
        return nc.dram_tensor(name, list(shape), dt, kind="ExternalInput").ap()

    def dscr(name, shape, dt=F32, out=False):
        kind = "ExternalOutput" if out else "Internal"
        return nc.dram_tensor(name, list(shape), dt, kind=kind).ap()

    def sb(name, shape, dt=F32):
        return es.enter_context(nc.sbuf_tensor(name, list(shape), dt))

    xown = din("xown", [NOWN, D])
    xoth = din("xoth", [NOWN, D])
    ctx = din("ctx", [CTX, D])
    cvecT = din("cvecT", [128, 16])
    rope = din("rope", [8192, 64])
    ident_in = din("ident", [128, 128])
    halo_mask = din("halo_mask", [128, 2])
    W = {}
    for pre, win_w in (("e", IN_W), ("o", 2048)):
        W[pre + "_w_mod"] = din(pre + "_w_mod", [D, 6 * D])
        W[pre + "_bmodT"] = din(pre + "_bmodT", [128, 48])
        W[pre + "_b_mod"] = din(pre + "_b_mod", [1, 6 * D])
        for v in ("g_pre_mix", "g_pre_ffn"):
            W[pre + "_" + v + "T"] = din(pre + "_" + v + "T", [128, 8])
        for v in ("g_post_mix", "g_post_ffn"):
            W[pre + "_" + v] = din(pre + "_" + v, [1, D])
        W[pre + "_w_in"] = din(pre + "_w_in", [D, win_w])
        W[pre + "_w_out"] = din(pre + "_w_out", [D, D])
    W["e_g_q"] = din("e_g_q", [1, 64])
    W["e_g_k"] = din("e_g_k", [1, 64])
    W["e_w_convT"] = din("e_w_convT", [128, 12])
    W["e_w_gate"] = din("e_w_gate", [D, D_FF])
    W["e_w_up"] = din("e_w_up", [D, D_FF])
    W["e_w_down"] = din("e_w_down", [D_FF, D])
    W["o_g_vT"] = din("o_g_vT", [128, 8])
    W["o_b_vT"] = din("o_b_vT", [128, 8])
    W["o_w_s"] = din("o_w_s", [8, 128, 128])
    W["o_b_s"] = din("o_b_s", [8, 128])
    W["o_w_router"] = din("o_w_router", [D, NEXP])
    W["o_b_router"] = din("o_b_router", [1, NEXP])
    W["o_w_gate"] = din("o_w_gate", [NEXP, D, E_FF])
    W["o_w_up"] = din("o_w_up", [NEXP, D, E_FF])
    W["o_w_down"] = din("o_w_down", [NEXP, E_FF, D])

    out_d = nc.dram_tensor("out", [NOWN, D], F32, kind="ExternalOutput").ap()
    xA = dscr("xA", [NOWN, D], out=dbg)
    xB = dscr("xB", [NOWN, D], out=dbg)
    xC = dscr("xC", [NOWN, D], out=dbg)
    hT_scr = dscr("hT_scr", [8, 128, NOWN + 2], BF16, out=dbg)

    ident_f = sb("ident_f", [128, 128])
    ident_b = sb("ident_b", [128, 128], BF16)
    eps_t = sb("eps_t", [128, 1])
    ones_f = sb("ones_f", [128, 64])
    ones_f_full = sb("ones_f_full", [128, 128])
    hmask = sb("hmask", [128, 2])
    AS = {}
    for L in ("e", "o"):
        for k in ("A_mix", "S_mix", "A_ffn", "S_ffn"):
            AS[L + k] = sb(f"{L}{k}", [128, 8, 2])
    Gt = {k: sb("G_" + k, [128, D]) for k in ("e_mix", "e_ffn", "o_mix", "o_ffn")}
    B_consts = Buf("consts")
    B_mod = Buf("mod")

    PS = [es.enter_context(nc.psum_tensor(f"ps{i}", [128, 1024], F32)) for i in range(4)]
    PSB = [[Buf(f"ps{i}a"), Buf(f"ps{i}b")] for i in range(4)]

    def psb16(i, half):
        return PS[i][:, half * 512:(half + 1) * 512].bitcast(BF16)

    P.dma(SP, ident_f[:], ident_in[:], writes=[B_consts])
    P.dma(SP, hmask[:], halo_mask[:], writes=[B_consts])
    P.op(DVE, lambda: nc.vector.tensor_copy(out=ident_b[:], in_=ident_f[:]), reads=[B_consts], writes=[B_consts])
    P.op(DVE, lambda: nc.vector.memset(eps_t[:], EPS), writes=[B_consts])
    P.op(DVE, lambda: nc.vector.memset(ones_f[:], 1.0), writes=[B_consts])
    P.op(DVE, lambda: nc.vector.memset(ones_f_full[:], 1.0), writes=[B_consts])

    def rstd_from_ss(ss_ap, n, tmp_ap, out_ap, bufs_r, bufs_w, width=1):
        P.op(ACT, lambda: nc.scalar.activation(out=tmp_ap, in_=ss_ap, func=AF.Sqrt, bias=eps_t[:], scale=1.0 / n),
             reads=bufs_r + [B_consts], writes=bufs_w)
        P.op(DVE, lambda: nc.vector.reciprocal(out=out_ap, in_=tmp_ap), reads=bufs_w, writes=bufs_w)

    def phase0():
        st = ExitStack()

        def sbt(name, shape, dt=F32):
            return st.enter_context(nc.sbuf_tensor(name, list(shape), dt))
        cs_raw = sbt("cs_raw", [128, 16])
        cs = sbt("cs", [128, 16], BF16)
        csb = sbt("csb", [128, 8, 128], BF16)
        wm = [sbt(f"wm{i}", [128, 8, 512], BF16) for i in range(2)]
        Bwm = [Buf("wm0"), Buf("wm1")]
        modt = sbt("modt", [128, 48, 2])
        bmodT = sbt("bmodT", [128, 48])
        gpreT = sbt("gpreT", [128, 16])
        brow = sbt("brow", [128, 512])
        grow = sbt("grow", [128, 512])
        Bc = Buf("cs")
        Bm = Buf("modt")
        Bv = Buf("vecs")
        Brow = Buf("rows")
        P.dma(SP, cs_raw[:], cvecT[:], writes=[Bc])
        P.op(ACT, lambda: nc.scalar.activation(out=cs[:], in_=cs_raw[:], func=AF.Silu), reads=[Bc], writes=[Bc])
        for kc in range(8):
            P.op(DVE, lambda kc=kc: nc.vector.tensor_copy(out=csb[:, kc, :], in_=cs[:, 2 * kc:2 * kc + 1].to_broadcast([128, 128])),
                 reads=[Bc], writes=[Bc])
        pi = 0
        for L in ("e", "o"):
            wmod = W[L + "_w_mod"]
            P.dma(SP, bmodT[:], W[L + "_bmodT"][:], writes=[Bv])
            P.dma(SP, gpreT[:, 0:8], W[L + "_g_pre_mixT"][:], writes=[Bv])
            P.dma(SP, gpreT[:, 8:16], W[L + "_g_pre_ffnT"][:], writes=[Bv])
            for piece in range(12):
                s, half = piece // 2, piece % 2
                wb, Bw = wm[pi % 2], Bwm[pi % 2]
                pi += 1
                src = wmod[:, piece * 512:(piece + 1) * 512].rearrange("(kc p) n -> p kc n", p=128)
                P.dma(POOL, wb[:], src, writes=[Bw])
                if s in (0, 1, 3, 4):
                    for j in range(4):
                        idx = s * 8 + half * 4 + j
                        for kc in range(8):
                            P.op(PE, lambda kc=kc, j=j, idx=idx, wb=wb: nc.tensor.matmul(
                                PS[3][:, 2 * idx:2 * idx + 2], lhsT=wb[:, kc, j * 128:(j + 1) * 128],
                                rhs=cs[:, 2 * kc:2 * kc + 2], start=(kc == 0), stop=(kc == 7)),
                                reads=[Bw, Bc], writes=[PSB[3][0]], inc=(kc == 7))
                else:
                    for kc in range(8):
                        P.op(PE, lambda kc=kc, wb=wb: nc.tensor.matmul(
                            PS[3][:, 512:1024], lhsT=csb[:, kc, :], rhs=wb[:, kc, :],
                            start=(kc == 0), stop=(kc == 7)),
                            reads=[Bw, Bc], writes=[PSB[3][1]], inc=(kc == 7))
                    key = L + ("_mix" if s == 2 else "_ffn")
                    col0 = s * D + half * 512
                    P.dma(SP, brow[:], W[L + "_b_mod"][:, col0:col0 + 512].partition_broadcast(128), writes=[Brow])
                    gp = W[L + ("_g_post_mix" if s == 2 else "_g_post_ffn")]
                    P.dma(SP, grow[:], gp[:, half * 512:(half + 1) * 512].partition_broadcast(128), writes=[Brow])
                    gdst = Gt[key][:, half * 512:(half + 1) * 512]
                    P.op(DVE, lambda gdst=gdst: nc.vector.tensor_tensor(out=gdst, in0=PS[3][:, 512:1024], in1=brow[:], op=ALU.add),
                         reads=[PSB[3][1], Brow], writes=[B_mod])
                    P.op(DVE, lambda gdst=gdst: nc.vector.tensor_tensor(out=gdst, in0=gdst, in1=grow[:], op=ALU.mult),
                         reads=[B_mod, Brow], writes=[B_mod, Brow])
            for j0 in (0, 24):
                P.op(DVE, lambda j0=j0: nc.vector.tensor_tensor(
                    out=modt[:, j0:j0 + 16, :], in0=PS[3][:, 2 * j0:2 * j0 + 32].rearrange("p (j r) -> p j r", r=2),
                    in1=bmodT[:, j0:j0 + 16].unsqueeze(2).to_broadcast([128, 16, 2]), op=ALU.add),
                    reads=[PSB[3][0], Bv], writes=[Bm])
            for nm, s_shift, s_scale, goff in (("mix", 0, 1, 0), ("ffn", 3, 4, 8)):
                A = AS[L + "A_" + nm]
                S = AS[L + "S_" + nm]
                P.op(DVE, lambda S=S, s_shift=s_shift: nc.vector.tensor_copy(out=S[:], in_=modt[:, s_shift * 8:s_shift * 8 + 8, :]),
                     reads=[Bm], writes=[B_mod])
                P.op(DVE, lambda A=A, s_scale=s_scale: nc.vector.tensor_scalar(
                    out=A[:], in0=modt[:, s_scale * 8:s_scale * 8 + 8, :], scalar1=1.0, scalar2=None, op0=ALU.add),
                    reads=[Bm], writes=[B_mod])
                P.op(DVE, lambda A=A, goff=goff: nc.vector.tensor_tensor(
                    out=A[:], in0=A[:], in1=gpreT[:, goff:goff + 8].unsqueeze(2).to_broadcast([128, 8, 2]), op=ALU.mult),
                    reads=[B_mod, Bv], writes=[B_mod, Bv, Bm])
        P.barrier()
        st.close()

    def prenorm_a(x_t, Bx, wk):
        P.op(ACT, lambda: nc.scalar.activation(out=wk["junk"][:], in_=x_t, func=AF.Square, accum_out=wk["ss"][:]),
             reads=[Bx], writes=[wk["Bs"]])
        rstd_from_ss(wk["ss"][:], D, wk["sd"][:], wk["rstd"][:], [wk["Bs"]], [wk["Bs"]])
        P.op(DVE, lambda: nc.vector.tensor_scalar(out=wk["xn"][:], in0=x_t, scalar1=wk["rstd"][:], scalar2=None, op0=ALU.mult),
             reads=[Bx, wk["Bs"]], writes=[wk["Bxn"]])

    def prenorm_b(A, S, r, hT_dst, B_h, wk, psi):
        pi_, ph = psi
        pv = psb16(pi_, ph)
        for c in range(8):
            P.op(PE, lambda c=c: nc.tensor.transpose(out=pv[:, c * 128:(c + 1) * 128], in_=wk["xn"][:, c * 128:(c + 1) * 128], identity=ident_b[:]),
                 reads=[wk["Bxn"], B_consts], writes=[PSB[pi_][ph]], inc=(c == 7))
        for c in range(8):
            if c % 2 == 0:
                P.op(DVE, lambda c=c: nc.vector.tensor_scalar(
                    out=hT_dst[:, c, :], in0=pv[:, c * 128:(c + 1) * 128], scalar1=A[:, c, r:r + 1], scalar2=S[:, c, r:r + 1],
                    op0=ALU.mult, op1=ALU.add), reads=[PSB[pi_][ph], B_mod], writes=[B_h])
            else:
                P.op(ACT, lambda c=c: nc.scalar.activation(
                    out=hT_dst[:, c, :], in_=pv[:, c * 128:(c + 1) * 128], func=AF.Identity,
                    bias=S[:, c, r:r + 1], scale=A[:, c, r:r + 1]), reads=[PSB[pi_][ph], B_mod], writes=[B_h])

    def prenorm_tile(x_t, Bx, A, S, r, hT_dst, B_h, wk, psi, fp32=False):
        prenorm_a(x_t, Bx, wk)
        prenorm_b(A, S, r, hT_dst, B_h, wk, psi)

    def epilogue(y_ap, By, x_src_dram, G, out_dram, wk):
        P.dma(SP, wk["x"][:], x_src_dram, writes=[wk["Bx"]])
        P.op(ACT, lambda: nc.scalar.activation(out=wk["junk"][:], in_=y_ap, func=AF.Square, accum_out=wk["ss"][:]),
             reads=By, writes=[wk["Bs"]])
        rstd_from_ss(wk["ss"][:], D, wk["sd"][:], wk["rstd"][:], [wk["Bs"]], [wk["Bs"]])
        P.op(DVE, lambda: nc.vector.scalar_tensor_tensor(out=wk["t"][:], in0=y_ap, scalar=wk["rstd"][:], in1=G[:], op0=ALU.mult, op1=ALU.mult),
             reads=By + [wk["Bs"], B_mod], writes=[wk["Bt"]])
        P.op(POOL, lambda: nc.gpsimd.tensor_tensor(out=wk["x"][:], in0=wk["x"][:], in1=wk["t"][:], op=ALU.add),
             reads=[wk["Bt"]], writes=[wk["Bx"]])
        P.dma(SP, out_dram, wk["x"][:], reads=[wk["Bx"]])

    def mk_wk(sbt, tag, n=2, with_x=True, with_xn=True):
        res = []
        for i in range(n):
            wk = {}
            wk["ss"] = sbt(f"{tag}ss{i}", [128, 1])
            wk["sd"] = sbt(f"{tag}sd{i}", [128, 1])
            wk["rstd"] = sbt(f"{tag}rstd{i}", [128, 1])
            wk["junk"] = sbt(f"{tag}junk{i}", [128, D], BF16)
            if with_xn:
                wk["xn"] = sbt(f"{tag}xn{i}", [128, D], BF16)
            wk["Bs"] = Buf("Bs")
            wk["Bxn"] = Buf("Bxn")
            if with_x:
                wk["x"] = sbt(f"{tag}x{i}", [128, D])
                wk["t"] = sbt(f"{tag}t{i}", [128, D])
                wk["Bx"] = Buf("Bx")
                wk["Bt"] = Buf("Bt")
            res.append(wk)
        return res

    def phaseA():
        st = ExitStack()

        def sbt(name, shape, dt=F32):
            return st.enter_context(nc.sbuf_tensor(name, list(shape), dt))
        NKT = 66
        A, S = AS["eA_mix"], AS["eS_mix"]
        w_in = W["e_w_in"]
        wqs = sbt("wqs", [128, 8, 2048], BF16)
        woA = sbt("woA", [64, 8, D], BF16)
        woC = sbt("woC", [128, 4, D], BF16)
        Bw = Buf("wA")
        w_in_v = w_in.rearrange("(kc p) n -> p kc n", p=128)
        for kc in range(8):
            P.dma(POOL, wqs[:, kc, 0:512], w_in_v[:, kc, 0:512], writes=[Bw])
            P.dma(POOL, wqs[:, kc, 512:2048], w_in_v[:, kc, 768:2304], writes=[Bw])
        wo = W["e_w_out"]
        P.dma(POOL, woA[:], wo[0:512, :].rearrange("(h p) n -> p h n", p=64), writes=[Bw])
        P.dma(POOL, woC[:], wo[512:1024, :].rearrange("(c p) n -> p c n", p=128), writes=[Bw])
        KT = sbt("KT", [128, 2, NKT * 128], BF16)
        VA = sbt("VA", [128, NKT, 2, 66], BF16)
        B_KV = Buf("KV")
        P.op(DVE, lambda: nc.vector.memset(VA[:, :, :, 64:66], 1.0), writes=[B_KV])
        gqB = sbt("gqB", [128, 64])
        gkB = sbt("gkB", [128, 64])
        wconv = sbt("wconv", [128, 12])
        Bg = Buf("g")
        P.dma(SP, gqB[:], W["e_g_q"][:].partition_broadcast(128), writes=[Bg])
        P.dma(SP, gkB[:], W["e_g_k"][:].partition_broadcast(128), writes=[Bg])
        P.dma(SP, wconv[:], W["e_w_convT"][:], writes=[Bg])

        NS = 2
        st_outer = st
        st = ExitStack()
        wkv = sbt("wkv", [128, 8, 256], BF16)
        P.dma(POOL, wkv[:], w_in_v[:, :, 512:768], writes=[Bw])
        wks = mk_wk(sbt, "a1", NS, with_x=True)
        hTt = [sbt(f"a1hT{i}", [128, 8, 128], BF16) for i in range(NS)]
        Bh = [Buf("hTt") for _ in range(NS)]
        ropet = [sbt(f"a1rope{i}", [128, 64]) for i in range(NS)]
        ksq = [sbt(f"a1ksq{i}", [128, 128]) for i in range(NS)]
        kss = [sbt(f"a1kss{i}", [128, 2]) for i in range(NS)]
        ksd = [sbt(f"a1ksd{i}", [128, 2]) for i in range(NS)]
        krs = [sbt(f"a1krs{i}", [128, 2]) for i in range(NS)]
        kn = [sbt(f"a1kn{i}", [128, 2, 64]) for i in range(NS)]
        ktmp = [sbt(f"a1ktmp{i}", [128, 4, 2, 32]) for i in range(NS)]
        kd = [sbt(f"a1kd{i}", [128, 2, 2, 64], BF16) for i in range(NS)]
        Bk = [Buf("k") for _ in range(NS)]
        Bkd = [Buf("kd") for _ in range(NS)]
        Brope = [Buf("rope") for _ in range(NS)]
        def a1_stage1(kt):
            sl = kt % NS
            wk = wks[sl]
            if kt < 2:
                src, r = ctx[kt * 128:(kt + 1) * 128, :], 1
            elif kt < 34:
                src, r = xown[(kt - 2) * 128:(kt - 1) * 128, :], 0
            else:
                src, r = xoth[(kt - 34) * 128:(kt - 33) * 128, :], 0
            P.dma(SP, wk["x"][:], src, writes=[wk["Bx"]])
            if kt >= 2:
                P.dma(SP, ropet[sl][:], rope[(kt - 2) * 128:(kt - 1) * 128, :], writes=[Brope[sl]])
            prenorm_tile(wk["x"][:], wk["Bx"], A, S, r, hTt[sl], Bh[sl], wk, (kt % 2, 0))
        def a1_stage2(kt):
            sl = kt % NS
            kb = kt % 2
            for kc in range(8):
                P.op(PE, lambda kc=kc, sl=sl: nc.tensor.matmul(PS[2][:, kb * 512:kb * 512 + 256], lhsT=hTt[sl][:, kc, :], rhs=wkv[:, kc, :],
                                                             start=(kc == 0), stop=(kc == 7)),
                     reads=[Bh[sl], Bw], writes=[PSB[2][kb]], inc=(kc == 7))
            kps = PS[2][:, kb * 512:kb * 512 + 128]
            vps = PS[2][:, kb * 512 + 128:kb * 512 + 256]
            P.op(ACT, lambda sl=sl: nc.scalar.activation(out=VA[:, kt, :, 0:64], in_=vps.rearrange("p (g d) -> p g d", g=2), func=AF.Copy),
                 reads=[PSB[2][kb]], writes=[B_KV])
            P.op(ACT, lambda sl=sl: nc.scalar.activation(out=ksq[sl][:], in_=kps, func=AF.Square), reads=[PSB[2][kb]], writes=[Bk[sl]])
            P.op(DVE, lambda sl=sl: nc.vector.tensor_reduce(out=kss[sl][:], in_=ksq[sl][:].rearrange("p (g d) -> p g d", g=2), axis=AX.X, op=ALU.add),
                 reads=[Bk[sl]], writes=[Bk[sl]])
            rstd_from_ss(kss[sl][:], 64, ksd[sl][:], krs[sl][:], [Bk[sl]], [Bk[sl]])
            P.op(DVE, lambda sl=sl: nc.vector.tensor_tensor(out=kn[sl][:], in0=kps.rearrange("p (g d) -> p g d", g=2),
                                                          in1=krs[sl][:].unsqueeze(2).to_broadcast([128, 2, 64]), op=ALU.mult),
                 reads=[PSB[2][kb], Bk[sl]], writes=[Bk[sl]])
            kdv = kd[sl]
            if kt < 2:
                P.op(DVE, lambda sl=sl, kdv=kdv: nc.vector.tensor_tensor(out=kdv[:, :, 0, :], in0=kn[sl][:],
                                                                       in1=gkB[:].unsqueeze(1).to_broadcast([128, 2, 64]), op=ALU.mult),
                     reads=[Bk[sl], Bg], writes=[Bkd[sl]])
            else:
                P.op(DVE, lambda sl=sl: nc.vector.tensor_tensor(out=kn[sl][:], in0=kn[sl][:],
                                                              in1=gkB[:].unsqueeze(1).to_broadcast([128, 2, 64]), op=ALU.mult),
                     reads=[Bk[sl], Bg], writes=[Bk[sl]])
                cosb = ropet[sl][:, 0:32].unsqueeze(1).to_broadcast([128, 2, 32])
                sinb = ropet[sl][:, 32:64].unsqueeze(1).to_broadcast([128, 2, 32])
                k1, k2 = kn[sl][:, :, 0:32], kn[sl][:, :, 32:64]
                tt = ktmp[sl]
                for j, (a, b_) in enumerate(((k1, cosb), (k2, sinb), (k2, cosb), (k1, sinb))):
                    P.op(DVE, lambda j=j, a=a, b_=b_, tt=tt: nc.vector.tensor_tensor(out=tt[:, j], in0=a, in1=b_, op=ALU.mult),
                         reads=[Bk[sl], Brope[sl]], writes=[Bk[sl]])
                P.op(DVE, lambda tt=tt, kdv=kdv: nc.vector.tensor_tensor(out=kdv[:, :, 0, 0:32], in0=tt[:, 0], in1=tt[:, 1], op=ALU.subtract),
                     reads=[Bk[sl]], writes=[Bkd[sl]])
                P.op(DVE, lambda tt=tt, kdv=kdv: nc.vector.tensor_tensor(out=kdv[:, :, 0, 32:64], in0=tt[:, 2], in1=tt[:, 3], op=ALU.add),
                     reads=[Bk[sl]], writes=[Bkd[sl]])
            P.op(DVE, lambda kdv=kdv: nc.vector.tensor_copy(out=kdv[:, :, 1, :], in_=kdv[:, :, 0, :]), reads=[Bkd[sl]], writes=[Bkd[sl]])
            pv = psb16(3, kb)
            for g in range(2):
                P.op(PE, lambda g=g, kdv=kdv: nc.tensor.transpose(out=pv[:, g * 128:(g + 1) * 128],
                                                                 in_=kdv[:, g].rearrange("p a d -> p (a d)"), identity=ident_b[:]),
                     reads=[Bkd[sl], B_consts], writes=[PSB[3][kb]], inc=(g == 1))
            P.op(DVE, lambda: nc.vector.tensor_copy(out=KT[:, :, kt * 128:(kt + 1) * 128], in_=pv[:, 0:256].rearrange("p (g t) -> p g t", g=2)),
                 reads=[PSB[3][kb]], writes=[B_KV])
            if 2 <= kt < 34:
                c0 = 1 + (kt - 2) * 128
                P.dma(SP, hT_scr[:, :, c0:c0 + 128].rearrange("c p t -> p c t"), hTt[sl][:], reads=[Bh[sl]])
            if kt == 34:
                P.dma(SP, hT_scr[:, :, NOWN + 1:NOWN + 2].rearrange("c p t -> p c t"), hTt[sl][:, :, 0:1], reads=[Bh[sl]], slow=True)
            if kt == 65:
                P.dma(SP, hT_scr[:, :, 0:1].rearrange("c p t -> p c t"), hTt[sl][:, :, 127:128], reads=[Bh[sl]], slow=True)
        a1_stage1(0)
        for kt in range(NKT):
            if kt + 1 < NKT:
                a1_stage1(kt + 1)
            a1_stage2(kt)
        B_scr = Buf("scr")
        P.barrier()
        st.close()
        st = st_outer

        hTb = [sbt(f"a2hT{i}", [128, 8, 514], BF16) for i in range(2)]
        BhT = [Buf("hTb") for _ in range(2)]
        qT = sbt("qT", [128, 4, 512], BF16)
        BqT = Buf("qT")
        qsq = sbt("qsq", [128, 512])
        qss = sbt("qss", [128, 8])
        qsd = sbt("qsd", [128, 8])
        qrs = sbt("qrs", [128, 8])
        qn = sbt("qn", [128, 8, 64])
        qtmp = sbt("qtmp", [128, 4, 8, 32])
        qr = sbt("qr", [128, 8, 64], BF16)
        ropeq = sbt("ropeq", [128, 64])
        Bq = Buf("q")
        Bqr = Buf("qr")
        Bropeq = Buf("ropeq")
        Zc = sbt("Zc", [128, 514])
        Z = sbt("Z", [128, 514])
        cv = sbt("cv", [128, 512])
        convT = sbt("convT", [128, 4, 512], BF16)
        Bz = Buf("Z")
        Bconv = Buf("convT")
        PT = [sbt(f"PT{i}", [128, 1024], BF16) for i in range(3)]
        BPT = [Buf("PT") for _ in range(3)]
        attnT = sbt("attnT", [64, 8, 512], BF16)
        Battn = Buf("attnT")
        oT = [sbt(f"oT{i}", [128, 512]) for i in range(2)]
        BoT = [Buf("oT") for _ in range(2)]
        bcb = [sbt(f"bcb{i}", [64, 512]) for i in range(2)]
        Bbcb = [Buf("bcb") for _ in range(2)]
        rcd = dscr("rcd", [4, 512])
        Brcd = [Buf("rcd") for _ in range(4)]
        rci = 0
        ewk = mk_wk(sbt, "a2e", 2, with_x=True, with_xn=False)
        pti = 0
        for blk in range(nblk_a):
            hb, Bhb = hTb[blk % 2], BhT[blk % 2]
            P.dma(SP, hb[:], hT_scr[:, :, blk * 512:blk * 512 + 514].rearrange("c p t -> p c t"), writes=[Bhb])
            for t in range(4):
                tok0 = blk * 512 + t * 128
                P.dma(SP, ropeq[:], rope[tok0:tok0 + 128, :], writes=[Bropeq])
                for kc in range(8):
                    P.op(PE, lambda kc=kc, t=t: nc.tensor.matmul(PS[3][:, 0:512], lhsT=hb[:, kc, 1 + t * 128:1 + (t + 1) * 128], rhs=wqs[:, kc, 0:512],
                                                                 start=(kc == 0), stop=(kc == 7)),
                         reads=[Bhb, Bw], writes=[PSB[3][0]], inc=(kc == 7))
                qps = PS[3][:, 0:512]
                P.op(ACT, lambda: nc.scalar.activation(out=qsq[:], in_=qps, func=AF.Square), reads=[PSB[3][0]], writes=[Bq])
                P.op(DVE, lambda: nc.vector.tensor_reduce(out=qss[:], in_=qsq[:].rearrange("p (h d) -> p h d", h=8), axis=AX.X, op=ALU.add),
                     reads=[Bq], writes=[Bq])
                rstd_from_ss(qss[:], 64, qsd[:], qrs[:], [Bq], [Bq])
                P.op(DVE, lambda: nc.vector.tensor_tensor(out=qn[:], in0=qps.rearrange("p (h d) -> p h d", h=8),
                                                          in1=qrs[:].unsqueeze(2).to_broadcast([128, 8, 64]), op=ALU.mult),
                     reads=[PSB[3][0], Bq], writes=[Bq])
                P.op(POOL, lambda: nc.gpsimd.tensor_tensor(out=qn[:], in0=qn[:], in1=gqB[:].unsqueeze(1).to_broadcast([128, 8, 64]), op=ALU.mult),
                     reads=[Bq, Bg], writes=[Bq])
                cosb = ropeq[:, 0:32].unsqueeze(1).to_broadcast([128, 8, 32])
                sinb = ropeq[:, 32:64].unsqueeze(1).to_broadcast([128, 8, 32])
                q1, q2 = qn[:, :, 0:32], qn[:, :, 32:64]
                for j, (a, b_) in enumerate(((q1, cosb), (q2, sinb), (q2, cosb), (q1, sinb))):
                    if j < 2:
                        P.op(DVE, lambda j=j, a=a, b_=b_: nc.vector.tensor_tensor(out=qtmp[:, j], in0=a, in1=b_, op=ALU.mult),
                             reads=[Bq, Bropeq], writes=[Bq])
                    else:
                        P.op(POOL, lambda j=j, a=a, b_=b_: nc.gpsimd.tensor_tensor(out=qtmp[:, j], in0=a, in1=b_, op=ALU.mult),
                             reads=[Bq, Bropeq], writes=[Bq])
                P.op(DVE, lambda: nc.vector.tensor_tensor(out=qr[:, :, 0:32], in0=qtmp[:, 0], in1=qtmp[:, 1], op=ALU.subtract),
                     reads=[Bq], writes=[Bqr])
                P.op(POOL, lambda: nc.gpsimd.tensor_tensor(out=qr[:, :, 32:64], in0=qtmp[:, 2], in1=qtmp[:, 3], op=ALU.add),
                     reads=[Bq], writes=[Bqr])
                pv = psb16(3, 1)
                for pr in range(4):
                    P.op(PE, lambda pr=pr: nc.tensor.transpose(out=pv[:, pr * 128:(pr + 1) * 128],
                                                              in_=qr[:, 2 * pr:2 * pr + 2, :].rearrange("p h d -> p (h d)"), identity=ident_b[:]),
                         reads=[Bqr, B_consts], writes=[PSB[3][1]], inc=(pr == 3))
                P.op(DVE, lambda t=t: nc.vector.tensor_copy(out=qT[:, :, t * 128:(t + 1) * 128], in_=pv[:, 0:512].rearrange("p (a t) -> p a t", a=4)),
                     reads=[PSB[3][1]], writes=[BqT])
            first, last = (blk == 0), (blk == 7)
            for c in range(4):
                def proj(colbase, ps_ap_main, ps_ap_halo, Bps, halo):
                    for kc in range(8):
                        P.op(PE, lambda kc=kc: nc.tensor.matmul(ps_ap_main, lhsT=wqs[:, kc, colbase:colbase + 128], rhs=hb[:, kc, 1:513],
                                                                start=(kc == 0), stop=(kc == 7)),
                             reads=[Bhb, Bw], writes=[Bps], inc=(kc == 7 and not halo))
                    if halo:
                        for kc in range(8):
                            P.op(PE, lambda kc=kc: nc.tensor.matmul(ps_ap_halo, lhsT=wqs[:, kc, colbase:colbase + 128], rhs=hb[:, kc, 0:514:513],
                                                                    start=(kc == 0), stop=(kc == 7)),
                                 reads=[Bhb, Bw], writes=[halo], inc=(kc == 7))
                proj(512 + 512 + c * 128, PS[3][:, 0:512], PS[1][:, 0:2], PSB[3][0], PSB[1][0])
                P.op(ACT, lambda: nc.scalar.activation(out=Zc[:, 1:513], in_=PS[3][:, 0:512], func=AF.Copy), reads=[PSB[3][0]], writes=[Bz])
                P.op(ACT, lambda: nc.scalar.activation(out=Zc[:, 0:514:513], in_=PS[1][:, 0:2], func=AF.Copy), reads=[PSB[1][0]], writes=[Bz])
                proj(512 + 1024 + c * 128, PS[3][:, 512:1024], PS[1][:, 512:514], PSB[3][1], PSB[1][1])
                P.op(DVE, lambda: nc.vector.tensor_tensor(out=Z[:, 1:513], in0=PS[3][:, 512:1024], in1=Zc[:, 1:513], op=ALU.mult),
                     reads=[PSB[3][1], Bz], writes=[Bz])
                P.op(DVE, lambda: nc.vector.tensor_tensor(out=Z[:, 0:514:513], in0=PS[1][:, 512:514], in1=Zc[:, 0:514:513], op=ALU.mult),
                     reads=[PSB[1][1], Bz], writes=[Bz])
                if first:
                    P.op(DVE, lambda: nc.vector.tensor_scalar(out=Z[:, 0:1], in0=Z[:, 0:1], scalar1=hmask[:, 0:1], scalar2=None, op0=ALU.mult),
                         reads=[Bz, B_consts], writes=[Bz])
                if last:
                    P.op(DVE, lambda: nc.vector.tensor_scalar(out=Z[:, 513:514], in0=Z[:, 513:514], scalar1=hmask[:, 1:2], scalar2=None, op0=ALU.mult),
                         reads=[Bz, B_consts], writes=[Bz])
                proj(512 + c * 128, PS[3][:, 0:512], None, PSB[3][0], None)
                P.op(DVE, lambda c=c: nc.vector.tensor_scalar(out=cv[:], in0=Z[:, 0:512], scalar1=wconv[:, 3 * c:3 * c + 1], scalar2=None, op0=ALU.mult),
                     reads=[Bz, Bg], writes=[Bz])
                P.op(DVE, lambda c=c: nc.vector.scalar_tensor_tensor(out=cv[:], in0=Z[:, 1:513], scalar=wconv[:, 3 * c + 1:3 * c + 2], in1=cv[:],
                                                                      op0=ALU.mult, op1=ALU.add), reads=[Bz, Bg], writes=[Bz])
                P.op(DVE, lambda c=c: nc.vector.scalar_tensor_tensor(out=cv[:], in0=Z[:, 2:514], scalar=wconv[:, 3 * c + 2:3 * c + 3], in1=cv[:],
                                                                      op0=ALU.mult, op1=ALU.add), reads=[Bz, Bg], writes=[Bz])
                P.op(DVE, lambda c=c: nc.vector.tensor_tensor(out=convT[:, c, :], in0=PS[3][:, 0:512], in1=cv[:], op=ALU.mult),
                     reads=[PSB[3][0], Bz], writes=[Bconv])
            Sbuf = [(PS[0], PSB[0]), (PS[1], PSB[1]), (PS[3], PSB[3])]
            seq = [(hp_, kt_) for hp_ in range(4) for kt_ in range(NKT)]

            def s_mm(i):
                hp, kt = seq[i]
                g = hp // 2
                ps_, pb_ = Sbuf[i % 3]
                P.op(PE, lambda: nc.tensor.matmul(ps_[:, 0:512], lhsT=KT[0:64, g, kt * 128:(kt + 1) * 128], rhs=qT[0:64, hp, :], start=True, stop=True),
                     reads=[B_KV, BqT], writes=[pb_[0]], inc=False)
                P.op(PE, lambda: nc.tensor.matmul(ps_[:, 512:1024], lhsT=KT[64:128, g, kt * 128:(kt + 1) * 128], rhs=qT[64:128, hp, :], start=True, stop=True),
                     reads=[B_KV, BqT], writes=[pb_[1]], inc=True)
            s_mm(0)
            s_mm(1)
            for i, (hp, kt) in enumerate(seq):
                g = hp // 2
                if i + 2 < len(seq):
                    s_mm(i + 2)
                ps_, pb_ = Sbuf[i % 3]
                pt, Bpt = PT[pti % 3], BPT[pti % 3]
                pti += 1
                P.op(ACT, lambda ps_=ps_, pt=pt: nc.scalar.activation(out=pt[:], in_=ps_[:], func=AF.Exp, scale=0.125),
                     reads=[pb_[0], pb_[1]], writes=[Bpt])
                for hh in range(2):
                    P.op(PE, lambda hh=hh, pt=pt: nc.tensor.matmul(PS[2][0:65, hh * 512:(hh + 1) * 512], lhsT=VA[:, kt, g, 0:65],
                                                                  rhs=pt[:, hh * 512:(hh + 1) * 512], start=(kt == 0), stop=(kt == NKT - 1)),
                         reads=[B_KV, Bpt], writes=[PSB[2][hh]], inc=(kt == NKT - 1 or hh == 1))
                if kt == NKT - 1:
                    for hh in range(2):
                        o = oT[hh]
                        P.op(DVE, lambda hh=hh, o=o: nc.vector.tensor_copy(out=o[0:64, :], in_=PS[2][0:64, hh * 512:(hh + 1) * 512]),
                             reads=[PSB[2][hh]], writes=[BoT[hh]])
                        P.op(DVE, lambda hh=hh, o=o: nc.vector.reciprocal(out=o[64:65, :], in_=PS[2][64:65, hh * 512:(hh + 1) * 512]),
                             reads=[PSB[2][hh]], writes=[BoT[hh]])
                        slot = rci % 4
                        rci += 1
                        P.dma(SP, rcd[slot:slot + 1, :], o[64:65, :], reads=[BoT[hh]], writes=[Brcd[slot]])
                        P.dma(SP, bcb[hh][:, :], rcd[slot:slot + 1, :].partition_broadcast(64), reads=[Brcd[slot]], writes=[Bbcb[hh]])
                    for hh in range(2):
                        h = 2 * hp + hh
                        P.op(DVE, lambda h=h, hh=hh: nc.vector.tensor_tensor(out=attnT[:, h, :], in0=oT[hh][0:64, :], in1=bcb[hh][:, :], op=ALU.mult),
                             reads=[BoT[hh], Bbcb[hh]], writes=[Battn])
            for t in range(4):
                wk = ewk[t % 2]
                for half in range(2):
                    n0 = half * 512
                    for h in range(8):
                        P.op(PE, lambda h=h, n0=n0, t=t: nc.tensor.matmul(PS[3][:, n0:n0 + 512], lhsT=attnT[:, h, t * 128:(t + 1) * 128],
                                                                           rhs=woA[:, h, n0:n0 + 512], start=(h == 0), stop=False),
                             reads=[Battn, Bw], writes=[PSB[3][half]], inc=False)
                    for c in range(4):
                        P.op(PE, lambda c=c, n0=n0, t=t: nc.tensor.matmul(PS[3][:, n0:n0 + 512], lhsT=convT[:, c, t * 128:(t + 1) * 128],
                                                                           rhs=woC[:, c, n0:n0 + 512], start=False, stop=(c == 3)),
                             reads=[Bconv, Bw], writes=[PSB[3][half]], inc=(c == 3))
                r0 = blk * 512 + t * 128
                epilogue(PS[3][:], [PSB[3][0], PSB[3][1]], xown[r0:r0 + 128, :], Gt["e_mix"], xA[r0:r0 + 128, :], wk)
        P.barrier()
        st.close()

    def phase_ffn(L, x_in, x_out, n_exp, ff, gch, wg_d, wu_d, wd_d, fp32_router):
        st = ExitStack()

        def sbt(name, shape, dt=F32):
            return st.enter_context(nc.sbuf_tensor(L + name, list(shape), dt))
        A, S = AS[L + "A_ffn"], AS[L + "S_ffn"]
        G = Gt[L + "_ffn"]
        TB = 2048
        NTB = TB // 128
        gw = gch * 128
        ngrp = ff // gw
        nhb = 1
        hT_l = [sbt(f"f_hT{i}", [128, 8, TB], BF16) for i in range(nhb)]
        BhT_l = [[Buf("f_hT") for _ in range(NTB)] for _ in range(nhb)]
        hT, BhT = hT_l[0], BhT_l[0]
        acc = sbt("f_acc", [128, NTB, D])
        Bacc = [Buf("f_acc") for _ in range(NTB)]
        wgb = [sbt(f"f_wg{i}", [128, 8, gw], BF16) for i in range(2)]
        wub = [sbt(f"f_wu{i}", [128, 8, gw], BF16) for i in range(2)]
        wdb = [sbt(f"f_wd{i}", [128, gch, D], BF16) for i in range(2)]
        Bwt = [Buf("f_w") for _ in range(2)]
        act = [sbt(f"f_act{i}", [128, gch, 512], BF16) for i in range(2)]
        Bact = [Buf("f_act") for _ in range(2)]
        sil = [sbt(f"f_sil{i}", [128, 512]) for i in range(2)]
        Bsil = [Buf("f_sil") for _ in range(2)]
        wks = mk_wk(sbt, "f", 2, with_x=True, with_xn=not fp32_router)
        ewks = wks if fp32_router else mk_wk(sbt, "fe", 2, with_x=True, with_xn=False)
        if fp32_router:
            gates = sbt("f_gates", [128, NTB, NEXP])
            Bgates = [Buf("gates") for _ in range(NTB)]
            wr = sbt("f_wr", [128, 8, NEXP])
            brB = sbt("f_brB", [128, NEXP])
            Bwr = Buf("wr")
            P.dma(SP, wr[:], W["o_w_router"].rearrange("(kc p) n -> p kc n", p=128), writes=[Bwr])
            P.dma(SP, brB[:], W["o_b_router"][:].partition_broadcast(128), writes=[Bwr])
            xn32 = [sbt(f"f_xn32{i}", [128, D]) for i in range(2)]
            h32 = sbt("f_h32", [128, 8, 128])
            Bx32 = [Buf("xn32") for _ in range(2)]
            Bh32 = Buf("h32")
            rt = {k: sbt("f_rt_" + k, [128, 8]) for k in ("lg", "m1", "l2", "m2", "g")}
            rs = {k: sbt("f_rs_" + k, [128, 1]) for k in ("mx1", "mx2", "d", "e", "den", "w1", "w2")}
            Brt = Buf("rt")
        wi = 0
        si = 0
        for tb in range(NOWN // TB):
            def f_pa(t, tbx=None):
                tbx = tb if tbx is None else tbx
                wk = wks[t % 2]
                r0 = tbx * TB + t * 128
                P.dma(SP, wk["x"][:], x_in[r0:r0 + 128, :], writes=[wk["Bx"]])
                if not fp32_router:
                    prenorm_a(wk["x"][:], wk["Bx"], wk)
                else:
                    xs = xn32[t % 2]
                    P.op(ACT, lambda wk=wk: nc.scalar.activation(out=wk["junk"][:], in_=wk["x"][:], func=AF.Square, accum_out=wk["ss"][:]),
                         reads=[wk["Bx"]], writes=[wk["Bs"]])
                    rstd_from_ss(wk["ss"][:], D, wk["sd"][:], wk["rstd"][:], [wk["Bs"]], [wk["Bs"]])
                    P.op(DVE, lambda wk=wk: nc.vector.tensor_scalar(out=xs[:], in0=wk["x"][:], scalar1=wk["rstd"][:], scalar2=None, op0=ALU.mult),
                         reads=[wk["Bx"], wk["Bs"]], writes=[Bx32[t % 2]])

            def f_pb(t, tbx=None):
                tbx = tb if tbx is None else tbx
                wk = wks[t % 2]
                if not fp32_router:
                    prenorm_b(A, S, 0, hT_l[tbx % nhb][:, :, t * 128:(t + 1) * 128], BhT_l[tbx % nhb][t], wk, (3, t % 2))
                    return
                hdst = hT[:, :, t * 128:(t + 1) * 128]
                xs = xn32[t % 2]
                pp = 3 if t % 2 == 0 else 1
                for c in range(8):
                    half = c // 4
                    P.op(PE, lambda c=c: nc.tensor.transpose(out=PS[pp][:, c * 128:(c + 1) * 128], in_=xs[:, c * 128:(c + 1) * 128], identity=ident_f[:]),
                         reads=[Bx32[t % 2], B_consts], writes=[PSB[pp][half]], inc=(c % 4 == 3))
                for c in range(8):
                    half = c // 4
                    P.op(DVE, lambda c=c: nc.vector.tensor_scalar(out=h32[:, c, :], in0=PS[pp][:, c * 128:(c + 1) * 128], scalar1=A[:, c, 0:1],
                                                                  scalar2=S[:, c, 0:1], op0=ALU.mult, op1=ALU.add),
                         reads=[PSB[pp][half], B_mod], writes=[Bh32])
                P.op(POOL, lambda hdst=hdst: nc.gpsimd.tensor_copy(out=hdst, in_=h32[:]), reads=[Bh32], writes=[BhT[t]])
                for kc in range(8):
                    P.op(PE, lambda kc=kc: nc.tensor.matmul(PS[2][:, 0:NEXP], lhsT=h32[:, kc, :], rhs=wr[:, kc, :], start=(kc == 0), stop=(kc == 7)),
                         reads=[Bh32, Bwr], writes=[PSB[2][0]], inc=(kc == 7))
                V = nc.vector
                P.op(DVE, lambda: V.tensor_tensor(out=rt["lg"][:], in0=PS[2][:, 0:NEXP], in1=brB[:], op=ALU.add), reads=[PSB[2][0], Bwr], writes=[Brt])
                P.op(DVE, lambda: V.tensor_reduce(out=rs["mx1"][:], in_=rt["lg"][:], axis=AX.X, op=ALU.max), reads=[Brt], writes=[Brt])
                P.op(DVE, lambda: V.tensor_scalar(out=rt["m1"][:], in0=rt["lg"][:], scalar1=rs["mx1"][:], scalar2=None, op0=ALU.is_equal), reads=[Brt], writes=[Brt])
                P.op(DVE, lambda: V.scalar_tensor_tensor(out=rt["l2"][:], in0=rt["m1"][:], scalar=-1e30, in1=rt["lg"][:], op0=ALU.mult, op1=ALU.add), reads=[Brt], writes=[Brt])
                P.op(DVE, lambda: V.tensor_reduce(out=rs["mx2"][:], in_=rt["l2"][:], axis=AX.X, op=ALU.max), reads=[Brt], writes=[Brt])
                P.op(DVE, lambda: V.tensor_scalar(out=rt["m2"][:], in0=rt["l2"][:], scalar1=rs["mx2"][:], scalar2=None, op0=ALU.is_equal), reads=[Brt], writes=[Brt])
                P.op(DVE, lambda: V.tensor_tensor(out=rs["d"][:], in0=rs["mx2"][:], in1=rs["mx1"][:], op=ALU.subtract), reads=[Brt], writes=[Brt])
                P.op(ACT, lambda: nc.scalar.activation(out=rs["e"][:], in_=rs["d"][:], func=AF.Exp), reads=[Brt], writes=[Brt])
                P.op(DVE, lambda: V.tensor_scalar(out=rs["den"][:], in0=rs["e"][:], scalar1=1.0, scalar2=None, op0=ALU.add), reads=[Brt], writes=[Brt])
                P.op(DVE, lambda: V.reciprocal(out=rs["w1"][:], in_=rs["den"][:]), reads=[Brt], writes=[Brt])
                P.op(DVE, lambda: V.tensor_tensor(out=rs["w2"][:], in0=rs["e"][:], in1=rs["w1"][:], op=ALU.mult), reads=[Brt], writes=[Brt])
                P.op(DVE, lambda: V.tensor_scalar(out=rt["g"][:], in0=rt["m1"][:], scalar1=rs["w1"][:], scalar2=None, op0=ALU.mult), reads=[Brt], writes=[Brt])
                P.op(DVE, lambda t=t: V.scalar_tensor_tensor(out=gates[:, t, :], in0=rt["m2"][:], scalar=rs["w2"][:], in1=rt["g"][:], op0=ALU.mult, op1=ALU.add),
                     reads=[Brt], writes=[Bgates[t]])
            if True:
                f_pa(0)
                for t in range(NTB):
                    if t + 1 < NTB:
                        f_pa(t + 1)
                    f_pb(t)
            hT, BhT = hT_l[tb % nhb], BhT_l[tb % nhb]
            if dbg and fp32_router and tb == 0:
                dg = dscr("dbg_gates", [128, NTB, NEXP], out=True)
                dh = dscr("dbg_hT", [128, 8, TB], BF16, out=True)
                P.dma(SP, dg[:], gates[:], reads=Bgates)
                P.dma(SP, dh[:], hT[:], reads=BhT)
            elist = cfg.get("exp_list", list(range(n_exp))) if n_exp > 1 else [0]
            items = [(e, grp, sbk) for e in elist for grp in range(ngrp) for sbk in range(TB // 512)]
            state = {}

            def load_w(e, grp):
                nonlocal wi
                wsl = wi % 2
                wi += 1
                f0 = grp * gw
                if n_exp == 1:
                    gsrc, usrc, dsrc = wg_d[:, f0:f0 + gw], wu_d[:, f0:f0 + gw], wd_d[f0:f0 + gw, :]
                else:
                    gsrc, usrc, dsrc = wg_d[e, :, f0:f0 + gw], wu_d[e, :, f0:f0 + gw], wd_d[e, f0:f0 + gw, :]
                P.dma(POOL, wgb[wsl][:], gsrc.rearrange("(kc p) n -> p kc n", p=128), writes=[Bwt[wsl]])
                P.dma(POOL, wub[wsl][:], usrc.rearrange("(kc p) n -> p kc n", p=128), writes=[Bwt[wsl]])
                P.dma(POOL, wdb[wsl][:], dsrc.rearrange("(j p) n -> p j n", p=128), writes=[Bwt[wsl]])
                state[(e, grp)] = wsl

            def gateup(idx):
                nonlocal si
                e, grp, sbk = items[idx]
                if (e, grp) not in state:
                    load_w(e, grp)
                wsl = state[(e, grp)]
                asl = idx % 2
                hTr = [BhT[sbk * 4 + i] for i in range(4)]
                for j in range(gch):
                    for (wbuf, half) in ((wgb[wsl], 0), (wub[wsl], 1)):
                        for kc in range(8):
                            P.op(PE, lambda kc=kc, j=j, wbuf=wbuf, half=half: nc.tensor.matmul(
                                PS[j % 2][:, half * 512:(half + 1) * 512],
                                lhsT=wbuf[:, kc, j * 128:(j + 1) * 128], rhs=hT[:, kc, sbk * 512:(sbk + 1) * 512],
                                start=(kc == 0), stop=(kc == 7)),
                                reads=hTr + [Bwt[wsl]], writes=[PSB[j % 2][half]], inc=(kc == 7))
                    psg = PS[j % 2]
                    ssl = si % 2
                    si += 1
                    P.op(ACT, lambda psg=psg, ssl=ssl: nc.scalar.activation(out=sil[ssl][:], in_=psg[:, 0:512], func=AF.Silu),
                         reads=[PSB[j % 2][0]], writes=[Bsil[ssl]])
                    P.op(DVE, lambda psg=psg, ssl=ssl, j=j, asl=asl: nc.vector.tensor_tensor(out=act[asl][:, j, :], in0=psg[:, 512:1024], in1=sil[ssl][:], op=ALU.mult),
                         reads=[PSB[j % 2][1], Bsil[ssl]], writes=[Bact[asl]])

            def down(idx):
                e, grp, sbk = items[idx]
                wsl = state[(e, grp)]
                asl = idx % 2
                firstacc = (e == elist[0] and grp == 0)
                for t4 in range(4):
                    t = sbk * 4 + t4
                    pso = 2 + (t4 % 2)
                    for half in range(2):
                        for j in range(gch):
                            P.op(PE, lambda j=j, half=half, t4=t4, pso=pso, asl=asl: nc.tensor.matmul(
                                PS[pso][:, half * 512:(half + 1) * 512], lhsT=act[asl][:, j, t4 * 128:(t4 + 1) * 128],
                                rhs=wdb[wsl][:, j, half * 512:(half + 1) * 512], start=(j == 0), stop=(j == gch - 1)),
                                reads=[Bact[asl], Bwt[wsl]], writes=[PSB[pso][half]], inc=(j == gch - 1))
                    rd = [PSB[pso][0], PSB[pso][1]]
                    if n_exp == 1:
                        if firstacc:
                            P.op(DVE, lambda t=t, pso=pso: nc.vector.tensor_copy(out=acc[:, t, :], in_=PS[pso][:]), reads=rd, writes=[Bacc[t]])
                        else:
                            P.op(DVE, lambda t=t, pso=pso: nc.vector.tensor_tensor(out=acc[:, t, :], in0=PS[pso][:], in1=acc[:, t, :], op=ALU.add),
                                 reads=rd, writes=[Bacc[t]])
                    else:
                        if firstacc:
                            P.op(DVE, lambda t=t, pso=pso, e=e: nc.vector.tensor_scalar(out=acc[:, t, :], in0=PS[pso][:], scalar1=gates[:, t, e:e + 1],
                                                                                     scalar2=None, op0=ALU.mult), reads=rd + [Bgates[t]], writes=[Bacc[t]])
                        else:
                            P.op(DVE, lambda t=t, pso=pso, e=e: nc.vector.scalar_tensor_tensor(out=acc[:, t, :], in0=PS[pso][:], scalar=gates[:, t, e:e + 1],
                                                                                            in1=acc[:, t, :], op0=ALU.mult, op1=ALU.add),
                                 reads=rd + [Bgates[t]], writes=[Bacc[t]])
            gateup(0)
            nxt_t = 0
            overlap_next = False
            for idx in range(len(items)):
                if idx + 1 < len(items):
                    gateup(idx + 1)
                down(idx)
                if overlap_next and idx >= 2 and idx % 2 == 0 and nxt_t < NTB:
                    f_pa(nxt_t, tb + 1)
                    f_pb(nxt_t, tb + 1)
                    nxt_t += 1
            if overlap_next:
                while nxt_t < NTB:
                    f_pa(nxt_t, tb + 1)
                    f_pb(nxt_t, tb + 1)
                    nxt_t += 1
            for t in range(NTB):
                r0 = tb * TB + t * 128
                epilogue(acc[:, t, :], [Bacc[t]], x_in[r0:r0 + 128, :], G, x_out[r0:r0 + 128, :], ewks[t % 2])
        P.barrier()
        st.close()

    def phase_moe(x_in, x_out):
        I32 = mybir.dt.int32
        A, S = AS["oA_ffn"], AS["oS_ffn"]
        G = Gt["o_ffn"]
        CAP = 4608
        NSUB = 9
        hsorted = dscr("hsorted", [NEXP * CAP, D], BF16)
        ysorted = dscr("ysorted", [NEXP * CAP, D], F32)
        flags_d = dscr("flags_d", [1, NEXP * 16], I32, out=dbg)
        st0 = ExitStack()

        def sb0(name, shape, dt=F32):
            return st0.enter_context(nc.sbuf_tensor("m_" + name, list(shape), dt))
        dest_i = sb0("dest_i", [128, NT_OWN, 2], I32)
        wts = sb0("wts", [128, NT_OWN, 2])
        Bdest = [Buf("dest") for _ in range(NT_OWN)]
        st = ExitStack()

        def sbt(name, shape, dt=F32):
            return st.enter_context(nc.sbuf_tensor("mr_" + name, list(shape), dt))
        wks = mk_wk(sbt, "mr", 2, with_x=True, with_xn=False)
        wr = sbt("wr", [128, 8, NEXP])
        brB = sbt("brB", [128, NEXP])
        Bwr = Buf("wr")
        P.dma(SP, wr[:], W["o_w_router"].rearrange("(kc p) n -> p kc n", p=128), writes=[Bwr])
        P.dma(SP, brB[:], W["o_b_router"][:].partition_broadcast(128), writes=[Bwr])
        xn32 = [sbt(f"xn32{i}", [128, D]) for i in range(2)]
        xnb = [sbt(f"xnb{i}", [128, D], BF16) for i in range(2)]
        Bx32 = [Buf("xn32") for _ in range(2)]
        Bxnb = [Buf("xnb") for _ in range(2)]
        h32 = sbt("h32", [128, 8, 128])
        Bh32 = Buf("h32")
        rt = {k: sbt("rt_" + k, [128, 8]) for k in ("lg", "m1", "l2", "m2", "pos", "t1", "t2", "macc", "ebase")}
        rs = {k: sbt("rs_" + k, [128, 1]) for k in ("mx1", "mx2", "d", "e", "den", "d1", "d2")}
        maskb = sbt("maskb", [128, 8], BF16)
        maccb = sbt("maccb", [128, 8], BF16)
        ustrict = sbt("ustrict", [128, 128], BF16)
        ones_b = sbt("ones_b", [128, 128], BF16)
        fl = sbt("fl", [128, NSUB, NEXP])
        fl_i = sbt("fl_i", [128, NEXP, 16], I32)
        nsub_f = sbt("nsub_f", [128, NEXP])
        Brt = Buf("rt")
        Bmacc = Buf("macc")
        Bc2 = Buf("c2")
        V = nc.vector
        ustr_f = sbt("ustr_f", [128, 128])
        P.op(DVE, lambda: V.memset(ones_b[:], 1.0), writes=[Bc2])
        P.op(DVE, lambda: V.memset(rt["macc"][:], 0.0), writes=[Bmacc])
        P.op(DVE, lambda: V.memset(maccb[:], 0.0), writes=[Bmacc])
        for e in range(NEXP):
            P.op(DVE, lambda e=e: V.memset(rt["ebase"][:, e:e + 1], float(e * CAP)), writes=[Bc2])
        P.op(DVE, lambda: V.tensor_tensor_scan(out=ustr_f[:], data0=ones_f_full[:], data1=ident_f[:], initial=0.0, op0=ALU.mult, op1=ALU.add),
             reads=[B_consts], writes=[Bc2])
        P.op(DVE, lambda: V.tensor_tensor(out=ustrict[:], in0=ustr_f[:], in1=ident_f[:], op=ALU.subtract), reads=[Bc2, B_consts], writes=[Bc2])

        def r_pa(t):
            wk = wks[t % 2]
            r0 = t * 128
            P.dma(SP, wk["x"][:], x_in[r0:r0 + 128, :], writes=[wk["Bx"]])
            xs = xn32[t % 2]
            P.op(ACT, lambda: nc.scalar.activation(out=wk["junk"][:], in_=wk["x"][:], func=AF.Square, accum_out=wk["ss"][:]),
                 reads=[wk["Bx"]], writes=[wk["Bs"]])
            rstd_from_ss(wk["ss"][:], D, wk["sd"][:], wk["rstd"][:], [wk["Bs"]], [wk["Bs"]])
            P.op(DVE, lambda: V.tensor_scalar(out=xs[:], in0=wk["x"][:], scalar1=wk["rstd"][:], scalar2=None, op0=ALU.mult),
                 reads=[wk["Bx"], wk["Bs"]], writes=[Bx32[t % 2]])
            P.op(ACT, lambda: nc.scalar.activation(out=xnb[t % 2][:], in_=xs[:], func=AF.Copy), reads=[Bx32[t % 2]], writes=[Bxnb[t % 2]])

        def r_pb(t):
            xs = xn32[t % 2]
            pp = 3 if t % 2 == 0 else 1
            for c in range(8):
                half = c // 4
                P.op(PE, lambda c=c: nc.tensor.transpose(out=PS[pp][:, c * 128:(c + 1) * 128], in_=xs[:, c * 128:(c + 1) * 128], identity=ident_f[:]),
                     reads=[Bx32[t % 2], B_consts], writes=[PSB[pp][half]], inc=(c % 4 == 3))
            for c in range(8):
                half = c // 4
                P.op(DVE, lambda c=c: V.tensor_scalar(out=h32[:, c, :], in0=PS[pp][:, c * 128:(c + 1) * 128], scalar1=A[:, c, 0:1],
                                                      scalar2=S[:, c, 0:1], op0=ALU.mult, op1=ALU.add),
                     reads=[PSB[pp][half], B_mod], writes=[Bh32])
            for kc in range(8):
                P.op(PE, lambda kc=kc: nc.tensor.matmul(PS[2][:, 0:NEXP], lhsT=h32[:, kc, :], rhs=wr[:, kc, :], start=(kc == 0), stop=(kc == 7)),
                     reads=[Bh32, Bwr], writes=[PSB[2][0]], inc=(kc == 7))
            P.op(DVE, lambda: V.tensor_tensor(out=rt["lg"][:], in0=PS[2][:, 0:NEXP], in1=brB[:], op=ALU.add), reads=[PSB[2][0], Bwr], writes=[Brt])
            P.op(DVE, lambda: V.tensor_reduce(out=rs["mx1"][:], in_=rt["lg"][:], axis=AX.X, op=ALU.max), reads=[Brt], writes=[Brt])
            P.op(DVE, lambda: V.tensor_scalar(out=rt["m1"][:], in0=rt["lg"][:], scalar1=rs["mx1"][:], scalar2=None, op0=ALU.is_equal), reads=[Brt], writes=[Brt])
            P.op(DVE, lambda: V.scalar_tensor_tensor(out=rt["l2"][:], in0=rt["m1"][:], scalar=-1e30, in1=rt["lg"][:], op0=ALU.mult, op1=ALU.add), reads=[Brt], writes=[Brt])
            P.op(DVE, lambda: V.tensor_reduce(out=rs["mx2"][:], in_=rt["l2"][:], axis=AX.X, op=ALU.max), reads=[Brt], writes=[Brt])
            P.op(DVE, lambda: V.tensor_scalar(out=rt["m2"][:], in0=rt["l2"][:], scalar1=rs["mx2"][:], scalar2=None, op0=ALU.is_equal), reads=[Brt], writes=[Brt])
            P.op(DVE, lambda: V.tensor_tensor(out=rs["d"][:], in0=rs["mx2"][:], in1=rs["mx1"][:], op=ALU.subtract), reads=[Brt], writes=[Brt])
            P.op(ACT, lambda: nc.scalar.activation(out=rs["e"][:], in_=rs["d"][:], func=AF.Exp), reads=[Brt], writes=[Brt])
            P.op(DVE, lambda: V.tensor_scalar(out=rs["den"][:], in0=rs["e"][:], scalar1=1.0, scalar2=None, op0=ALU.add), reads=[Brt], writes=[Brt])
            P.op(DVE, lambda: V.reciprocal(out=wts[:, t, 0:1], in_=rs["den"][:]), reads=[Brt], writes=[Bdest[t]])
            P.op(DVE, lambda: V.tensor_tensor(out=wts[:, t, 1:2], in0=rs["e"][:], in1=wts[:, t, 0:1], op=ALU.mult), reads=[Brt, Bdest[t]], writes=[Bdest[t]])
            P.op(DVE, lambda: V.tensor_tensor(out=maskb[:], in0=rt["m1"][:], in1=rt["m2"][:], op=ALU.add), reads=[Brt], writes=[Brt])
            P.op(PE, lambda: nc.tensor.matmul(PS[2][:, 512:512 + NEXP], lhsT=ustrict[:], rhs=maskb[:], start=True, stop=False),
                 reads=[Brt, Bc2], writes=[PSB[2][1]], inc=False)
            P.op(PE, lambda: nc.tensor.matmul(PS[2][:, 512:512 + NEXP], lhsT=ones_b[:], rhs=maccb[:], start=False, stop=True),
                 reads=[Bmacc, Bc2], writes=[PSB[2][1]], inc=True)
            P.op(DVE, lambda: V.tensor_tensor(out=rt["pos"][:], in0=PS[2][:, 512:512 + NEXP], in1=rt["ebase"][:], op=ALU.add), reads=[PSB[2][1], Bc2], writes=[Brt])
            P.op(DVE, lambda: V.tensor_tensor(out=rt["t1"][:], in0=rt["pos"][:], in1=rt["m1"][:], op=ALU.mult), reads=[Brt], writes=[Brt])
            P.op(DVE, lambda: V.tensor_reduce(out=rs["d1"][:], in_=rt["t1"][:], axis=AX.X, op=ALU.add), reads=[Brt], writes=[Brt])
            P.op(DVE, lambda: V.tensor_tensor(out=rt["t2"][:], in0=rt["pos"][:], in1=rt["m2"][:], op=ALU.mult), reads=[Brt], writes=[Brt])
            P.op(DVE, lambda: V.tensor_reduce(out=rs["d2"][:], in_=rt["t2"][:], axis=AX.X, op=ALU.add), reads=[Brt], writes=[Brt])
            P.op(DVE, lambda: V.tensor_copy(out=dest_i[:, t, 0:1], in_=rs["d1"][:]), reads=[Brt], writes=[Bdest[t]])
            P.op(DVE, lambda: V.tensor_copy(out=dest_i[:, t, 1:2], in_=rs["d2"][:]), reads=[Brt], writes=[Bdest[t]])
            P.op(DVE, lambda: V.tensor_tensor(out=rt["macc"][:], in0=rt["macc"][:], in1=maskb[:], op=ALU.add), reads=[Brt, Bmacc], writes=[Bmacc])
            P.op(DVE, lambda: V.tensor_copy(out=maccb[:], in_=rt["macc"][:]), reads=[Bmacc], writes=[Bmacc])
            for k in range(2):
                P.dma_ind(hsorted[:, :], bass.IndirectOffsetOnAxis(ap=dest_i[:, t, k:k + 1], axis=0), xnb[t % 2][:, :], None,
                          reads=[Bxnb[t % 2], Bdest[t]])
        r_pa(0)
        for t in range(NT_OWN):
            if t + 1 < NT_OWN:
                r_pa(t + 1)
            r_pb(t)
        P.op(PE, lambda: nc.tensor.matmul(PS[2][:, 0:NEXP], lhsT=ones_b[:], rhs=maccb[:], start=True, stop=True), reads=[Bmacc, Bc2], writes=[PSB[2][0]], inc=True)
        Bfl = Buf("fl")
        for j in range(NSUB):
            P.op(DVE, lambda j=j: V.tensor_scalar(out=fl[:, j, :], in0=PS[2][:, 0:NEXP], scalar1=float(j * 512) + 0.5, scalar2=None, op0=ALU.is_gt),
                 reads=[PSB[2][0]], writes=[Bfl])
        P.op(DVE, lambda: V.memset(fl_i[:], 0), writes=[Bfl])
        P.op(DVE, lambda: V.tensor_reduce(out=nsub_f[:], in_=fl[:].rearrange("p j e -> p e j"), axis=AX.X, op=ALU.add), reads=[Bfl], writes=[Bfl])
        P.op(DVE, lambda: V.tensor_copy(out=fl_i[:, :, 0:1], in_=nsub_f[:].unsqueeze(2)), reads=[Bfl], writes=[Bfl])
        tok_flags = P.dma(SP, flags_d[:, :], fl_i[0:1, :, :].rearrange("p a b -> p (a b)"), reads=[Bfl])
        if dbg:
            ddst = dscr("dbg_dest", [128, NT_OWN, 2], I32, out=True)
            P.dma(SP, ddst[:], dest_i[:], reads=Bdest)
            dw = dscr("dbg_wts", [128, NT_OWN, 2], out=True)
            P.dma(SP, dw[:], wts[:], reads=Bdest)
        P.barrier()
        st.close()

        st = ExitStack()

        def sbt(name, shape, dt=F32):
            return st.enter_context(nc.sbuf_tensor("me_" + name, list(shape), dt))
        gch, gw, ngrp = 4, 512, E_FF // 512
        wgb = [sbt(f"wg{i}", [128, 8, gw], BF16) for i in range(2)]
        wub = [sbt(f"wu{i}", [128, 8, gw], BF16) for i in range(2)]
        wdb = [sbt(f"wd{i}", [128, gch, D], BF16) for i in range(2)]
        Bwt = [Buf("w") for _ in range(2)]
        act = [sbt(f"act{i}", [128, gch, 512], BF16) for i in range(2)]
        Bact = [[Buf("act") for _ in range(4)] for _ in range(2)]
        sil = [sbt(f"sil{i}", [128, 512]) for i in range(2)]
        Bsil = [Buf("sil") for _ in range(2)]
        hTc = sbt("hTc", [128, 8, 1536], BF16)
        BhTc = [Buf("hTc") for _ in range(12)]
        acc = sbt("acc", [128, 12, D])
        Bacc = [Buf("acc") for _ in range(12)]
        xr = [sbt(f"xr{i}", [128, D], BF16) for i in range(2)]
        Bxr = [Buf("xr") for _ in range(2)]
        all_eng = [mybir.EngineType.PE, mybir.EngineType.Activation, mybir.EngineType.DVE, mybir.EngineType.Pool, mybir.EngineType.SP]
        Rn = nc.alloc_registers("nsub", all_eng)
        for E in (PE, ACT, DVE, POOL, SP):
            E.wait(tok_flags)
        wi = 0
        si = 0
        xi = 0
        for e in range(NEXP):
            for reg in Rn:
                nc.reg_load(reg, flags_d[0:1, e * 16:e * 16 + 1])
            for c in range(3):
                for s_ in range(3):
                    j = 3 * c + s_

                    def prep(j=j, s_=s_):
                        nonlocal xi
                        for t4 in range(4):
                            k = xi % 2
                            xi += 1
                            row0 = e * CAP + j * 512 + t4 * 128
                            P.dma(SP, xr[k][:], hsorted[row0:row0 + 128, :], writes=[Bxr[k]])
                            ti = s_ * 4 + t4
                            wkx = {"xn": xr[k], "Bxn": Bxr[k]}
                            prenorm_b(A, S, 0, hTc[:, :, ti * 128:(ti + 1) * 128], BhTc[ti], wkx, (3, t4 % 2))
                    P.predicated(Rn, j, prep)
                for grp in range(ngrp):
                    wsl = wi % 2
                    wi += 1
                    f0 = grp * gw

                    def loadw(wsl=wsl, f0=f0):
                        P.dma(POOL, wgb[wsl][:], W["o_w_gate"][e, :, f0:f0 + gw].rearrange("(kc p) n -> p kc n", p=128), writes=[Bwt[wsl]])
                        P.dma(POOL, wub[wsl][:], W["o_w_up"][e, :, f0:f0 + gw].rearrange("(kc p) n -> p kc n", p=128), writes=[Bwt[wsl]])
                        P.dma(POOL, wdb[wsl][:], W["o_w_down"][e, f0:f0 + gw, :].rearrange("(j p) n -> p j n", p=128), writes=[Bwt[wsl]])
                    P.predicated(Rn, 3 * c, loadw)
                    for s_ in range(3):
                        j = 3 * c + s_

                        def body(s_=s_, wsl=wsl, grp=grp):
                            nonlocal si
                            asl = si % 2
                            hTr = [BhTc[s_ * 4 + i] for i in range(4)]
                            for jj in range(gch):
                                for (wbuf, half) in ((wgb[wsl], 0), (wub[wsl], 1)):
                                    for kc in range(8):
                                        P.op(PE, lambda kc=kc, jj=jj, wbuf=wbuf, half=half: nc.tensor.matmul(
                                            PS[jj % 2][:, half * 512:(half + 1) * 512],
                                            lhsT=wbuf[:, kc, jj * 128:(jj + 1) * 128], rhs=hTc[:, kc, s_ * 512:(s_ + 1) * 512],
                                            start=(kc == 0), stop=(kc == 7)),
                                            reads=hTr + [Bwt[wsl]], writes=[PSB[jj % 2][half]], inc=(kc == 7))
                                psg = PS[jj % 2]
                                ssl = si % 2
                                si += 1
                                P.op(ACT, lambda psg=psg, ssl=ssl: nc.scalar.activation(out=sil[ssl][:], in_=psg[:, 0:512], func=AF.Silu),
                                     reads=[PSB[jj % 2][0]], writes=[Bsil[ssl]])
                                P.op(DVE, lambda psg=psg, ssl=ssl, jj=jj: nc.vector.tensor_tensor(out=act[asl][:, jj, :], in0=psg[:, 512:1024], in1=sil[ssl][:], op=ALU.mult),
                                     reads=[PSB[jj % 2][1], Bsil[ssl]], writes=[Bact[asl][jj]])
                            for tp in range(2):
                                for jj in range(gch):
                                    for t4 in (2 * tp, 2 * tp + 1):
                                        pso = 2 + (t4 % 2)
                                        for half in range(2):
                                            P.op(PE, lambda jj=jj, half=half, t4=t4, pso=pso: nc.tensor.matmul(
                                                PS[pso][:, half * 512:(half + 1) * 512], lhsT=act[asl][:, jj, t4 * 128:(t4 + 1) * 128],
                                                rhs=wdb[wsl][:, jj, half * 512:(half + 1) * 512], start=(jj == 0), stop=(jj == gch - 1)),
                                                reads=[Bact[asl][jj], Bwt[wsl]], writes=[PSB[pso][half]], inc=(jj == gch - 1))
                                for t4 in (2 * tp, 2 * tp + 1):
                                    ti = s_ * 4 + t4
                                    pso = 2 + (t4 % 2)
                                    rd = [PSB[pso][0], PSB[pso][1]]
                                    if grp == 0:
                                        P.op(DVE, lambda ti=ti, pso=pso: nc.vector.tensor_copy(out=acc[:, ti, :], in_=PS[pso][:]), reads=rd, writes=[Bacc[ti]])
                                    else:
                                        P.op(DVE, lambda ti=ti, pso=pso: nc.vector.tensor_tensor(out=acc[:, ti, :], in0=PS[pso][:], in1=acc[:, ti, :], op=ALU.add),
                                             reads=rd, writes=[Bacc[ti]])
                                    if grp == ngrp - 1:
                                        row0 = e * CAP + (3 * c + s_) * 512 + t4 * 128
                                        P.dma(SP, ysorted[row0:row0 + 128, :], acc[:, ti, :], reads=[Bacc[ti]])
                        P.predicated(Rn, j, body)
        P.barrier()
        st.close()

        st = ExitStack()

        def sbt(name, shape, dt=F32):
            return st.enter_context(nc.sbuf_tensor("mc_" + name, list(shape), dt))
        wks = mk_wk(sbt, "mc", 2, with_x=True, with_xn=False)
        NY = 4
        y1 = [sbt(f"y1{i}", [128, D]) for i in range(NY)]
        y2 = [sbt(f"y2{i}", [128, D]) for i in range(NY)]
        By = [Buf("y") for _ in range(NY)]

        def gather(t):
            k = t % NY
            P.dma_ind(y1[k][:, :], None, ysorted[:, :], bass.IndirectOffsetOnAxis(ap=dest_i[:, t, 0:1], axis=0), reads=[Bdest[t]], writes=[By[k]])
            P.dma_ind(y2[k][:, :], None, ysorted[:, :], bass.IndirectOffsetOnAxis(ap=dest_i[:, t, 1:2], axis=0), reads=[Bdest[t]], writes=[By[k]])
        gather(0)
        gather(1)
        for t in range(NT_OWN):
            k = t % NY
            if t + 2 < NT_OWN:
                gather(t + 2)
            P.op(DVE, lambda: nc.vector.tensor_scalar(out=y1[k][:], in0=y1[k][:], scalar1=wts[:, t, 0:1], scalar2=None, op0=ALU.mult), reads=[By[k], Bdest[t]], writes=[By[k]])
            P.op(DVE, lambda: nc.vector.scalar_tensor_tensor(out=y1[k][:], in0=y2[k][:], scalar=wts[:, t, 1:2], in1=y1[k][:], op0=ALU.mult, op1=ALU.add),
                 reads=[By[k], Bdest[t]], writes=[By[k]])
            r0 = t * 128
            epilogue(y1[k][:], [By[k]], x_in[r0:r0 + 128, :], G, x_out[r0:r0 + 128, :], wks[t % 2])
        P.barrier()
        st.close()
        st0.close()

    def phaseC():
        st = ExitStack()

        def sbt(name, shape, dt=F32):
            return st.enter_context(nc.sbuf_tensor(name, list(shape), dt))
        A, S = AS["oA_mix"], AS["oS_mix"]
        G = Gt["o_mix"]
        wi_ = sbt("c_win", [128, 8, 2048], BF16)
        wo_ = sbt("c_wout", [128, 8, D], BF16)
        Bw = Buf("c_w")
        P.dma(POOL, wi_[:, :, 0:1024], W["o_w_in"][:, 0:1024].rearrange("(kc p) n -> p kc n", p=128), writes=[Bw])
        P.dma(POOL, wi_[:, :, 1024:2048], W["o_w_in"][:, 1024:2048].rearrange("(kc p) n -> p kc n", p=128), writes=[Bw])
        P.dma(POOL, wo_[:], W["o_w_out"].rearrange("(kc p) n -> p kc n", p=128), writes=[Bw])
        ws_b = sbt("c_wsb", [128, 8, 128], BF16)
        WsT = sbt("c_WsT", [128, 8, 128], BF16)
        ones_b = sbt("c_ones", [128, 128], BF16)
        Rg = sbt("c_Rg", [128, 8, 128])
        bsB = sbt("c_bsB", [128, 8, 128])
        gvT = sbt("c_gvT", [128, 8])
        bvT = sbt("c_bvT", [128, 8])
        Bs_ = Buf("c_s")
        P.dma(POOL, ws_b[:], W["o_w_s"].rearrange("g p q -> p g q"), writes=[Bs_])
        P.dma(SP, gvT[:], W["o_g_vT"][:], writes=[Bs_])
        P.dma(SP, bvT[:], W["o_b_vT"][:], writes=[Bs_])
        for g in range(8):
            P.dma(SP, bsB[:, g, :], W["o_b_s"][g:g + 1, :].partition_broadcast(128), writes=[Bs_])
        P.op(DVE, lambda: nc.vector.memset(ones_b[:], 1.0), writes=[Bs_])
        pv = psb16(3, 0)
        for g in range(8):
            P.op(PE, lambda g=g: nc.tensor.transpose(out=pv[:, g * 128:(g + 1) * 128], in_=ws_b[:, g, :], identity=ident_b[:]),
                 reads=[Bs_, B_consts], writes=[PSB[3][0]], inc=(g == 7))
        P.op(DVE, lambda: nc.vector.tensor_copy(out=WsT[:], in_=pv[:, 0:1024].rearrange("p (g q) -> p g q", g=8)), reads=[PSB[3][0]], writes=[Bs_])
        for g in range(8):
            P.op(PE, lambda g=g: nc.tensor.matmul(PS[3][:, 512 + (g % 4) * 128:512 + (g % 4 + 1) * 128], lhsT=ones_b[:], rhs=WsT[:, g, :], start=True, stop=True),
                 reads=[Bs_], writes=[PSB[3][1]], inc=True)
            P.op(DVE, lambda g=g: nc.vector.scalar_tensor_tensor(out=Rg[:, g, :], in0=PS[3][:, 512 + (g % 4) * 128:512 + (g % 4 + 1) * 128],
                                                                  scalar=bvT[:, g:g + 1], in1=bsB[:, g, :], op0=ALU.mult, op1=ALU.add),
                 reads=[PSB[3][1], Bs_], writes=[Bs_])
        hTb = [sbt(f"c_hT{i}", [128, 8, 512], BF16) for i in range(2)]
        BhT = [Buf("c_hT") for _ in range(2)]
        uT = sbt("c_uT", [128, 8, 512], BF16)
        Bu = Buf("c_uT")
        usT = sbt("c_usT", [128, 8, 512], BF16)
        Bus = Buf("c_usT")
        sT = sbt("c_sT", [128, 512])
        BsT = Buf("c_sT")
        vraw = [sbt(f"c_vraw{i}", [128, D]) for i in range(2)]
        vn = [sbt(f"c_vn{i}", [128, D], BF16) for i in range(4)]
        Bvr = [Buf("c_vraw") for _ in range(2)]
        Bvn = [Buf("c_vn") for _ in range(4)]
        stats = [sbt(f"c_stats{i}", [128, 2, 6]) for i in range(2)]
        mv = [sbt(f"c_mv{i}", [128, 2]) for i in range(2)]
        vsd = [sbt(f"c_vsd{i}", [128, 1]) for i in range(2)]
        vrs = [sbt(f"c_vrs{i}", [128, 1]) for i in range(2)]
        wks = mk_wk(sbt, "c", 2, with_x=True)
        ewks = mk_wk(sbt, "ce", 2, with_x=True, with_xn=False)
        gi = 0

        def gelu_from_psum(ps_ap, Bps, out_ap, Bout):
            P.op(ACT, lambda: nc.scalar.activation(out=out_ap, in_=ps_ap, func=AF.Gelu_apprx_tanh), reads=Bps, writes=[Bout])

        def c_prenorm(blk):
            hb, Bhb = hTb[blk % 2], BhT[blk % 2]

            def c_pa(t):
                wk = wks[t % 2]
                r0 = blk * 512 + t * 128
                P.dma(SP, wk["x"][:], xB[r0:r0 + 128, :], writes=[wk["Bx"]])
                prenorm_a(wk["x"][:], wk["Bx"], wk)
            c_pa(0)
            for t in range(4):
                if t + 1 < 4:
                    c_pa(t + 1)
                prenorm_b(A, S, 0, hb[:, :, t * 128:(t + 1) * 128], Bhb, wks[t % 2], (3, t % 2))

        c_prenorm(0)
        for blk in range(8):
            hb, Bhb = hTb[blk % 2], BhT[blk % 2]
            for f in range(8):
                pi_ = f % 2
                for kc in range(8):
                    P.op(PE, lambda kc=kc, f=f, pi_=pi_: nc.tensor.matmul(PS[pi_][:, 0:512], lhsT=wi_[:, kc, f * 128:(f + 1) * 128], rhs=hb[:, kc, :],
                                                                            start=(kc == 0), stop=(kc == 7)),
                         reads=[Bhb, Bw], writes=[PSB[pi_][0]], inc=(kc == 7))
                gelu_from_psum(PS[pi_][:, 0:512], [PSB[pi_][0]], uT[:, f, :], Bu)
            for t in range(4):
                k2 = t % 2
                for half in range(2):
                    for kc in range(8):
                        P.op(PE, lambda kc=kc, half=half, t=t: nc.tensor.matmul(PS[2][:, half * 512:(half + 1) * 512], lhsT=hb[:, kc, t * 128:(t + 1) * 128],
                                                                                 rhs=wi_[:, kc, 1024 + half * 512:1024 + (half + 1) * 512], start=(kc == 0), stop=(kc == 7)),
                             reads=[Bhb, Bw], writes=[PSB[2][half]], inc=(kc == 7))
                    gelu_from_psum(PS[2][:, half * 512:(half + 1) * 512], [PSB[2][half]], vraw[k2][:, half * 512:(half + 1) * 512], Bvr[k2])
                for half in range(2):
                    P.op(DVE, lambda half=half, k2=k2: nc.vector.bn_stats(out=stats[k2][:, half, :], in_=vraw[k2][:, half * 512:(half + 1) * 512]),
                         reads=[Bvr[k2]], writes=[Bvr[k2]])
                P.op(DVE, lambda k2=k2: nc.vector.bn_aggr(out=mv[k2][:], in_=stats[k2][:].rearrange("p a s -> p (a s)")), reads=[Bvr[k2]], writes=[Bvr[k2]])
                P.op(ACT, lambda k2=k2: nc.scalar.activation(out=vsd[k2][:], in_=mv[k2][:, 1:2], func=AF.Sqrt, bias=eps_t[:], scale=1.0),
                     reads=[Bvr[k2], B_consts], writes=[Bvr[k2]])
                P.op(DVE, lambda k2=k2: nc.vector.reciprocal(out=vrs[k2][:], in_=vsd[k2][:]), reads=[Bvr[k2]], writes=[Bvr[k2]])
                P.op(DVE, lambda k2=k2, t=t: nc.vector.tensor_scalar(out=vn[t][:], in0=vraw[k2][:], scalar1=mv[k2][:, 0:1], scalar2=vrs[k2][:],
                                                                   op0=ALU.subtract, op1=ALU.mult), reads=[Bvr[k2]], writes=[Bvn[t]])
            for g in range(8):
                pi_ = g % 2
                for t in range(4):
                    P.op(PE, lambda g=g, t=t, pi_=pi_: nc.tensor.matmul(PS[pi_][:, 512 + t * 128:512 + (t + 1) * 128], lhsT=vn[t][:, g * 128:(g + 1) * 128],
                                                                          rhs=WsT[:, g, :], start=True, stop=True),
                         reads=[Bvn[t], Bs_], writes=[PSB[pi_][1]], inc=(t == 3))
                P.op(DVE, lambda g=g, pi_=pi_: nc.vector.scalar_tensor_tensor(
                    out=sT[:].rearrange("p (c q) -> p c q", c=4), in0=PS[pi_][:, 512:1024].rearrange("p (c q) -> p c q", c=4), scalar=gvT[:, g:g + 1],
                    in1=Rg[:, g, :].unsqueeze(1).to_broadcast([128, 4, 128]), op0=ALU.mult, op1=ALU.add),
                    reads=[PSB[pi_][1], Bs_], writes=[BsT])
                P.op(DVE, lambda g=g: nc.vector.tensor_tensor(out=usT[:, g, :], in0=uT[:, g, :], in1=sT[:], op=ALU.mult), reads=[Bu, BsT], writes=[Bus])
            if blk + 1 < 8:
                c_prenorm(blk + 1)
            for t in range(4):
                wk = ewks[t % 2]
                py = 3 if t % 2 == 0 else 2
                for half in range(2):
                    for c in range(8):
                        P.op(PE, lambda c=c, half=half, t=t: nc.tensor.matmul(PS[py][:, half * 512:(half + 1) * 512], lhsT=usT[:, c, t * 128:(t + 1) * 128],
                                                                               rhs=wo_[:, c, half * 512:(half + 1) * 512], start=(c == 0), stop=(c == 7)),
                             reads=[Bus, Bw], writes=[PSB[py][half]], inc=(c == 7))
                r0 = blk * 512 + t * 128
                epilogue(PS[py][:], [PSB[py][0], PSB[py][1]], xB[r0:r0 + 128, :], G, xC[r0:r0 + 128, :], wk)
        P.barrier()
        st.close()

    if "0" in phases:
        phase0()
    if "A" in phases:
        phaseA()
    if "B" in phases:
        phase_ffn("e", xA, xB, 1, D_FF, 2, W["e_w_gate"], W["e_w_up"], W["e_w_down"], False)
    if "C" in phases:
        phaseC()
    if "D" in phases:
        if cfg.get("dense_moe", False):
            phase_ffn("o", xown if cfg.get("d_in") == "xown" else xC, out_d, NEXP, E_FF, 4, W["o_w_gate"], W["o_w_up"], W["o_w_down"], True)
        else:
            phase_moe(xown if cfg.get("d_in") == "xown" else xC, out_d)
    if dbg:
        dump = dscr("dbg_mod", [128, 8 * 2 * 8 + 4 * 0], out=True)
        i = 0
        for L in ("e", "o"):
            for k in ("A_mix", "S_mix", "A_ffn", "S_ffn"):
                P.dma(SP, dump[:, i * 16:(i + 1) * 16], AS[L + k][:].rearrange("p c r -> p (c r)"), reads=[B_mod])
                i += 1
        dumpG = dscr("dbg_G", [4, 128, D], out=True)
        for i, k in enumerate(("e_mix", "e_ffn", "o_mix", "o_ffn")):
            P.dma(SP, dumpG[i], Gt[k][:], reads=[B_mod])
    P.drain_all(SP)
    P.drain_all(POOL)
    es.close()
    return nc


def _rope_tables():
    rows = SEQ // 64
    r, col = np.meshgrid(np.arange(rows, dtype=np.float32), np.arange(64, dtype=np.float32), indexing="ij")
    inv = (1.0 / (10000.0 ** (np.arange(0, 32, 2, dtype=np.float32) / 32.0))).astype(np.float32)
    ang = np.concatenate([r.reshape(-1, 1) * inv, col.reshape(-1, 1) * inv], axis=-1).astype(np.float32)
    return np.concatenate([np.cos(ang), np.sin(ang)], axis=-1).astype(np.float32)


def _fm(v):
    return np.ascontiguousarray(np.asarray(v, np.float32).reshape(8, 128).T)


def _core_inputs(inp, r, rope_tab):
    b, h = r // 2, r % 2
    f = lambda a: np.ascontiguousarray(np.asarray(a, np.float32))
    x = inp["x"]
    own = f(x[b, h * NOWN:(h + 1) * NOWN])
    oth = f(x[b, (1 - h) * NOWN:(2 - h) * NOWN])
    cv = np.stack([np.asarray(inp["c"][b], np.float32), np.asarray(inp["c_ctx"], np.float32)], axis=-1)
    cvecT = np.ascontiguousarray(cv.reshape(8, 128, 2).transpose(1, 0, 2).reshape(128, 16))
    rope_c = np.concatenate([rope_tab[h * NOWN:(h + 1) * NOWN], rope_tab[(1 - h) * NOWN:(2 - h) * NOWN]], axis=0)
    hm = np.zeros((128, 2), np.float32)
    hm[:, 0] = float(h)
    hm[:, 1] = float(1 - h)
    m = {"xown": own, "xoth": oth, "ctx": f(inp["ctx"][b]), "cvecT": cvecT, "rope": np.ascontiguousarray(rope_c),
         "ident": np.eye(128, dtype=np.float32), "halo_mask": hm}
    m["e_g_pre_mixT"] = _fm(inp["e_g_pre_mix"][0])
    m["e_g_pre_ffnT"] = _fm(inp["e_g_pre_ffn"][0])
    m["o_g_pre_mixT"] = _fm(inp["o_g_pre_mix"][0])
    m["o_g_pre_ffnT"] = _fm(inp["o_g_pre_ffn"][0])
    m["e_g_post_mix"] = f(inp["e_g_post_mix"][0]).reshape(1, -1)
    m["e_g_post_ffn"] = f(inp["e_g_post_ffn"][0]).reshape(1, -1)
    m["o_g_post_mix"] = f(inp["o_g_post_mix"][0]).reshape(1, -1)
    m["o_g_post_ffn"] = f(inp["o_g_post_ffn"][0]).reshape(1, -1)
    for pre in ("e", "o"):
        m[pre + "_w_mod"] = f(inp[pre + "_w_mod"][0])
        m[pre + "_bmodT"] = np.ascontiguousarray(np.asarray(inp[pre + "_b_mod"][0], np.float32).reshape(48, 128).T)
        m[pre + "_b_mod"] = f(inp[pre + "_b_mod"][0]).reshape(1, -1)
        m[pre + "_w_in"] = f(inp[pre + "_w_in"][0])
        m[pre + "_w_out"] = f(inp[pre + "_w_out"][0])
    m["e_g_q"] = f(inp["e_g_q"][0]).reshape(1, 64)
    m["e_g_k"] = f(inp["e_g_k"][0]).reshape(1, 64)
    wc = np.asarray(inp["e_w_conv"][0], np.float32)
    m["e_w_convT"] = np.ascontiguousarray(wc.reshape(3, 4, 128).transpose(2, 1, 0).reshape(128, 12))
    m["e_w_gate"] = f(inp["e_w_gate"][0])
    m["e_w_up"] = f(inp["e_w_up"][0])
    m["e_w_down"] = f(inp["e_w_down"][0])
    m["o_g_vT"] = _fm(inp["o_g_v"][0])
    m["o_b_vT"] = _fm(inp["o_b_v"][0])
    m["o_w_s"] = f(inp["o_w_s"][0])
    m["o_b_s"] = f(inp["o_b_s"][0])
    m["o_w_router"] = f(inp["o_w_router"][0])
    m["o_b_router"] = f(inp["o_b_router"][0]).reshape(1, NEXP)
    m["o_w_gate"] = f(inp["o_w_gate"][0])
    m["o_w_up"] = f(inp["o_w_up"][0])
    m["o_w_down"] = f(inp["o_w_down"][0])
    return m


def kernel(**inputs):
    nc = _build({})
    rope_tab = _rope_tables()
    in_maps = [_core_inputs(inputs, r, rope_tab) for r in range(8)]
    res = run_bass_kernel_spmd(nc, in_maps, core_ids=list(range(8)))
    out = np.empty((NB, SEQ, D), np.float32)
    for r in range(8):
        b, h = r // 2, r % 2
        out[b, h * NOWN:(h + 1) * NOWN] = res.results[r]["out"]
    return out
```

```python
import numpy as np
from contextlib import ExitStack
import concourse.bass as bass
import concourse.mybir as mybir
from concourse.bass_utils import run_bass_kernel_spmd

F32 = mybir.dt.float32
BF16 = mybir.dt.bfloat16
AF = mybir.ActivationFunctionType
ALU = mybir.AluOpType
AX = mybir.AxisListType

D = 1024
SEQ = 8192
NB = 4
CTX = 256
NOWN = 4096
NT_OWN = 32
EPS = 1e-6
IN_W = 2304
D_FF = 2816
E_FF = 3584
NEXP = 8


class Buf:
    __slots__ = ("name", "w", "r")

    def __init__(self, name):
        self.name = name
        self.w = None
        self.r = {}


class Eng:
    def __init__(self, nc, name, eng):
        self.name = name
        self.e = eng
        self.sem = nc.alloc_semaphore("sem_" + name)
        self.cnt = 0
        self.waited = {}

    def wait(self, tok):
        if tok is None:
            return
        key, sem, val, _ = tok
        if self.waited.get(key, 0) >= val:
            return
        self.e.wait_ge(sem, val)
        self.waited[key] = val


class Prog:
    def __init__(self, nc):
        self.nc = nc
        self.PE = Eng(nc, "pe", nc.tensor)
        self.ACT = Eng(nc, "act", nc.scalar)
        self.DVE = Eng(nc, "dve", nc.vector)
        self.POOL = Eng(nc, "pool", nc.gpsimd)
        self.SP = Eng(nc, "sp", nc.sync)
        self.dsem = {}
        for q, n in (("sp", 12), ("pool", 8)):
            self.dsem[q] = [[nc.alloc_semaphore(f"dq_{q}{i}"), 0, f"dq_{q}{i}"] for i in range(n)]
        self.dnext = {"sp": 0, "pool": 0}

    def _deps(self, E, reads, writes):
        for b in reads:
            E.wait(b.w)
        for b in writes:
            if b.w is not None and not (E.name == "pe" and b.w[3] == "pe"):
                E.wait(b.w)
            for t in b.r.values():
                if not (t[3] == E.name and E.name == "pe"):
                    E.wait(t)

    def _record(self, tok, ename, reads, writes):
        for b in reads:
            b.r[ename] = tok
        for b in writes:
            b.w = tok
            b.r = {}

    def op(self, E, fn, reads=(), writes=(), inc=True):
        self._deps(E, reads, writes)
        ins = fn()
        if inc:
            E.cnt += 1
            ins.then_inc(E.sem, 1)
            tok = (E.name, E.sem, E.cnt, E.name)
        else:
            tok = (E.name, E.sem, E.cnt + 1, E.name)
        self._record(tok, E.name, reads, writes)
        return ins

    def dma(self, Q, out, in_, reads=(), writes=(), slow=False):
        q = Q.name
        slot = self.dsem[q][self.dnext[q]]
        self.dnext[q] = (self.dnext[q] + 1) % len(self.dsem[q])
        if slot[1] > 0:
            Q.wait((slot[2], slot[0], slot[1], "dma"))
        self._deps(Q, reads, writes)
        if slow:
            ins = Q.e.dma_start(out=out, in_=in_, allow_slow_non_contiguous=True)
        else:
            ins = Q.e.dma_start(out=out, in_=in_)
        slot[1] += 16
        ins.then_inc(slot[0], 16)
        tok = (slot[2], slot[0], slot[1], "dma")
        for b in reads:
            b.r["dma_" + slot[2]] = tok
        for b in writes:
            b.w = tok
            b.r = {}
        return tok

    def dma_ind(self, out, out_offset, in_, in_offset, reads=(), writes=()):
        Q = self.POOL
        q = "pool"
        slot = self.dsem[q][self.dnext[q]]
        self.dnext[q] = (self.dnext[q] + 1) % len(self.dsem[q])
        if slot[1] > 0:
            Q.wait((slot[2], slot[0], slot[1], "dma"))
        self._deps(Q, reads, writes)
        ins = Q.e.indirect_dma_start(out=out, out_offset=out_offset, in_=in_, in_offset=in_offset)
        slot[1] += 16
        ins.then_inc(slot[0], 16)
        tok = (slot[2], slot[0], slot[1], "dma")
        for b in reads:
            b.r["dma_" + slot[2]] = tok
        for b in writes:
            b.w = tok
            b.r = {}
        return tok

    def predicated(self, regs, thresh, body):
        nc = self.nc
        engs = (self.PE, self.ACT, self.DVE, self.POOL, self.SP)
        cnt0 = {E.name: E.cnt for E in engs}
        waited0 = {E.name: dict(E.waited) for E in engs}
        d0 = {q: [sl[1] for sl in slots] for q, slots in self.dsem.items()}
        with nc.If_cmp(regs, thresh, "IS_GT"):
            body()
        for E in engs:
            E.waited = waited0[E.name]
        with nc.Else():
            for E in engs:
                delta = E.cnt - cnt0[E.name]
                if delta > 0:
                    E.e.drain()
                    E.e.sem_inc(E.sem, delta)
            for q, slots in self.dsem.items():
                Q = self.SP if q == "sp" else self.POOL
                for i, sl in enumerate(slots):
                    delta = sl[1] - d0[q][i]
                    if delta > 0:
                        if d0[q][i] > 0:
                            Q.e.wait_ge(sl[0], d0[q][i])
                        Q.e.sem_inc(sl[0], delta)
        for E in engs:
            E.waited = waited0[E.name]
        if getattr(self, "_dbgreg", False):
            self._nreg = getattr(self, "_nreg", 0) + 1
            for E in engs:
                got = []
                try:
                    while True:
                        got.append(E.e.alloc_register(f"probe_{E.name}_{self._nreg}_{len(got)}"))
                except Exception:
                    pass
                for r in got:
                    E.e.free_register(r)
                if self._nreg <= 3 or self._nreg % 20 == 0:
                    print("region", self._nreg, E.name, "free regs", len(got), flush=True)

    def barrier(self):
        engs = (self.PE, self.ACT, self.DVE, self.POOL, self.SP)
        for E in engs:
            for O in engs:
                if O is not E and O.cnt > 0:
                    E.wait((O.name, O.sem, O.cnt, O.name))
            self.drain_all(E)

    def drain_all(self, E):
        for q in self.dsem.values():
            for slot in q:
                if slot[1] > 0:
                    E.wait((slot[2], slot[0], slot[1], "dma"))


def _build(cfg):
    dbg = cfg.get("dbg", False)
    phases = cfg.get("phases", "0ABCD")
    nblk_a = cfg.get("nblk_a", 8)
    nc = bass.Bass("TRN2", target_bir_lowering=False)
    es = ExitStack()
    P = Prog(nc)
    PE, ACT, DVE, POOL, SP = P.PE, P.ACT, P.DVE, P.POOL, P.SP

    def din(name, shape, dt=F32):
        return nc.dram_tensor(name, list(shape), dt, kind="ExternalInput").ap()

    def dscr(name, shape, dt=F32, out=False):
        kind = "ExternalOutput" if out else "Internal"
        return nc.dram_tensor(name, list(shape), dt, kind=kind).ap()

    def sb(name, shape, dt=F32):
        return es.enter_context(nc.sbuf_tensor(name, list(shape), dt))

    xown = din("xown", [NOWN, D])
    xoth = din("xoth", [NOWN, D])
    ctx = din("ctx", [CTX, D])
    cvecT = din("cvecT", [128, 16])
    rope = din("rope", [8192, 64])
    ident_in = din("ident", [128, 128])
    halo_mask = din("halo_mask", [128, 2])
    W = {}
    for pre, win_w in (("e", IN_W), ("o", 2048)):
        W[pre + "_w_mod"] = din(pre + "_w_mod", [D, 6 * D])
        W[pre + "_bmodT"] = din(pre + "_bmodT", [128, 48])
        W[pre + "_b_mod"] = din(pre + "_b_mod", [1, 6 * D])
        for v in ("g_pre_mix", "g_pre_ffn"):
            W[pre + "_" + v + "T"] = din(pre + "_" + v + "T", [128, 8])
        for v in ("g_post_mix", "g_post_ffn"):
            W[pre + "_" + v] = din(pre + "_" + v, [1, D])
        W[pre + "_w_in"] = din(pre + "_w_in", [D, win_w])
        W[pre + "_w_out"] = din(pre + "_w_out", [D, D])
    W["e_g_q"] = din("e_g_q", [1, 64])
    W["e_g_k"] = din("e_g_k", [1, 64])
    W["e_w_convT"] = din("e_w_convT", [128, 12])
    W["e_w_gate"] = din("e_w_gate", [D, D_FF])
    W["e_w_up"] = din("e_w_up", [D, D_FF])
    W["e_w_down"] = din("e_w_down", [D_FF, D])
    W["o_g_vT"] = din("o_g_vT", [128, 8])
    W["o_b_vT"] = din("o_b_vT", [128, 8])
    W["o_w_s"] = din("o_w_s", [8, 128, 128])
    W["o_b_s"] = din("o_b_s", [8, 128])
    W["o_w_router"] = din("o_w_router", [D, NEXP])
    W["o_b_router"] = din("o_b_router", [1, NEXP])
    W["o_w_gate"] = din("o_w_gate", [NEXP, D, E_FF])
    W["o_w_up"] = din("o_w_up", [NEXP, D, E_FF])
    W["o_w_down"] = din("o_w_down", [NEXP, E_FF, D])

    out_d = nc.dram_tensor("out", [NOWN, D], F32, kind="ExternalOutput").ap()
    xA = dscr("xA", [NOWN, D], out=dbg)
    xB = dscr("xB", [NOWN, D], out=dbg)
    xC = dscr("xC", [NOWN, D], out=dbg)
    hT_scr = dscr("hT_scr", [8, 128, NOWN + 2], BF16, out=dbg)

    ident_f = sb("ident_f", [128, 128])
    ident_b = sb("ident_b", [128, 128], BF16)
    eps_t = sb("eps_t", [128, 1])
    ones_f = sb("ones_f", [128, 64])
    ones_f_full = sb("ones_f_full", [128, 128])
    hmask = sb("hmask", [128, 2])
    AS = {}
    for L in ("e", "o"):
        for k in ("A_mix", "S_mix", "A_ffn", "S_ffn"):
            AS[L + k] = sb(f"{L}{k}", [128, 8, 2])
    Gt = {k: sb("G_" + k, [128, D]) for k in ("e_mix", "e_ffn", "o_mix", "o_ffn")}
    B_consts = Buf("consts")
    B_mod = Buf("mod")

    PS = [es.enter_context(nc.psum_tensor(f"ps{i}", [128, 1024], F32)) for i in range(4)]
    PSB = [[Buf(f"ps{i}a"), Buf(f"ps{i}b")] for i in range(4)]

    def psb16(i, half):
        return PS[i][:, half * 512:(half + 1) * 512].bitcast(BF16)

    P.dma(SP, ident_f[:], ident_in[:], writes=[B_consts])
    P.dma(SP, hmask[:], halo_mask[:], writes=[B_consts])
    P.op(DVE, lambda: nc.vector.tensor_copy(out=ident_b[:], in_=ident_f[:]), reads=[B_consts], writes=[B_consts])
    P.op(DVE, lambda: nc.vector.memset(eps_t[:], EPS), writes=[B_consts])
    P.op(DVE, lambda: nc.vector.memset(ones_f[:], 1.0), writes=[B_consts])
    P.op(DVE, lambda: nc.vector.memset(ones_f_full[:], 1.0), writes=[B_consts])

    def rstd_from_ss(ss_ap, n, tmp_ap, out_ap, bufs_r, bufs_w, width=1):
        P.op(ACT, lambda: nc.scalar.activation(out=tmp_ap, in_=ss_ap, func=AF.Sqrt, bias=eps_t[:], scale=1.0 / n),
             reads=bufs_r + [B_consts], writes=bufs_w)
        P.op(DVE, lambda: nc.vector.reciprocal(out=out_ap, in_=tmp_ap), reads=bufs_w, writes=bufs_w)

    def phase0():
        st = ExitStack()

        def sbt(name, shape, dt=F32):
            return st.enter_context(nc.sbuf_tensor(name, list(shape), dt))
        cs_raw = sbt("cs_raw", [128, 16])
        cs = sbt("cs", [128, 16], BF16)
        csb = sbt("csb", [128, 8, 128], BF16)
        wm = [sbt(f"wm{i}", [128, 8, 512], BF16) for i in range(2)]
        Bwm = [Buf("wm0"), Buf("wm1")]
        modt = sbt("modt", [128, 48, 2])
        bmodT = sbt("bmodT", [128, 48])
        gpreT = sbt("gpreT", [128, 16])
        brow = sbt("brow", [128, 512])
        grow = sbt("grow", [128, 512])
        Bc = Buf("cs")
        Bm = Buf("modt")
        Bv = Buf("vecs")
        Brow = Buf("rows")
        P.dma(SP, cs_raw[:], cvecT[:], writes=[Bc])
        P.op(ACT, lambda: nc.scalar.activation(out=cs[:], in_=cs_raw[:], func=AF.Silu), reads=[Bc], writes=[Bc])
        for kc in range(8):
            P.op(DVE, lambda kc=kc: nc.vector.tensor_copy(out=csb[:, kc, :], in_=cs[:, 2 * kc:2 * kc + 1].to_broadcast([128, 128])),
                 reads=[Bc], writes=[Bc])
        pi = 0
        for L in ("e", "o"):
            wmod = W[L + "_w_mod"]
            P.dma(SP, bmodT[:], W[L + "_bmodT"][:], writes=[Bv])
            P.dma(SP, gpreT[:, 0:8], W[L + "_g_pre_mixT"][:], writes=[Bv])
            P.dma(SP, gpreT[:, 8:16], W[L + "_g_pre_ffnT"][:], writes=[Bv])
            for piece in range(12):
                s, half = piece // 2, piece % 2
                wb, Bw = wm[pi % 2], Bwm[pi % 2]
                pi += 1
                src = wmod[:, piece * 512:(piece + 1) * 512].rearrange("(kc p) n -> p kc n", p=128)
                P.dma(POOL, wb[:], src, writes=[Bw])
                if s in (0, 1, 3, 4):
                    for j in range(4):
                        idx = s * 8 + half * 4 + j
                        for kc in range(8):
                            P.op(PE, lambda kc=kc, j=j, idx=idx, wb=wb: nc.tensor.matmul(
                                PS[3][:, 2 * idx:2 * idx + 2], lhsT=wb[:, kc, j * 128:(j + 1) * 128],
                                rhs=cs[:, 2 * kc:2 * kc + 2], start=(kc == 0), stop=(kc == 7)),
                                reads=[Bw, Bc], writes=[PSB[3][0]], inc=(kc == 7))
                else:
                    for kc in range(8):
                        P.op(PE, lambda kc=kc, wb=wb: nc.tensor.matmul(
                            PS[3][:, 512:1024], lhsT=csb[:, kc, :], rhs=wb[:, kc, :],
                            start=(kc == 0), stop=(kc == 7)),
                            reads=[Bw, Bc], writes=[PSB[3][1]], inc=(kc == 7))
                    key = L + ("_mix" if s == 2 else "_ffn")
                    col0 = s * D + half * 512
                    P.dma(SP, brow[:], W[L + "_b_mod"][:, col0:col0 + 512].partition_broadcast(128), writes=[Brow])
                    gp = W[L + ("_g_post_mix" if s == 2 else "_g_post_ffn")]
                    P.dma(SP, grow[:], gp[:, half * 512:(half + 1) * 512].partition_broadcast(128), writes=[Brow])
                    gdst = Gt[key][:, half * 512:(half + 1) * 512]
                    P.op(DVE, lambda gdst=gdst: nc.vector.tensor_tensor(out=gdst, in0=PS[3][:, 512:1024], in1=brow[:], op=ALU.add),
                         reads=[PSB[3][1], Brow], writes=[B_mod])
                    P.op(DVE, lambda gdst=gdst: nc.vector.tensor_tensor(out=gdst, in0=gdst, in1=grow[:], op=ALU.mult),
                         reads=[B_mod, Brow], writes=[B_mod, Brow])
            for j0 in (0, 24):
                P.op(DVE, lambda j0=j0: nc.vector.tensor_tensor(
                    out=modt[:, j0:j0 + 16, :], in0=PS[3][:, 2 * j0:2 * j0 + 32].rearrange("p (j r) -> p j r", r=2),
                    in1=bmodT[:, j0:j0 + 16].unsqueeze(2).to_broadcast([128, 16, 2]), op=ALU.add),
                    reads=[PSB[3][0], Bv], writes=[Bm])
            for nm, s_shift, s_scale, goff in (("mix", 0, 1, 0), ("ffn", 3, 4, 8)):
                A = AS[L + "A_" + nm]
                S = AS[L + "S_" + nm]
                P.op(DVE, lambda S=S, s_shift=s_shift: nc.vector.tensor_copy(out=S[:], in_=modt[:, s_shift * 8:s_shift * 8 + 8, :]),
                     reads=[Bm], writes=[B_mod])
                P.op(DVE, lambda A=A, s_scale=s_scale: nc.vector.tensor_scalar(
                    out=A[:], in0=modt[:, s_scale * 8:s_scale * 8 + 8, :], scalar1=1.0, scalar2=None, op0=ALU.add),
                    reads=[Bm], writes=[B_mod])
                P.op(DVE, lambda A=A, goff=goff: nc.vector.tensor_tensor(
                    out=A[:], in0=A[:], in1=gpreT[:, goff:goff + 8].unsqueeze(2).to_broadcast([128, 8, 2]), op=ALU.mult),
                    reads=[B_mod, Bv], writes=[B_mod, Bv, Bm])
        P.barrier()
        st.close()

    def prenorm_a(x_t, Bx, wk):
        P.op(ACT, lambda: nc.scalar.activation(out=wk["junk"][:], in_=x_t, func=AF.Square, accum_out=wk["ss"][:]),
             reads=[Bx], writes=[wk["Bs"]])
        rstd_from_ss(wk["ss"][:], D, wk["sd"][:], wk["rstd"][:], [wk["Bs"]], [wk["Bs"]])
        P.op(DVE, lambda: nc.vector.tensor_scalar(out=wk["xn"][:], in0=x_t, scalar1=wk["rstd"][:], scalar2=None, op0=ALU.mult),
             reads=[Bx, wk["Bs"]], writes=[wk["Bxn"]])

    def prenorm_b(A, S, r, hT_dst, B_h, wk, psi):
        pi_, ph = psi
        pv = psb16(pi_, ph)
        for c in range(8):
            P.op(PE, lambda c=c: nc.tensor.transpose(out=pv[:, c * 128:(c + 1) * 128], in_=wk["xn"][:, c * 128:(c + 1) * 128], identity=ident_b[:]),
                 reads=[wk["Bxn"], B_consts], writes=[PSB[pi_][ph]], inc=(c == 7))
        for c in range(8):
            if c % 2 == 0:
                P.op(DVE, lambda c=c: nc.vector.tensor_scalar(
                    out=hT_dst[:, c, :], in0=pv[:, c * 128:(c + 1) * 128], scalar1=A[:, c, r:r + 1], scalar2=S[:, c, r:r + 1],
                    op0=ALU.mult, op1=ALU.add), reads=[PSB[pi_][ph], B_mod], writes=[B_h])
            else:
                P.op(ACT, lambda c=c: nc.scalar.activation(
                    out=hT_dst[:, c, :], in_=pv[:, c * 128:(c + 1) * 128], func=AF.Identity,
                    bias=S[:, c, r:r + 1], scale=A[:, c, r:r + 1]), reads=[PSB[pi_][ph], B_mod], writes=[B_h])

    def prenorm_tile(x_t, Bx, A, S, r, hT_dst, B_h, wk, psi, fp32=False):
        prenorm_a(x_t, Bx, wk)
        prenorm_b(A, S, r, hT_dst, B_h, wk, psi)

    def epilogue(y_ap, By, x_src_dram, G, out_dram, wk):
        P.dma(SP, wk["x"][:], x_src_dram, writes=[wk["Bx"]])
        P.op(ACT, lambda: nc.scalar.activation(out=wk["junk"][:], in_=y_ap, func=AF.Square, accum_out=wk["ss"][:]),
             reads=By, writes=[wk["Bs"]])
        rstd_from_ss(wk["ss"][:], D, wk["sd"][:], wk["rstd"][:], [wk["Bs"]], [wk["Bs"]])
        P.op(DVE, lambda: nc.vector.scalar_tensor_tensor(out=wk["t"][:], in0=y_ap, scalar=wk["rstd"][:], in1=G[:], op0=ALU.mult, op1=ALU.mult),
             reads=By + [wk["Bs"], B_mod], writes=[wk["Bt"]])
        P.op(POOL, lambda: nc.gpsimd.tensor_tensor(out=wk["x"][:], in0=wk["x"][:], in1=wk["t"][:], op=ALU.add),
             reads=[wk["Bt"]], writes=[wk["Bx"]])
        P.dma(SP, out_dram, wk["x"][:], reads=[wk["Bx"]])

    def mk_wk(sbt, tag, n=2, with_x=True, with_xn=True):
        res = []
        for i in range(n):
            wk = {}
            wk["ss"] = sbt(f"{tag}ss{i}", [128, 1])
            wk["sd"] = sbt(f"{tag}sd{i}", [128, 1])
            wk["rstd"] = sbt(f"{tag}rstd{i}", [128, 1])
            wk["junk"] = sbt(f"{tag}junk{i}", [128, D], BF16)
            if with_xn:
                wk["xn"] = sbt(f"{tag}xn{i}", [128, D], BF16)
            wk["Bs"] = Buf("Bs")
            wk["Bxn"] = Buf("Bxn")
            if with_x:
                wk["x"] = sbt(f"{tag}x{i}", [128, D])
                wk["t"] = sbt(f"{tag}t{i}", [128, D])
                wk["Bx"] = Buf("Bx")
                wk["Bt"] = Buf("Bt")
            res.append(wk)
        return res

    def phaseA():
        st = ExitStack()

        def sbt(name, shape, dt=F32):
            return st.enter_context(nc.sbuf_tensor(name, list(shape), dt))
        NKT = 66
        A, S = AS["eA_mix"], AS["eS_mix"]
        w_in = W["e_w_in"]
        wqs = sbt("wqs", [128, 8, 2048], BF16)
        woA = sbt("woA", [64, 8, D], BF16)
        woC = sbt("woC", [128, 4, D], BF16)
        Bw = Buf("wA")
        w_in_v = w_in.rearrange("(kc p) n -> p kc n", p=128)
        for kc in range(8):
            P.dma(POOL, wqs[:, kc, 0:512], w_in_v[:, kc, 0:512], writes=[Bw])
            P.dma(POOL, wqs[:, kc, 512:2048], w_in_v[:, kc, 768:2304], writes=[Bw])
        wo = W["e_w_out"]
        P.dma(POOL, woA[:], wo[0:512, :].rearrange("(h p) n -> p h n", p=64), writes=[Bw])
        P.dma(POOL, woC[:], wo[512:1024, :].rearrange("(c p) n -> p c n", p=128), writes=[Bw])
        KT = sbt("KT", [128, 2, NKT * 128], BF16)
        VA = sbt("VA", [128, NKT, 2, 66], BF16)
        B_KV = Buf("KV")
        P.op(DVE, lambda: nc.vector.memset(VA[:, :, :, 64:66], 1.0), writes=[B_KV])
        gqB = sbt("gqB", [128, 64])
        gkB = sbt("gkB", [128, 64])
        wconv = sbt("wconv", [128, 12])
        Bg = Buf("g")
        P.dma(SP, gqB[:], W["e_g_q"][:].partition_broadcast(128), writes=[Bg])
        P.dma(SP, gkB[:], W["e_g_k"][:].partition_broadcast(128), writes=[Bg])
        P.dma(SP, wconv[:], W["e_w_convT"][:], writes=[Bg])

        NS = 2
        st_outer = st
        st = ExitStack()
        wkv = sbt("wkv", [128, 8, 256], BF16)
        P.dma(POOL, wkv[:], w_in_v[:, :, 512:768], writes=[Bw])
        wks = mk_wk(sbt, "a1", NS, with_x=True)
        hTt = [sbt(f"a1hT{i}", [128, 8, 128], BF16) for i in range(NS)]
        Bh = [Buf("hTt") for _ in range(NS)]
        ropet = [sbt(f"a1rope{i}", [128, 64]) for i in range(NS)]
        ksq = [sbt(f"a1ksq{i}", [128, 128]) for i in range(NS)]
        kss = [sbt(f"a1kss{i}", [128, 2]) for i in range(NS)]
        ksd = [sbt(f"a1ksd{i}", [128, 2]) for i in range(NS)]
        krs = [sbt(f"a1krs{i}", [128, 2]) for i in range(NS)]
        kn = [sbt(f"a1kn{i}", [128, 2, 64]) for i in range(NS)]
        ktmp = [sbt(f"a1ktmp{i}", [128, 4, 2, 32]) for i in range(NS)]
        kd = [sbt(f"a1kd{i}", [128, 2, 2, 64], BF16) for i in range(NS)]
        Bk = [Buf("k") for _ in range(NS)]
        Bkd = [Buf("kd") for _ in range(NS)]
        Brope = [Buf("rope") for _ in range(NS)]
        def a1_stage1(kt):
            sl = kt % NS
            wk = wks[sl]
            if kt < 2:
                src, r = ctx[kt * 128:(kt + 1) * 128, :], 1
            elif kt < 34:
                src, r = xown[(kt - 2) * 128:(kt - 1) * 128, :], 0
            else:
                src, r = xoth[(kt - 34) * 128:(kt - 33) * 128, :], 0
            P.dma(SP, wk["x"][:], src, writes=[wk["Bx"]])
            if kt >= 2:
                P.dma(SP, ropet[sl][:], rope[(kt - 2) * 128:(kt - 1) * 128, :], writes=[Brope[sl]])
            prenorm_tile(wk["x"][:], wk["Bx"], A, S, r, hTt[sl], Bh[sl], wk, (kt % 2, 0))
        def a1_stage2(kt):
            sl = kt % NS
            kb = kt % 2
            for kc in range(8):
                P.op(PE, lambda kc=kc, sl=sl: nc.tensor.matmul(PS[2][:, kb * 512:kb * 512 + 256], lhsT=hTt[sl][:, kc, :], rhs=wkv[:, kc, :],
                                                             start=(kc == 0), stop=(kc == 7)),
                     reads=[Bh[sl], Bw], writes=[PSB[2][kb]], inc=(kc == 7))
            kps = PS[2][:, kb * 512:kb * 512 + 128]
            vps = PS[2][:, kb * 512 + 128:kb * 512 + 256]
            P.op(ACT, lambda sl=sl: nc.scalar.activation(out=VA[:, kt, :, 0:64], in_=vps.rearrange("p (g d) -> p g d", g=2), func=AF.Copy),
                 reads=[PSB[2][kb]], writes=[B_KV])
            P.op(ACT, lambda sl=sl: nc.scalar.activation(out=ksq[sl][:], in_=kps, func=AF.Square), reads=[PSB[2][kb]], writes=[Bk[sl]])
            P.op(DVE, lambda sl=sl: nc.vector.tensor_reduce(out=kss[sl][:], in_=ksq[sl][:].rearrange("p (g d) -> p g d", g=2), axis=AX.X, op=ALU.add),
                 reads=[Bk[sl]], writes=[Bk[sl]])
            rstd_from_ss(kss[sl][:], 64, ksd[sl][:], krs[sl][:], [Bk[sl]], [Bk[sl]])
            P.op(DVE, lambda sl=sl: nc.vector.tensor_tensor(out=kn[sl][:], in0=kps.rearrange("p (g d) -> p g d", g=2),
                                                          in1=krs[sl][:].unsqueeze(2).to_broadcast([128, 2, 64]), op=ALU.mult),
                 reads=[PSB[2][kb], Bk[sl]], writes=[Bk[sl]])
            kdv = kd[sl]
            if kt < 2:
                P.op(DVE, lambda sl=sl, kdv=kdv: nc.vector.tensor_tensor(out=kdv[:, :, 0, :], in0=kn[sl][:],
                                                                       in1=gkB[:].unsqueeze(1).to_broadcast([128, 2, 64]), op=ALU.mult),
                     reads=[Bk[sl], Bg], writes=[Bkd[sl]])
            else:
                P.op(DVE, lambda sl=sl: nc.vector.tensor_tensor(out=kn[sl][:], in0=kn[sl][:],
                                                              in1=gkB[:].unsqueeze(1).to_broadcast([128, 2, 64]), op=ALU.mult),
                     reads=[Bk[sl], Bg], writes=[Bk[sl]])
                cosb = ropet[sl][:, 0:32].unsqueeze(1).to_broadcast([128, 2, 32])
                sinb = ropet[sl][:, 32:64].unsqueeze(1).to_broadcast([128, 2, 32])
                k1, k2 = kn[sl][:, :, 0:32], kn[sl][:, :, 32:64]
                tt = ktmp[sl]
                for j, (a, b_) in enumerate(((k1, cosb), (k2, sinb), (k2, cosb), (k1, sinb))):
                    P.op(DVE, lambda j=j, a=a, b_=b_, tt=tt: nc.vector.tensor_tensor(out=tt[:, j], in0=a, in1=b_, op=ALU.mult),
                         reads=[Bk[sl], Brope[sl]], writes=[Bk[sl]])
                P.op(DVE, lambda tt=tt, kdv=kdv: nc.vector.tensor_tensor(out=kdv[:, :, 0, 0:32], in0=tt[:, 0], in1=tt[:, 1], op=ALU.subtract),
                     reads=[Bk[sl]], writes=[Bkd[sl]])
                P.op(DVE, lambda tt=tt, kdv=kdv: nc.vector.tensor_tensor(out=kdv[:, :, 0, 32:64], in0=tt[:, 2], in1=tt[:, 3], op=ALU.add),
                     reads=[Bk[sl]], writes=[Bkd[sl]])
            P.op(DVE, lambda kdv=kdv: nc.vector.tensor_copy(out=kdv[:, :, 1, :], in_=kdv[:, :, 0, :]), reads=[Bkd[sl]], writes=[Bkd[sl]])
            pv = psb16(3, kb)
            for g in range(2):
                P.op(PE, lambda g=g, kdv=kdv: nc.tensor.transpose(out=pv[:, g * 128:(g + 1) * 128],
                                                                 in_=kdv[:, g].rearrange("p a d -> p (a d)"), identity=ident_b[:]),
                     reads=[Bkd[sl], B_consts], writes=[PSB[3][kb]], inc=(g == 1))
            P.op(DVE, lambda: nc.vector.tensor_copy(out=KT[:, :, kt * 128:(kt + 1) * 128], in_=pv[:, 0:256].rearrange("p (g t) -> p g t", g=2)),
                 reads=[PSB[3][kb]], writes=[B_KV])
            if 2 <= kt < 34:
                c0 = 1 + (kt - 2) * 128
                P.dma(SP, hT_scr[:, :, c0:c0 + 128].rearrange("c p t -> p c t"), hTt[sl][:], reads=[Bh[sl]])
            if kt == 34:
                P.dma(SP, hT_scr[:, :, NOWN + 1:NOWN + 2].rearrange("c p t -> p c t"), hTt[sl][:, :, 0:1], reads=[Bh[sl]], slow=True)
            if kt == 65:
                P.dma(SP, hT_scr[:, :, 0:1].rearrange("c p t -> p c t"), hTt[sl][:, :, 127:128], reads=[Bh[sl]], slow=True)
        a1_stage1(0)
        for kt in range(NKT):
            if kt + 1 < NKT:
                a1_stage1(kt + 1)
            a1_stage2(kt)
        B_scr = Buf("scr")
        P.barrier()
        st.close()
        st = st_outer

        hTb = [sbt(f"a2hT{i}", [128, 8, 514], BF16) for i in range(2)]
        BhT = [Buf("hTb") for _ in range(2)]
        qT = sbt("qT", [128, 4, 512], BF16)
        BqT = Buf("qT")
        qsq = sbt("qsq", [128, 512])
        qss = sbt("qss", [128, 8])
        qsd = sbt("qsd", [128, 8])
        qrs = sbt("qrs", [128, 8])
        qn = sbt("qn", [128, 8, 64])
        qtmp = sbt("qtmp", [128, 4, 8, 32])
        qr = sbt("qr", [128, 8, 64], BF16)
        ropeq = sbt("ropeq", [128, 64])
        Bq = Buf("q")
        Bqr = Buf("qr")
        Bropeq = Buf("ropeq")
        Zc = sbt("Zc", [128, 514])
        Z = sbt("Z", [128, 514])
        cv = sbt("cv", [128, 512])
        convT = sbt("convT", [128, 4, 512], BF16)
        Bz = Buf("Z")
        Bconv = Buf("convT")
        PT = [sbt(f"PT{i}", [128, 1024], BF16) for i in range(3)]
        BPT = [Buf("PT") for _ in range(3)]
        attnT = sbt("attnT", [64, 8, 512], BF16)
        Battn = Buf("attnT")
        oT = [sbt(f"oT{i}", [128, 512]) for i in range(2)]
        BoT = [Buf("oT") for _ in range(2)]
        bcb = [sbt(f"bcb{i}", [64, 512]) for i in range(2)]
        Bbcb = [Buf("bcb") for _ in range(2)]
        rcd = dscr("rcd", [4, 512])
        Brcd = [Buf("rcd") for _ in range(4)]
        rci = 0
        ewk = mk_wk(sbt, "a2e", 2, with_x=True, with_xn=False)
        pti = 0
        def a2_load(blk):
            hb, Bhb = hTb[blk % 2], BhT[blk % 2]
            P.dma(SP, hb[:], hT_scr[:, :, blk * 512:blk * 512 + 514].rearrange("c p t -> p c t"), writes=[Bhb])

        def a2_qproc_tile(blk, t):
            hb, Bhb = hTb[blk % 2], BhT[blk % 2]
            tok0 = blk * 512 + t * 128
            P.dma(SP, ropeq[:], rope[tok0:tok0 + 128, :], writes=[Bropeq])
            for kc in range(8):
                P.op(PE, lambda kc=kc, t=t: nc.tensor.matmul(PS[3][:, 0:512], lhsT=hb[:, kc, 1 + t * 128:1 + (t + 1) * 128], rhs=wqs[:, kc, 0:512],
                                                             start=(kc == 0), stop=(kc == 7)),
                     reads=[Bhb, Bw], writes=[PSB[3][0]], inc=(kc == 7))
            qps = PS[3][:, 0:512]
            P.op(ACT, lambda: nc.scalar.activation(out=qsq[:], in_=qps, func=AF.Square), reads=[PSB[3][0]], writes=[Bq])
            P.op(DVE, lambda: nc.vector.tensor_reduce(out=qss[:], in_=qsq[:].rearrange("p (h d) -> p h d", h=8), axis=AX.X, op=ALU.add),
                 reads=[Bq], writes=[Bq])
            rstd_from_ss(qss[:], 64, qsd[:], qrs[:], [Bq], [Bq])
            P.op(DVE, lambda: nc.vector.tensor_tensor(out=qn[:], in0=qps.rearrange("p (h d) -> p h d", h=8),
                                                      in1=qrs[:].unsqueeze(2).to_broadcast([128, 8, 64]), op=ALU.mult),
                 reads=[PSB[3][0], Bq], writes=[Bq])
            P.op(POOL, lambda: nc.gpsimd.tensor_tensor(out=qn[:], in0=qn[:], in1=gqB[:].unsqueeze(1).to_broadcast([128, 8, 64]), op=ALU.mult),
                 reads=[Bq, Bg], writes=[Bq])
            cosb = ropeq[:, 0:32].unsqueeze(1).to_broadcast([128, 8, 32])
            sinb = ropeq[:, 32:64].unsqueeze(1).to_broadcast([128, 8, 32])
            q1, q2 = qn[:, :, 0:32], qn[:, :, 32:64]
            for j, (a, b_) in enumerate(((q1, cosb), (q2, sinb), (q2, cosb), (q1, sinb))):
                if j < 2:
                    P.op(DVE, lambda j=j, a=a, b_=b_: nc.vector.tensor_tensor(out=qtmp[:, j], in0=a, in1=b_, op=ALU.mult),
                         reads=[Bq, Bropeq], writes=[Bq])
                else:
                    P.op(POOL, lambda j=j, a=a, b_=b_: nc.gpsimd.tensor_tensor(out=qtmp[:, j], in0=a, in1=b_, op=ALU.mult),
                         reads=[Bq, Bropeq], writes=[Bq])
            P.op(DVE, lambda: nc.vector.tensor_tensor(out=qr[:, :, 0:32], in0=qtmp[:, 0], in1=qtmp[:, 1], op=ALU.subtract),
                 reads=[Bq], writes=[Bqr])
            P.op(POOL, lambda: nc.gpsimd.tensor_tensor(out=qr[:, :, 32:64], in0=qtmp[:, 2], in1=qtmp[:, 3], op=ALU.add),
                 reads=[Bq], writes=[Bqr])
            pv = psb16(3, 1)
            for pr in range(4):
                P.op(PE, lambda pr=pr: nc.tensor.transpose(out=pv[:, pr * 128:(pr + 1) * 128],
                                                          in_=qr[:, 2 * pr:2 * pr + 2, :].rearrange("p h d -> p (h d)"), identity=ident_b[:]),
                     reads=[Bqr, B_consts], writes=[PSB[3][1]], inc=(pr == 3))
            P.op(DVE, lambda t=t: nc.vector.tensor_copy(out=qT[:, :, t * 128:(t + 1) * 128], in_=pv[:, 0:512].rearrange("p (a t) -> p a t", a=4)),
                 reads=[PSB[3][1]], writes=[BqT])

        def a2_conv(blk):
            hb, Bhb = hTb[blk % 2], BhT[blk % 2]
            first, last = (blk == 0), (blk == 7)
            for c in range(4):
                def proj(colbase, ps_ap_main, ps_ap_halo, Bps, halo):
                    for kc in range(8):
                        P.op(PE, lambda kc=kc: nc.tensor.matmul(ps_ap_main, lhsT=wqs[:, kc, colbase:colbase + 128], rhs=hb[:, kc, 1:513],
                                                                start=(kc == 0), stop=(kc == 7)),
                             reads=[Bhb, Bw], writes=[Bps], inc=(kc == 7 and not halo))
                    if halo:
                        for kc in range(8):
                            P.op(PE, lambda kc=kc: nc.tensor.matmul(ps_ap_halo, lhsT=wqs[:, kc, colbase:colbase + 128], rhs=hb[:, kc, 0:514:513],
                                                                    start=(kc == 0), stop=(kc == 7)),
                                 reads=[Bhb, Bw], writes=[halo], inc=(kc == 7))
                proj(512 + 512 + c * 128, PS[3][:, 0:512], PS[1][:, 0:2], PSB[3][0], PSB[1][0])
                P.op(ACT, lambda: nc.scalar.activation(out=Zc[:, 1:513], in_=PS[3][:, 0:512], func=AF.Copy), reads=[PSB[3][0]], writes=[Bz])
                P.op(ACT, lambda: nc.scalar.activation(out=Zc[:, 0:514:513], in_=PS[1][:, 0:2], func=AF.Copy), reads=[PSB[1][0]], writes=[Bz])
                proj(512 + 1024 + c * 128, PS[3][:, 512:1024], PS[1][:, 512:514], PSB[3][1], PSB[1][1])
                P.op(DVE, lambda: nc.vector.tensor_tensor(out=Z[:, 1:513], in0=PS[3][:, 512:1024], in1=Zc[:, 1:513], op=ALU.mult),
                     reads=[PSB[3][1], Bz], writes=[Bz])
                P.op(DVE, lambda: nc.vector.tensor_tensor(out=Z[:, 0:514:513], in0=PS[1][:, 512:514], in1=Zc[:, 0:514:513], op=ALU.mult),
                     reads=[PSB[1][1], Bz], writes=[Bz])
                if first:
                    P.op(DVE, lambda: nc.vector.tensor_scalar(out=Z[:, 0:1], in0=Z[:, 0:1], scalar1=hmask[:, 0:1], scalar2=None, op0=ALU.mult),
                         reads=[Bz, B_consts], writes=[Bz])
                if last:
                    P.op(DVE, lambda: nc.vector.tensor_scalar(out=Z[:, 513:514], in0=Z[:, 513:514], scalar1=hmask[:, 1:2], scalar2=None, op0=ALU.mult),
                         reads=[Bz, B_consts], writes=[Bz])
                proj(512 + c * 128, PS[3][:, 0:512], None, PSB[3][0], None)
                P.op(DVE, lambda c=c: nc.vector.tensor_scalar(out=cv[:], in0=Z[:, 0:512], scalar1=wconv[:, 3 * c:3 * c + 1], scalar2=None, op0=ALU.mult),
                     reads=[Bz, Bg], writes=[Bz])
                P.op(DVE, lambda c=c: nc.vector.scalar_tensor_tensor(out=cv[:], in0=Z[:, 1:513], scalar=wconv[:, 3 * c + 1:3 * c + 2], in1=cv[:],
                                                                      op0=ALU.mult, op1=ALU.add), reads=[Bz, Bg], writes=[Bz])
                P.op(DVE, lambda c=c: nc.vector.scalar_tensor_tensor(out=cv[:], in0=Z[:, 2:514], scalar=wconv[:, 3 * c + 2:3 * c + 3], in1=cv[:],
                                                                      op0=ALU.mult, op1=ALU.add), reads=[Bz, Bg], writes=[Bz])
                P.op(DVE, lambda c=c: nc.vector.tensor_tensor(out=convT[:, c, :], in0=PS[3][:, 0:512], in1=cv[:], op=ALU.mult),
                     reads=[PSB[3][0], Bz], writes=[Bconv])

        def a2_attn(blk):
            nonlocal pti, rci
            Sbuf = [(PS[0], PSB[0]), (PS[1], PSB[1]), (PS[3], PSB[3])]
            seq = [(hp_, kt_) for hp_ in range(4) for kt_ in range(NKT)]

            def s_mm(i):
                hp, kt = seq[i]
                g = hp // 2
                ps_, pb_ = Sbuf[i % 3]
                P.op(PE, lambda: nc.tensor.matmul(ps_[:, 0:512], lhsT=KT[0:64, g, kt * 128:(kt + 1) * 128], rhs=qT[0:64, hp, :], start=True, stop=True),
                     reads=[B_KV, BqT], writes=[pb_[0]], inc=False)
                P.op(PE, lambda: nc.tensor.matmul(ps_[:, 512:1024], lhsT=KT[64:128, g, kt * 128:(kt + 1) * 128], rhs=qT[64:128, hp, :], start=True, stop=True),
                     reads=[B_KV, BqT], writes=[pb_[1]], inc=True)
            s_mm(0)
            s_mm(1)
            for i, (hp, kt) in enumerate(seq):
                g = hp // 2
                if i + 2 < len(seq):
                    s_mm(i + 2)
                ps_, pb_ = Sbuf[i % 3]
                pt, Bpt = PT[pti % 3], BPT[pti % 3]
                pti += 1
                P.op(ACT, lambda ps_=ps_, pt=pt: nc.scalar.activation(out=pt[:], in_=ps_[:], func=AF.Exp, scale=0.125),
                     reads=[pb_[0], pb_[1]], writes=[Bpt])
                for hh in range(2):
                    P.op(PE, lambda hh=hh, pt=pt: nc.tensor.matmul(PS[2][0:65, hh * 512:(hh + 1) * 512], lhsT=VA[:, kt, g, 0:65],
                                                                  rhs=pt[:, hh * 512:(hh + 1) * 512], start=(kt == 0), stop=(kt == NKT - 1)),
                         reads=[B_KV, Bpt], writes=[PSB[2][hh]], inc=(kt == NKT - 1 or hh == 1))
                if kt == NKT - 1:
                    for hh in range(2):
                        o = oT[hh]
                        P.op(DVE, lambda hh=hh, o=o: nc.vector.tensor_copy(out=o[0:64, :], in_=PS[2][0:64, hh * 512:(hh + 1) * 512]),
                             reads=[PSB[2][hh]], writes=[BoT[hh]])
                        P.op(DVE, lambda hh=hh, o=o: nc.vector.reciprocal(out=o[64:65, :], in_=PS[2][64:65, hh * 512:(hh + 1) * 512]),
                             reads=[PSB[2][hh]], writes=[BoT[hh]])
                        slot = rci % 4
                        rci += 1
                        P.dma(SP, rcd[slot:slot + 1, :], o[64:65, :], reads=[BoT[hh]], writes=[Brcd[slot]])
                        P.dma(SP, bcb[hh][:, :], rcd[slot:slot + 1, :].partition_broadcast(64), reads=[Brcd[slot]], writes=[Bbcb[hh]])
                    for hh in range(2):
                        h = 2 * hp + hh
                        P.op(DVE, lambda h=h, hh=hh: nc.vector.tensor_tensor(out=attnT[:, h, :], in0=oT[hh][0:64, :], in1=bcb[hh][:, :], op=ALU.mult),
                             reads=[BoT[hh], Bbcb[hh]], writes=[Battn])

        def a2_wout_tile(blk, t):
            PSy, PSBy = (PS[0], PSB[0]) if t % 2 == 0 else (PS[1], PSB[1])
            wk = ewk[t % 2]
            for half in range(2):
                n0 = half * 512
                for h in range(8):
                    P.op(PE, lambda h=h, n0=n0, t=t: nc.tensor.matmul(PSy[:, n0:n0 + 512], lhsT=attnT[:, h, t * 128:(t + 1) * 128],
                                                                       rhs=woA[:, h, n0:n0 + 512], start=(h == 0), stop=False),
                         reads=[Battn, Bw], writes=[PSBy[half]], inc=False)
                for c in range(4):
                    P.op(PE, lambda c=c, n0=n0, t=t: nc.tensor.matmul(PSy[:, n0:n0 + 512], lhsT=convT[:, c, t * 128:(t + 1) * 128],
                                                                       rhs=woC[:, c, n0:n0 + 512], start=False, stop=(c == 3)),
                         reads=[Bconv, Bw], writes=[PSBy[half]], inc=(c == 3))
            r0 = blk * 512 + t * 128
            epilogue(PSy[:], [PSBy[0], PSBy[1]], xown[r0:r0 + 128, :], Gt["e_mix"], xA[r0:r0 + 128, :], wk)

        a2_load(0)
        for t in range(4):
            a2_qproc_tile(0, t)
        for blk in range(nblk_a):
            a2_conv(blk)
            if blk + 1 < nblk_a:
                a2_load(blk + 1)
            a2_attn(blk)
            for t in range(4):
                if blk + 1 < nblk_a:
                    a2_qproc_tile(blk + 1, t)
                a2_wout_tile(blk, t)
        P.barrier()
        st.close()

    def phase_ffn(L, x_in, x_out, n_exp, ff, gch, wg_d, wu_d, wd_d, fp32_router):
        st = ExitStack()

        def sbt(name, shape, dt=F32):
            return st.enter_context(nc.sbuf_tensor(L + name, list(shape), dt))
        A, S = AS[L + "A_ffn"], AS[L + "S_ffn"]
        G = Gt[L + "_ffn"]
        TB = 2048
        NTB = TB // 128
        gw = gch * 128
        ngrp = ff // gw
        nhb = 1
        hT_l = [sbt(f"f_hT{i}", [128, 8, TB], BF16) for i in range(nhb)]
        BhT_l = [[Buf("f_hT") for _ in range(NTB)] for _ in range(nhb)]
        hT, BhT = hT_l[0], BhT_l[0]
        acc = sbt("f_acc", [128, NTB, D])
        Bacc = [Buf("f_acc") for _ in range(NTB)]
        wgb = [sbt(f"f_wg{i}", [128, 8, gw], BF16) for i in range(2)]
        wub = [sbt(f"f_wu{i}", [128, 8, gw], BF16) for i in range(2)]
        wdb = [sbt(f"f_wd{i}", [128, gch, D], BF16) for i in range(2)]
        Bwt = [Buf("f_w") for _ in range(2)]
        act = [sbt(f"f_act{i}", [128, gch, 512], BF16) for i in range(2)]
        Bact = [Buf("f_act") for _ in range(2)]
        sil = [sbt(f"f_sil{i}", [128, 512]) for i in range(2)]
        Bsil = [Buf("f_sil") for _ in range(2)]
        wks = mk_wk(sbt, "f", 2, with_x=True, with_xn=not fp32_router)
        ewks = wks if fp32_router else mk_wk(sbt, "fe", 2, with_x=True, with_xn=False)
        if fp32_router:
            gates = sbt("f_gates", [128, NTB, NEXP])
            Bgates = [Buf("gates") for _ in range(NTB)]
            wr = sbt("f_wr", [128, 8, NEXP])
            brB = sbt("f_brB", [128, NEXP])
            Bwr = Buf("wr")
            P.dma(SP, wr[:], W["o_w_router"].rearrange("(kc p) n -> p kc n", p=128), writes=[Bwr])
            P.dma(SP, brB[:], W["o_b_router"][:].partition_broadcast(128), writes=[Bwr])
            xn32 = [sbt(f"f_xn32{i}", [128, D]) for i in range(2)]
            h32 = sbt("f_h32", [128, 8, 128])
            Bx32 = [Buf("xn32") for _ in range(2)]
            Bh32 = Buf("h32")
            rt = {k: sbt("f_rt_" + k, [128, 8]) for k in ("lg", "m1", "l2", "m2", "g")}
            rs = {k: sbt("f_rs_" + k, [128, 1]) for k in ("mx1", "mx2", "d", "e", "den", "w1", "w2")}
            Brt = Buf("rt")
        wi = 0
        si = 0
        for tb in range(NOWN // TB):
            def f_pa(t, tbx=None):
                tbx = tb if tbx is None else tbx
                wk = wks[t % 2]
                r0 = tbx * TB + t * 128
                P.dma(SP, wk["x"][:], x_in[r0:r0 + 128, :], writes=[wk["Bx"]])
                if not fp32_router:
                    prenorm_a(wk["x"][:], wk["Bx"], wk)
                else:
                    xs = xn32[t % 2]
                    P.op(ACT, lambda wk=wk: nc.scalar.activation(out=wk["junk"][:], in_=wk["x"][:], func=AF.Square, accum_out=wk["ss"][:]),
                         reads=[wk["Bx"]], writes=[wk["Bs"]])
                    rstd_from_ss(wk["ss"][:], D, wk["sd"][:], wk["rstd"][:], [wk["Bs"]], [wk["Bs"]])
                    P.op(DVE, lambda wk=wk: nc.vector.tensor_scalar(out=xs[:], in0=wk["x"][:], scalar1=wk["rstd"][:], scalar2=None, op0=ALU.mult),
                         reads=[wk["Bx"], wk["Bs"]], writes=[Bx32[t % 2]])

            def f_pb(t, tbx=None):
                tbx = tb if tbx is None else tbx
                wk = wks[t % 2]
                if not fp32_router:
                    prenorm_b(A, S, 0, hT_l[tbx % nhb][:, :, t * 128:(t + 1) * 128], BhT_l[tbx % nhb][t], wk, (3, t % 2))
                    return
                hdst = hT[:, :, t * 128:(t + 1) * 128]
                xs = xn32[t % 2]
                pp = 3 if t % 2 == 0 else 1
                for c in range(8):
                    half = c // 4
                    P.op(PE, lambda c=c: nc.tensor.transpose(out=PS[pp][:, c * 128:(c + 1) * 128], in_=xs[:, c * 128:(c + 1) * 128], identity=ident_f[:]),
                         reads=[Bx32[t % 2], B_consts], writes=[PSB[pp][half]], inc=(c % 4 == 3))
                for c in range(8):
                    half = c // 4
                    P.op(DVE, lambda c=c: nc.vector.tensor_scalar(out=h32[:, c, :], in0=PS[pp][:, c * 128:(c + 1) * 128], scalar1=A[:, c, 0:1],
                                                                  scalar2=S[:, c, 0:1], op0=ALU.mult, op1=ALU.add),
                         reads=[PSB[pp][half], B_mod], writes=[Bh32])
                P.op(POOL, lambda hdst=hdst: nc.gpsimd.tensor_copy(out=hdst, in_=h32[:]), reads=[Bh32], writes=[BhT[t]])
                for kc in range(8):
                    P.op(PE, lambda kc=kc: nc.tensor.matmul(PS[2][:, 0:NEXP], lhsT=h32[:, kc, :], rhs=wr[:, kc, :], start=(kc == 0), stop=(kc == 7)),
                         reads=[Bh32, Bwr], writes=[PSB[2][0]], inc=(kc == 7))
                V = nc.vector
                P.op(DVE, lambda: V.tensor_tensor(out=rt["lg"][:], in0=PS[2][:, 0:NEXP], in1=brB[:], op=ALU.add), reads=[PSB[2][0], Bwr], writes=[Brt])
                P.op(DVE, lambda: V.tensor_reduce(out=rs["mx1"][:], in_=rt["lg"][:], axis=AX.X, op=ALU.max), reads=[Brt], writes=[Brt])
                P.op(DVE, lambda: V.tensor_scalar(out=rt["m1"][:], in0=rt["lg"][:], scalar1=rs["mx1"][:], scalar2=None, op0=ALU.is_equal), reads=[Brt], writes=[Brt])
                P.op(DVE, lambda: V.scalar_tensor_tensor(out=rt["l2"][:], in0=rt["m1"][:], scalar=-1e30, in1=rt["lg"][:], op0=ALU.mult, op1=ALU.add), reads=[Brt], writes=[Brt])
                P.op(DVE, lambda: V.tensor_reduce(out=rs["mx2"][:], in_=rt["l2"][:], axis=AX.X, op=ALU.max), reads=[Brt], writes=[Brt])
                P.op(DVE, lambda: V.tensor_scalar(out=rt["m2"][:], in0=rt["l2"][:], scalar1=rs["mx2"][:], scalar2=None, op0=ALU.is_equal), reads=[Brt], writes=[Brt])
                P.op(DVE, lambda: V.tensor_tensor(out=rs["d"][:], in0=rs["mx2"][:], in1=rs["mx1"][:], op=ALU.subtract), reads=[Brt], writes=[Brt])
                P.op(ACT, lambda: nc.scalar.activation(out=rs["e"][:], in_=rs["d"][:], func=AF.Exp), reads=[Brt], writes=[Brt])
                P.op(DVE, lambda: V.tensor_scalar(out=rs["den"][:], in0=rs["e"][:], scalar1=1.0, scalar2=None, op0=ALU.add), reads=[Brt], writes=[Brt])
                P.op(DVE, lambda: V.reciprocal(out=rs["w1"][:], in_=rs["den"][:]), reads=[Brt], writes=[Brt])
                P.op(DVE, lambda: V.tensor_tensor(out=rs["w2"][:], in0=rs["e"][:], in1=rs["w1"][:], op=ALU.mult), reads=[Brt], writes=[Brt])
                P.op(DVE, lambda: V.tensor_scalar(out=rt["g"][:], in0=rt["m1"][:], scalar1=rs["w1"][:], scalar2=None, op0=ALU.mult), reads=[Brt], writes=[Brt])
                P.op(DVE, lambda t=t: V.scalar_tensor_tensor(out=gates[:, t, :], in0=rt["m2"][:], scalar=rs["w2"][:], in1=rt["g"][:], op0=ALU.mult, op1=ALU.add),
                     reads=[Brt], writes=[Bgates[t]])
            if True:
                f_pa(0)
                for t in range(NTB):
                    if t + 1 < NTB:
                        f_pa(t + 1)
                    f_pb(t)
            hT, BhT = hT_l[tb % nhb], BhT_l[tb % nhb]
            if dbg and fp32_router and tb == 0:
                dg = dscr("dbg_gates", [128, NTB, NEXP], out=True)
                dh = dscr("dbg_hT", [128, 8, TB], BF16, out=True)
                P.dma(SP, dg[:], gates[:], reads=Bgates)
                P.dma(SP, dh[:], hT[:], reads=BhT)
            elist = cfg.get("exp_list", list(range(n_exp))) if n_exp > 1 else [0]
            items = [(e, grp, sbk) for e in elist for grp in range(ngrp) for sbk in range(TB // 512)]
            state = {}

            def load_w(e, grp):
                nonlocal wi
                wsl = wi % 2
                wi += 1
                f0 = grp * gw
                if n_exp == 1:
                    gsrc, usrc, dsrc = wg_d[:, f0:f0 + gw], wu_d[:, f0:f0 + gw], wd_d[f0:f0 + gw, :]
                else:
                    gsrc, usrc, dsrc = wg_d[e, :, f0:f0 + gw], wu_d[e, :, f0:f0 + gw], wd_d[e, f0:f0 + gw, :]
                P.dma(POOL, wgb[wsl][:], gsrc.rearrange("(kc p) n -> p kc n", p=128), writes=[Bwt[wsl]])
                P.dma(POOL, wub[wsl][:], usrc.rearrange("(kc p) n -> p kc n", p=128), writes=[Bwt[wsl]])
                P.dma(POOL, wdb[wsl][:], dsrc.rearrange("(j p) n -> p j n", p=128), writes=[Bwt[wsl]])
                state[(e, grp)] = wsl

            def gateup(idx):
                nonlocal si
                e, grp, sbk = items[idx]
                if (e, grp) not in state:
                    load_w(e, grp)
                wsl = state[(e, grp)]
                asl = idx % 2
                hTr = [BhT[sbk * 4 + i] for i in range(4)]
                for j in range(gch):
                    for (wbuf, half) in ((wgb[wsl], 0), (wub[wsl], 1)):
                        for kc in range(8):
                            P.op(PE, lambda kc=kc, j=j, wbuf=wbuf, half=half: nc.tensor.matmul(
                                PS[j % 2][:, half * 512:(half + 1) * 512],
                                lhsT=wbuf[:, kc, j * 128:(j + 1) * 128], rhs=hT[:, kc, sbk * 512:(sbk + 1) * 512],
                                start=(kc == 0), stop=(kc == 7)),
                                reads=hTr + [Bwt[wsl]], writes=[PSB[j % 2][half]], inc=(kc == 7))
                    psg = PS[j % 2]
                    ssl = si % 2
                    si += 1
                    P.op(ACT, lambda psg=psg, ssl=ssl: nc.scalar.activation(out=sil[ssl][:], in_=psg[:, 0:512], func=AF.Silu),
                         reads=[PSB[j % 2][0]], writes=[Bsil[ssl]])
                    P.op(DVE, lambda psg=psg, ssl=ssl, j=j, asl=asl: nc.vector.tensor_tensor(out=act[asl][:, j, :], in0=psg[:, 512:1024], in1=sil[ssl][:], op=ALU.mult),
                         reads=[PSB[j % 2][1], Bsil[ssl]], writes=[Bact[asl]])

            def down(idx):
                e, grp, sbk = items[idx]
                wsl = state[(e, grp)]
                asl = idx % 2
                firstacc = (e == elist[0] and grp == 0)
                for t4 in range(4):
                    t = sbk * 4 + t4
                    pso = 2 + (t4 % 2)
                    for half in range(2):
                        for j in range(gch):
                            P.op(PE, lambda j=j, half=half, t4=t4, pso=pso, asl=asl: nc.tensor.matmul(
                                PS[pso][:, half * 512:(half + 1) * 512], lhsT=act[asl][:, j, t4 * 128:(t4 + 1) * 128],
                                rhs=wdb[wsl][:, j, half * 512:(half + 1) * 512], start=(j == 0), stop=(j == gch - 1)),
                                reads=[Bact[asl], Bwt[wsl]], writes=[PSB[pso][half]], inc=(j == gch - 1))
                    rd = [PSB[pso][0], PSB[pso][1]]
                    if n_exp == 1:
                        if firstacc:
                            P.op(DVE, lambda t=t, pso=pso: nc.vector.tensor_copy(out=acc[:, t, :], in_=PS[pso][:]), reads=rd, writes=[Bacc[t]])
                        else:
                            P.op(DVE, lambda t=t, pso=pso: nc.vector.tensor_tensor(out=acc[:, t, :], in0=PS[pso][:], in1=acc[:, t, :], op=ALU.add),
                                 reads=rd, writes=[Bacc[t]])
                    else:
                        if firstacc:
                            P.op(DVE, lambda t=t, pso=pso, e=e: nc.vector.tensor_scalar(out=acc[:, t, :], in0=PS[pso][:], scalar1=gates[:, t, e:e + 1],
                                                                                     scalar2=None, op0=ALU.mult), reads=rd + [Bgates[t]], writes=[Bacc[t]])
                        else:
                            P.op(DVE, lambda t=t, pso=pso, e=e: nc.vector.scalar_tensor_tensor(out=acc[:, t, :], in0=PS[pso][:], scalar=gates[:, t, e:e + 1],
                                                                                            in1=acc[:, t, :], op0=ALU.mult, op1=ALU.add),
                                 reads=rd + [Bgates[t]], writes=[Bacc[t]])
            gateup(0)
            nxt_t = 0
            overlap_next = False
            for idx in range(len(items)):
                if idx + 1 < len(items):
                    gateup(idx + 1)
                down(idx)
                if overlap_next and idx >= 2 and idx % 2 == 0 and nxt_t < NTB:
                    f_pa(nxt_t, tb + 1)
                    f_pb(nxt_t, tb + 1)
                    nxt_t += 1
            if overlap_next:
                while nxt_t < NTB:
                    f_pa(nxt_t, tb + 1)
                    f_pb(nxt_t, tb + 1)
                    nxt_t += 1
            for t in range(NTB):
                r0 = tb * TB + t * 128
                epilogue(acc[:, t, :], [Bacc[t]], x_in[r0:r0 + 128, :], G, x_out[r0:r0 + 128, :], ewks[t % 2])
        P.barrier()
        st.close()

    def phase_moe(x_in, x_out):
        I32 = mybir.dt.int32
        A, S = AS["oA_ffn"], AS["oS_ffn"]
        G = Gt["o_ffn"]
        CAP = 4608
        NSUB = 9
        hsorted = dscr("hsorted", [NEXP * CAP, D], BF16)
        ysorted = dscr("ysorted", [NEXP * CAP, D], F32)
        flags_d = dscr("flags_d", [1, NEXP * 16], I32, out=dbg)
        st0 = ExitStack()

        def sb0(name, shape, dt=F32):
            return st0.enter_context(nc.sbuf_tensor("m_" + name, list(shape), dt))
        dest_i = sb0("dest_i", [128, NT_OWN, 2], I32)
        wts = sb0("wts", [128, NT_OWN, 2])
        Bdest = [Buf("dest") for _ in range(NT_OWN)]
        st = ExitStack()

        def sbt(name, shape, dt=F32):
            return st.enter_context(nc.sbuf_tensor("mr_" + name, list(shape), dt))
        wks = mk_wk(sbt, "mr", 2, with_x=True, with_xn=False)
        wr = sbt("wr", [128, 8, NEXP])
        brB = sbt("brB", [128, NEXP])
        Bwr = Buf("wr")
        P.dma(SP, wr[:], W["o_w_router"].rearrange("(kc p) n -> p kc n", p=128), writes=[Bwr])
        P.dma(SP, brB[:], W["o_b_router"][:].partition_broadcast(128), writes=[Bwr])
        xn32 = [sbt(f"xn32{i}", [128, D]) for i in range(2)]
        xnb = [sbt(f"xnb{i}", [128, D], BF16) for i in range(2)]
        Bx32 = [Buf("xn32") for _ in range(2)]
        Bxnb = [Buf("xnb") for _ in range(2)]
        h32 = sbt("h32", [128, 8, 128])
        Bh32 = Buf("h32")
        rt = {k: sbt("rt_" + k, [128, 8]) for k in ("lg", "m1", "l2", "m2", "pos", "t1", "t2", "macc", "ebase")}
        rs = {k: sbt("rs_" + k, [128, 1]) for k in ("mx1", "mx2", "d", "e", "den", "d1", "d2")}
        maskb = sbt("maskb", [128, 8], BF16)
        maccb = sbt("maccb", [128, 8], BF16)
        ustrict = sbt("ustrict", [128, 128], BF16)
        ones_b = sbt("ones_b", [128, 128], BF16)
        fl = sbt("fl", [128, NSUB, NEXP])
        fl_i = sbt("fl_i", [128, NEXP, 16], I32)
        nsub_f = sbt("nsub_f", [128, NEXP])
        Brt = Buf("rt")
        Bmacc = Buf("macc")
        Bc2 = Buf("c2")
        V = nc.vector
        ustr_f = sbt("ustr_f", [128, 128])
        P.op(DVE, lambda: V.memset(ones_b[:], 1.0), writes=[Bc2])
        P.op(DVE, lambda: V.memset(rt["macc"][:], 0.0), writes=[Bmacc])
        P.op(DVE, lambda: V.memset(maccb[:], 0.0), writes=[Bmacc])
        for e in range(NEXP):
            P.op(DVE, lambda e=e: V.memset(rt["ebase"][:, e:e + 1], float(e * CAP)), writes=[Bc2])
        P.op(DVE, lambda: V.tensor_tensor_scan(out=ustr_f[:], data0=ones_f_full[:], data1=ident_f[:], initial=0.0, op0=ALU.mult, op1=ALU.add),
             reads=[B_consts], writes=[Bc2])
        P.op(DVE, lambda: V.tensor_tensor(out=ustrict[:], in0=ustr_f[:], in1=ident_f[:], op=ALU.subtract), reads=[Bc2, B_consts], writes=[Bc2])

        def r_pa(t):
            wk = wks[t % 2]
            r0 = t * 128
            P.dma(SP, wk["x"][:], x_in[r0:r0 + 128, :], writes=[wk["Bx"]])
            xs = xn32[t % 2]
            P.op(ACT, lambda: nc.scalar.activation(out=wk["junk"][:], in_=wk["x"][:], func=AF.Square, accum_out=wk["ss"][:]),
                 reads=[wk["Bx"]], writes=[wk["Bs"]])
            rstd_from_ss(wk["ss"][:], D, wk["sd"][:], wk["rstd"][:], [wk["Bs"]], [wk["Bs"]])
            P.op(DVE, lambda: V.tensor_scalar(out=xs[:], in0=wk["x"][:], scalar1=wk["rstd"][:], scalar2=None, op0=ALU.mult),
                 reads=[wk["Bx"], wk["Bs"]], writes=[Bx32[t % 2]])
            P.op(ACT, lambda: nc.scalar.activation(out=xnb[t % 2][:], in_=xs[:], func=AF.Copy), reads=[Bx32[t % 2]], writes=[Bxnb[t % 2]])

        def r_pb(t):
            xs = xn32[t % 2]
            pp = 3 if t % 2 == 0 else 1
            for c in range(8):
                half = c // 4
                P.op(PE, lambda c=c: nc.tensor.transpose(out=PS[pp][:, c * 128:(c + 1) * 128], in_=xs[:, c * 128:(c + 1) * 128], identity=ident_f[:]),
                     reads=[Bx32[t % 2], B_consts], writes=[PSB[pp][half]], inc=(c % 4 == 3))
            for c in range(8):
                half = c // 4
                P.op(DVE, lambda c=c: V.tensor_scalar(out=h32[:, c, :], in0=PS[pp][:, c * 128:(c + 1) * 128], scalar1=A[:, c, 0:1],
                                                      scalar2=S[:, c, 0:1], op0=ALU.mult, op1=ALU.add),
                     reads=[PSB[pp][half], B_mod], writes=[Bh32])
            for kc in range(8):
                P.op(PE, lambda kc=kc: nc.tensor.matmul(PS[2][:, 0:NEXP], lhsT=h32[:, kc, :], rhs=wr[:, kc, :], start=(kc == 0), stop=(kc == 7)),
                     reads=[Bh32, Bwr], writes=[PSB[2][0]], inc=(kc == 7))
            P.op(DVE, lambda: V.tensor_tensor(out=rt["lg"][:], in0=PS[2][:, 0:NEXP], in1=brB[:], op=ALU.add), reads=[PSB[2][0], Bwr], writes=[Brt])
            P.op(DVE, lambda: V.tensor_reduce(out=rs["mx1"][:], in_=rt["lg"][:], axis=AX.X, op=ALU.max), reads=[Brt], writes=[Brt])
            P.op(DVE, lambda: V.tensor_scalar(out=rt["m1"][:], in0=rt["lg"][:], scalar1=rs["mx1"][:], scalar2=None, op0=ALU.is_equal), reads=[Brt], writes=[Brt])
            P.op(DVE, lambda: V.scalar_tensor_tensor(out=rt["l2"][:], in0=rt["m1"][:], scalar=-1e30, in1=rt["lg"][:], op0=ALU.mult, op1=ALU.add), reads=[Brt], writes=[Brt])
            P.op(DVE, lambda: V.tensor_reduce(out=rs["mx2"][:], in_=rt["l2"][:], axis=AX.X, op=ALU.max), reads=[Brt], writes=[Brt])
            P.op(DVE, lambda: V.tensor_scalar(out=rt["m2"][:], in0=rt["l2"][:], scalar1=rs["mx2"][:], scalar2=None, op0=ALU.is_equal), reads=[Brt], writes=[Brt])
            P.op(DVE, lambda: V.tensor_tensor(out=rs["d"][:], in0=rs["mx2"][:], in1=rs["mx1"][:], op=ALU.subtract), reads=[Brt], writes=[Brt])
            P.op(ACT, lambda: nc.scalar.activation(out=rs["e"][:], in_=rs["d"][:], func=AF.Exp), reads=[Brt], writes=[Brt])
            P.op(DVE, lambda: V.tensor_scalar(out=rs["den"][:], in0=rs["e"][:], scalar1=1.0, scalar2=None, op0=ALU.add), reads=[Brt], writes=[Brt])
            P.op(DVE, lambda: V.reciprocal(out=wts[:, t, 0:1], in_=rs["den"][:]), reads=[Brt], writes=[Bdest[t]])
            P.op(DVE, lambda: V.tensor_tensor(out=wts[:, t, 1:2], in0=rs["e"][:], in1=wts[:, t, 0:1], op=ALU.mult), reads=[Brt, Bdest[t]], writes=[Bdest[t]])
            P.op(DVE, lambda: V.tensor_tensor(out=maskb[:], in0=rt["m1"][:], in1=rt["m2"][:], op=ALU.add), reads=[Brt], writes=[Brt])
            P.op(PE, lambda: nc.tensor.matmul(PS[2][:, 512:512 + NEXP], lhsT=ustrict[:], rhs=maskb[:], start=True, stop=False),
                 reads=[Brt, Bc2], writes=[PSB[2][1]], inc=False)
            P.op(PE, lambda: nc.tensor.matmul(PS[2][:, 512:512 + NEXP], lhsT=ones_b[:], rhs=maccb[:], start=False, stop=True),
                 reads=[Bmacc, Bc2], writes=[PSB[2][1]], inc=True)
            P.op(DVE, lambda: V.tensor_tensor(out=rt["pos"][:], in0=PS[2][:, 512:512 + NEXP], in1=rt["ebase"][:], op=ALU.add), reads=[PSB[2][1], Bc2], writes=[Brt])
            P.op(DVE, lambda: V.tensor_tensor(out=rt["t1"][:], in0=rt["pos"][:], in1=rt["m1"][:], op=ALU.mult), reads=[Brt], writes=[Brt])
            P.op(DVE, lambda: V.tensor_reduce(out=rs["d1"][:], in_=rt["t1"][:], axis=AX.X, op=ALU.add), reads=[Brt], writes=[Brt])
            P.op(DVE, lambda: V.tensor_tensor(out=rt["t2"][:], in0=rt["pos"][:], in1=rt["m2"][:], op=ALU.mult), reads=[Brt], writes=[Brt])
            P.op(DVE, lambda: V.tensor_reduce(out=rs["d2"][:], in_=rt["t2"][:], axis=AX.X, op=ALU.add), reads=[Brt], writes=[Brt])
            P.op(DVE, lambda: V.tensor_copy(out=dest_i[:, t, 0:1], in_=rs["d1"][:]), reads=[Brt], writes=[Bdest[t]])
            P.op(DVE, lambda: V.tensor_copy(out=dest_i[:, t, 1:2], in_=rs["d2"][:]), reads=[Brt], writes=[Bdest[t]])
            P.op(DVE, lambda: V.tensor_tensor(out=rt["macc"][:], in0=rt["macc"][:], in1=maskb[:], op=ALU.add), reads=[Brt, Bmacc], writes=[Bmacc])
            P.op(DVE, lambda: V.tensor_copy(out=maccb[:], in_=rt["macc"][:]), reads=[Bmacc], writes=[Bmacc])
            for k in range(2):
                P.dma_ind(hsorted[:, :], bass.IndirectOffsetOnAxis(ap=dest_i[:, t, k:k + 1], axis=0), xnb[t % 2][:, :], None,
                          reads=[Bxnb[t % 2], Bdest[t]])
        r_pa(0)
        for t in range(NT_OWN):
            if t + 1 < NT_OWN:
                r_pa(t + 1)
            r_pb(t)
        P.op(PE, lambda: nc.tensor.matmul(PS[2][:, 0:NEXP], lhsT=ones_b[:], rhs=maccb[:], start=True, stop=True), reads=[Bmacc, Bc2], writes=[PSB[2][0]], inc=True)
        Bfl = Buf("fl")
        for j in range(NSUB):
            P.op(DVE, lambda j=j: V.tensor_scalar(out=fl[:, j, :], in0=PS[2][:, 0:NEXP], scalar1=float(j * 512) + 0.5, scalar2=None, op0=ALU.is_gt),
                 reads=[PSB[2][0]], writes=[Bfl])
        P.op(DVE, lambda: V.memset(fl_i[:], 0), writes=[Bfl])
        P.op(DVE, lambda: V.tensor_reduce(out=nsub_f[:], in_=fl[:].rearrange("p j e -> p e j"), axis=AX.X, op=ALU.add), reads=[Bfl], writes=[Bfl])
        P.op(DVE, lambda: V.tensor_copy(out=fl_i[:, :, 0:1], in_=nsub_f[:].unsqueeze(2)), reads=[Bfl], writes=[Bfl])
        tok_flags = P.dma(SP, flags_d[:, :], fl_i[0:1, :, :].rearrange("p a b -> p (a b)"), reads=[Bfl])
        if dbg:
            ddst = dscr("dbg_dest", [128, NT_OWN, 2], I32, out=True)
            P.dma(SP, ddst[:], dest_i[:], reads=Bdest)
            dw = dscr("dbg_wts", [128, NT_OWN, 2], out=True)
            P.dma(SP, dw[:], wts[:], reads=Bdest)
        P.barrier()
        st.close()

        st = ExitStack()

        def sbt(name, shape, dt=F32):
            return st.enter_context(nc.sbuf_tensor("me_" + name, list(shape), dt))
        gch, gw, ngrp = 4, 512, E_FF // 512
        wgb = [sbt(f"wg{i}", [128, 8, gw], BF16) for i in range(2)]
        wub = [sbt(f"wu{i}", [128, 8, gw], BF16) for i in range(2)]
        wdb = [sbt(f"wd{i}", [128, gch, D], BF16) for i in range(2)]
        Bwt = [Buf("w") for _ in range(2)]
        act = [sbt(f"act{i}", [128, gch, 512], BF16) for i in range(2)]
        Bact = [[Buf("act") for _ in range(4)] for _ in range(2)]
        sil = [sbt(f"sil{i}", [128, 512]) for i in range(2)]
        Bsil = [Buf("sil") for _ in range(2)]
        hTc = sbt("hTc", [128, 8, 1536], BF16)
        BhTc = [Buf("hTc") for _ in range(12)]
        acc = sbt("acc", [128, 12, D])
        Bacc = [Buf("acc") for _ in range(12)]
        xr = [sbt(f"xr{i}", [128, D], BF16) for i in range(2)]
        Bxr = [Buf("xr") for _ in range(2)]
        all_eng = [mybir.EngineType.PE, mybir.EngineType.Activation, mybir.EngineType.DVE, mybir.EngineType.Pool, mybir.EngineType.SP]
        Rn = nc.alloc_registers("nsub", all_eng)
        for E in (PE, ACT, DVE, POOL, SP):
            E.wait(tok_flags)
        wi = 0
        si = 0
        xi = 0
        for e in range(NEXP):
            for reg in Rn:
                nc.reg_load(reg, flags_d[0:1, e * 16:e * 16 + 1])
            for c in range(3):
                for s_ in range(3):
                    j = 3 * c + s_

                    def prep(j=j, s_=s_):
                        nonlocal xi
                        for t4 in range(4):
                            k = xi % 2
                            xi += 1
                            row0 = e * CAP + j * 512 + t4 * 128
                            P.dma(SP, xr[k][:], hsorted[row0:row0 + 128, :], writes=[Bxr[k]])
                            ti = s_ * 4 + t4
                            wkx = {"xn": xr[k], "Bxn": Bxr[k]}
                            prenorm_b(A, S, 0, hTc[:, :, ti * 128:(ti + 1) * 128], BhTc[ti], wkx, (3, t4 % 2))
                    P.predicated(Rn, j, prep)
                for grp in range(ngrp):
                    wsl = wi % 2
                    wi += 1
                    f0 = grp * gw

                    def loadw(wsl=wsl, f0=f0):
                        P.dma(POOL, wgb[wsl][:], W["o_w_gate"][e, :, f0:f0 + gw].rearrange("(kc p) n -> p kc n", p=128), writes=[Bwt[wsl]])
                        P.dma(POOL, wub[wsl][:], W["o_w_up"][e, :, f0:f0 + gw].rearrange("(kc p) n -> p kc n", p=128), writes=[Bwt[wsl]])
                        P.dma(POOL, wdb[wsl][:], W["o_w_down"][e, f0:f0 + gw, :].rearrange("(j p) n -> p j n", p=128), writes=[Bwt[wsl]])
                    P.predicated(Rn, 3 * c, loadw)
                    for s_ in range(3):
                        j = 3 * c + s_

                        def body(s_=s_, wsl=wsl, grp=grp):
                            nonlocal si
                            asl = si % 2
                            hTr = [BhTc[s_ * 4 + i] for i in range(4)]
                            for jj in range(gch):
                                for (wbuf, half) in ((wgb[wsl], 0), (wub[wsl], 1)):
                                    for kc in range(8):
                                        P.op(PE, lambda kc=kc, jj=jj, wbuf=wbuf, half=half: nc.tensor.matmul(
                                            PS[jj % 2][:, half * 512:(half + 1) * 512],
                                            lhsT=wbuf[:, kc, jj * 128:(jj + 1) * 128], rhs=hTc[:, kc, s_ * 512:(s_ + 1) * 512],
                                            start=(kc == 0), stop=(kc == 7)),
                                            reads=hTr + [Bwt[wsl]], writes=[PSB[jj % 2][half]], inc=(kc == 7))
                                psg = PS[jj % 2]
                                ssl = si % 2
                                si += 1
                                P.op(ACT, lambda psg=psg, ssl=ssl: nc.scalar.activation(out=sil[ssl][:], in_=psg[:, 0:512], func=AF.Silu),
                                     reads=[PSB[jj % 2][0]], writes=[Bsil[ssl]])
                                P.op(DVE, lambda psg=psg, ssl=ssl, jj=jj: nc.vector.tensor_tensor(out=act[asl][:, jj, :], in0=psg[:, 512:1024], in1=sil[ssl][:], op=ALU.mult),
                                     reads=[PSB[jj % 2][1], Bsil[ssl]], writes=[Bact[asl][jj]])
                            for tp in range(2):
                                for jj in range(gch):
                                    for t4 in (2 * tp, 2 * tp + 1):
                                        pso = 2 + (t4 % 2)
                                        for half in range(2):
                                            P.op(PE, lambda jj=jj, half=half, t4=t4, pso=pso: nc.tensor.matmul(
                                                PS[pso][:, half * 512:(half + 1) * 512], lhsT=act[asl][:, jj, t4 * 128:(t4 + 1) * 128],
                                                rhs=wdb[wsl][:, jj, half * 512:(half + 1) * 512], start=(jj == 0), stop=(jj == gch - 1)),
                                                reads=[Bact[asl][jj], Bwt[wsl]], writes=[PSB[pso][half]], inc=(jj == gch - 1))
                                for t4 in (2 * tp, 2 * tp + 1):
                                    ti = s_ * 4 + t4
                                    pso = 2 + (t4 % 2)
                                    rd = [PSB[pso][0], PSB[pso][1]]
                                    if grp == 0:
                                        P.op(DVE, lambda ti=ti, pso=pso: nc.vector.tensor_copy(out=acc[:, ti, :], in_=PS[pso][:]), reads=rd, writes=[Bacc[ti]])
                                    else:
                                        P.op(DVE, lambda ti=ti, pso=pso: nc.vector.tensor_tensor(out=acc[:, ti, :], in0=PS[pso][:], in1=acc[:, ti, :], op=ALU.add),
                                             reads=rd, writes=[Bacc[ti]])
                                    if grp == ngrp - 1:
                                        row0 = e * CAP + (3 * c + s_) * 512 + t4 * 128
                                        P.dma(SP, ysorted[row0:row0 + 128, :], acc[:, ti, :], reads=[Bacc[ti]])
                        P.predicated(Rn, j, body)
        P.barrier()
        st.close()

        st = ExitStack()

        def sbt(name, shape, dt=F32):
            return st.enter_context(nc.sbuf_tensor("mc_" + name, list(shape), dt))
        wks = mk_wk(sbt, "mc", 2, with_x=True, with_xn=False)
        NY = 4
        y1 = [sbt(f"y1{i}", [128, D]) for i in range(NY)]
        y2 = [sbt(f"y2{i}", [128, D]) for i in range(NY)]
        By = [Buf("y") for _ in range(NY)]

        def gather(t):
            k = t % NY
            P.dma_ind(y1[k][:, :], None, ysorted[:, :], bass.IndirectOffsetOnAxis(ap=dest_i[:, t, 0:1], axis=0), reads=[Bdest[t]], writes=[By[k]])
            P.dma_ind(y2[k][:, :], None, ysorted[:, :], bass.IndirectOffsetOnAxis(ap=dest_i[:, t, 1:2], axis=0), reads=[Bdest[t]], writes=[By[k]])
        gather(0)
        gather(1)
        for t in range(NT_OWN):
            k = t % NY
            if t + 2 < NT_OWN:
                gather(t + 2)
            P.op(DVE, lambda: nc.vector.tensor_scalar(out=y1[k][:], in0=y1[k][:], scalar1=wts[:, t, 0:1], scalar2=None, op0=ALU.mult), reads=[By[k], Bdest[t]], writes=[By[k]])
            P.op(DVE, lambda: nc.vector.scalar_tensor_tensor(out=y1[k][:], in0=y2[k][:], scalar=wts[:, t, 1:2], in1=y1[k][:], op0=ALU.mult, op1=ALU.add),
                 reads=[By[k], Bdest[t]], writes=[By[k]])
            r0 = t * 128
            epilogue(y1[k][:], [By[k]], x_in[r0:r0 + 128, :], G, x_out[r0:r0 + 128, :], wks[t % 2])
        P.barrier()
        st.close()
        st0.close()

    def phaseC():
        st = ExitStack()

        def sbt(name, shape, dt=F32):
            return st.enter_context(nc.sbuf_tensor(name, list(shape), dt))
        A, S = AS["oA_mix"], AS["oS_mix"]
        G = Gt["o_mix"]
        wi_ = sbt("c_win", [128, 8, 2048], BF16)
        wo_ = sbt("c_wout", [128, 8, D], BF16)
        Bw = Buf("c_w")
        P.dma(POOL, wi_[:, :, 0:1024], W["o_w_in"][:, 0:1024].rearrange("(kc p) n -> p kc n", p=128), writes=[Bw])
        P.dma(POOL, wi_[:, :, 1024:2048], W["o_w_in"][:, 1024:2048].rearrange("(kc p) n -> p kc n", p=128), writes=[Bw])
        P.dma(POOL, wo_[:], W["o_w_out"].rearrange("(kc p) n -> p kc n", p=128), writes=[Bw])
        ws_b = sbt("c_wsb", [128, 8, 128], BF16)
        WsT = sbt("c_WsT", [128, 8, 128], BF16)
        ones_b = sbt("c_ones", [128, 128], BF16)
        Rg = sbt("c_Rg", [128, 8, 128])
        bsB = sbt("c_bsB", [128, 8, 128])
        gvT = sbt("c_gvT", [128, 8])
        bvT = sbt("c_bvT", [128, 8])
        Bs_ = Buf("c_s")
        P.dma(POOL, ws_b[:], W["o_w_s"].rearrange("g p q -> p g q"), writes=[Bs_])
        P.dma(SP, gvT[:], W["o_g_vT"][:], writes=[Bs_])
        P.dma(SP, bvT[:], W["o_b_vT"][:], writes=[Bs_])
        for g in range(8):
            P.dma(SP, bsB[:, g, :], W["o_b_s"][g:g + 1, :].partition_broadcast(128), writes=[Bs_])
        P.op(DVE, lambda: nc.vector.memset(ones_b[:], 1.0), writes=[Bs_])
        pv = psb16(3, 0)
        for g in range(8):
            P.op(PE, lambda g=g: nc.tensor.transpose(out=pv[:, g * 128:(g + 1) * 128], in_=ws_b[:, g, :], identity=ident_b[:]),
                 reads=[Bs_, B_consts], writes=[PSB[3][0]], inc=(g == 7))
        P.op(DVE, lambda: nc.vector.tensor_copy(out=WsT[:], in_=pv[:, 0:1024].rearrange("p (g q) -> p g q", g=8)), reads=[PSB[3][0]], writes=[Bs_])
        for g in range(8):
            P.op(PE, lambda g=g: nc.tensor.matmul(PS[3][:, 512 + (g % 4) * 128:512 + (g % 4 + 1) * 128], lhsT=ones_b[:], rhs=WsT[:, g, :], start=True, stop=True),
                 reads=[Bs_], writes=[PSB[3][1]], inc=True)
            P.op(DVE, lambda g=g: nc.vector.scalar_tensor_tensor(out=Rg[:, g, :], in0=PS[3][:, 512 + (g % 4) * 128:512 + (g % 4 + 1) * 128],
                                                                  scalar=bvT[:, g:g + 1], in1=bsB[:, g, :], op0=ALU.mult, op1=ALU.add),
                 reads=[PSB[3][1], Bs_], writes=[Bs_])
        hTb = [sbt(f"c_hT{i}", [128, 8, 512], BF16) for i in range(2)]
        BhT = [Buf("c_hT") for _ in range(2)]
        uT = sbt("c_uT", [128, 8, 512], BF16)
        Bu = Buf("c_uT")
        usT = sbt("c_usT", [128, 8, 512], BF16)
        Bus = Buf("c_usT")
        sT = sbt("c_sT", [128, 512])
        BsT = Buf("c_sT")
        vraw = [sbt(f"c_vraw{i}", [128, D]) for i in range(2)]
        vn = [sbt(f"c_vn{i}", [128, D], BF16) for i in range(4)]
        Bvr = [Buf("c_vraw") for _ in range(2)]
        Bvn = [Buf("c_vn") for _ in range(4)]
        stats = [sbt(f"c_stats{i}", [128, 2, 6]) for i in range(2)]
        mv = [sbt(f"c_mv{i}", [128, 2]) for i in range(2)]
        vsd = [sbt(f"c_vsd{i}", [128, 1]) for i in range(2)]
        vrs = [sbt(f"c_vrs{i}", [128, 1]) for i in range(2)]
        wks = mk_wk(sbt, "c", 2, with_x=True)
        ewks = mk_wk(sbt, "ce", 2, with_x=True, with_xn=False)
        gi = 0

        def gelu_from_psum(ps_ap, Bps, out_ap, Bout):
            P.op(ACT, lambda: nc.scalar.activation(out=out_ap, in_=ps_ap, func=AF.Gelu_apprx_tanh), reads=Bps, writes=[Bout])

        def c_prenorm(blk):
            hb, Bhb = hTb[blk % 2], BhT[blk % 2]

            def c_pa(t):
                wk = wks[t % 2]
                r0 = blk * 512 + t * 128
                P.dma(SP, wk["x"][:], xB[r0:r0 + 128, :], writes=[wk["Bx"]])
                prenorm_a(wk["x"][:], wk["Bx"], wk)
            c_pa(0)
            for t in range(4):
                if t + 1 < 4:
                    c_pa(t + 1)
                prenorm_b(A, S, 0, hb[:, :, t * 128:(t + 1) * 128], Bhb, wks[t % 2], (3, t % 2))

        c_prenorm(0)
        for blk in range(8):
            hb, Bhb = hTb[blk % 2], BhT[blk % 2]
            for f in range(8):
                pi_ = f % 2
                for kc in range(8):
                    P.op(PE, lambda kc=kc, f=f, pi_=pi_: nc.tensor.matmul(PS[pi_][:, 0:512], lhsT=wi_[:, kc, f * 128:(f + 1) * 128], rhs=hb[:, kc, :],
                                                                            start=(kc == 0), stop=(kc == 7)),
                         reads=[Bhb, Bw], writes=[PSB[pi_][0]], inc=(kc == 7))
                gelu_from_psum(PS[pi_][:, 0:512], [PSB[pi_][0]], uT[:, f, :], Bu)
            for t in range(4):
                k2 = t % 2
                for half in range(2):
                    for kc in range(8):
                        P.op(PE, lambda kc=kc, half=half, t=t: nc.tensor.matmul(PS[2][:, half * 512:(half + 1) * 512], lhsT=hb[:, kc, t * 128:(t + 1) * 128],
                                                                                 rhs=wi_[:, kc, 1024 + half * 512:1024 + (half + 1) * 512], start=(kc == 0), stop=(kc == 7)),
                             reads=[Bhb, Bw], writes=[PSB[2][half]], inc=(kc == 7))
                    gelu_from_psum(PS[2][:, half * 512:(half + 1) * 512], [PSB[2][half]], vraw[k2][:, half * 512:(half + 1) * 512], Bvr[k2])
                for half in range(2):
                    P.op(DVE, lambda half=half, k2=k2: nc.vector.bn_stats(out=stats[k2][:, half, :], in_=vraw[k2][:, half * 512:(half + 1) * 512]),
                         reads=[Bvr[k2]], writes=[Bvr[k2]])
                P.op(DVE, lambda k2=k2: nc.vector.bn_aggr(out=mv[k2][:], in_=stats[k2][:].rearrange("p a s -> p (a s)")), reads=[Bvr[k2]], writes=[Bvr[k2]])
                P.op(ACT, lambda k2=k2: nc.scalar.activation(out=vsd[k2][:], in_=mv[k2][:, 1:2], func=AF.Sqrt, bias=eps_t[:], scale=1.0),
                     reads=[Bvr[k2], B_consts], writes=[Bvr[k2]])
                P.op(DVE, lambda k2=k2: nc.vector.reciprocal(out=vrs[k2][:], in_=vsd[k2][:]), reads=[Bvr[k2]], writes=[Bvr[k2]])
                P.op(DVE, lambda k2=k2, t=t: nc.vector.tensor_scalar(out=vn[t][:], in0=vraw[k2][:], scalar1=mv[k2][:, 0:1], scalar2=vrs[k2][:],
                                                                   op0=ALU.subtract, op1=ALU.mult), reads=[Bvr[k2]], writes=[Bvn[t]])
            for g in range(8):
                pi_ = g % 2
                for t in range(4):
                    P.op(PE, lambda g=g, t=t, pi_=pi_: nc.tensor.matmul(PS[pi_][:, 512 + t * 128:512 + (t + 1) * 128], lhsT=vn[t][:, g * 128:(g + 1) * 128],
                                                                          rhs=WsT[:, g, :], start=True, stop=True),
                         reads=[Bvn[t], Bs_], writes=[PSB[pi_][1]], inc=(t == 3))
                P.op(DVE, lambda g=g, pi_=pi_: nc.vector.scalar_tensor_tensor(
                    out=sT[:].rearrange("p (c q) -> p c q", c=4), in0=PS[pi_][:, 512:1024].rearrange("p (c q) -> p c q", c=4), scalar=gvT[:, g:g + 1],
                    in1=Rg[:, g, :].unsqueeze(1).to_broadcast([128, 4, 128]), op0=ALU.mult, op1=ALU.add),
                    reads=[PSB[pi_][1], Bs_], writes=[BsT])
                P.op(DVE, lambda g=g: nc.vector.tensor_tensor(out=usT[:, g, :], in0=uT[:, g, :], in1=sT[:], op=ALU.mult), reads=[Bu, BsT], writes=[Bus])
            if blk + 1 < 8:
                c_prenorm(blk + 1)
            for t in range(4):
                wk = ewks[t % 2]
                py = 3 if t % 2 == 0 else 2
                for half in range(2):
                    for c in range(8):
                        P.op(PE, lambda c=c, half=half, t=t: nc.tensor.matmul(PS[py][:, half * 512:(half + 1) * 512], lhsT=usT[:, c, t * 128:(t + 1) * 128],
                                                                               rhs=wo_[:, c, half * 512:(half + 1) * 512], start=(c == 0), stop=(c == 7)),
                             reads=[Bus, Bw], writes=[PSB[py][half]], inc=(c == 7))
                r0 = blk * 512 + t * 128
                epilogue(PS[py][:], [PSB[py][0], PSB[py][1]], xB[r0:r0 + 128, :], G, xC[r0:r0 + 128, :], wk)
        P.barrier()
        st.close()

    if "0" in phases:
        phase0()
    if "A" in phases:
        phaseA()
    if "B" in phases:
        phase_ffn("e", xA, xB, 1, D_FF, 2, W["e_w_gate"], W["e_w_up"], W["e_w_down"], False)
    if "C" in phases:
        phaseC()
    if "D" in phases:
        if cfg.get("dense_moe", False):
            phase_ffn("o", xown if cfg.get("d_in") == "xown" else xC, out_d, NEXP, E_FF, 4, W["o_w_gate"], W["o_w_up"], W["o_w_down"], True)
        else:
            phase_moe(xown if cfg.get("d_in") == "xown" else xC, out_d)
    if dbg:
        dump = dscr("dbg_mod", [128, 8 * 2 * 8 + 4 * 0], out=True)
        i = 0
        for L in ("e", "o"):
            for k in ("A_mix", "S_mix", "A_ffn", "S_ffn"):
                P.dma(SP, dump[:, i * 16:(i + 1) * 16], AS[L + k][:].rearrange("p c r -> p (c r)"), reads=[B_mod])
                i += 1
        dumpG = dscr("dbg_G", [4, 128, D], out=True)
        for i, k in enumerate(("e_mix", "e_ffn", "o_mix", "o_ffn")):
            P.dma(SP, dumpG[i], Gt[k][:], reads=[B_mod])
    P.drain_all(SP)
    P.drain_all(POOL)
    es.close()
    return nc


def _rope_tables():
    rows = SEQ // 64
    r, col = np.meshgrid(np.arange(rows, dtype=np.float32), np.arange(64, dtype=np.float32), indexing="ij")
    inv = (1.0 / (10000.0 ** (np.arange(0, 32, 2, dtype=np.float32) / 32.0))).astype(np.float32)
    ang = np.concatenate([r.reshape(-1, 1) * inv, col.reshape(-1, 1) * inv], axis=-1).astype(np.float32)
    return np.concatenate([np.cos(ang), np.sin(ang)], axis=-1).astype(np.float32)


def _fm(v):
    return np.ascontiguousarray(np.asarray(v, np.float32).reshape(8, 128).T)


def _core_inputs(inp, r, rope_tab):
    b, h = r // 2, r % 2
    f = lambda a: np.ascontiguousarray(np.asarray(a, np.float32))
    x = inp["x"]
    own = f(x[b, h * NOWN:(h + 1) * NOWN])
    oth = f(x[b, (1 - h) * NOWN:(2 - h) * NOWN])
    cv = np.stack([np.asarray(inp["c"][b], np.float32), np.asarray(inp["c_ctx"], np.float32)], axis=-1)
    cvecT = np.ascontiguousarray(cv.reshape(8, 128, 2).transpose(1, 0, 2).reshape(128, 16))
    rope_c = np.concatenate([rope_tab[h * NOWN:(h + 1) * NOWN], rope_tab[(1 - h) * NOWN:(2 - h) * NOWN]], axis=0)
    hm = np.zeros((128, 2), np.float32)
    hm[:, 0] = float(h)
    hm[:, 1] = float(1 - h)
    m = {"xown": own, "xoth": oth, "ctx": f(inp["ctx"][b]), "cvecT": cvecT, "rope": np.ascontiguousarray(rope_c),
         "ident": np.eye(128, dtype=np.float32), "halo_mask": hm}
    m["e_g_pre_mixT"] = _fm(inp["e_g_pre_mix"][0])
    m["e_g_pre_ffnT"] = _fm(inp["e_g_pre_ffn"][0])
    m["o_g_pre_mixT"] = _fm(inp["o_g_pre_mix"][0])
    m["o_g_pre_ffnT"] = _fm(inp["o_g_pre_ffn"][0])
    m["e_g_post_mix"] = f(inp["e_g_post_mix"][0]).reshape(1, -1)
    m["e_g_post_ffn"] = f(inp["e_g_post_ffn"][0]).reshape(1, -1)
    m["o_g_post_mix"] = f(inp["o_g_post_mix"][0]).reshape(1, -1)
    m["o_g_post_ffn"] = f(inp["o_g_post_ffn"][0]).reshape(1, -1)
    for pre in ("e", "o"):
        m[pre + "_w_mod"] = f(inp[pre + "_w_mod"][0])
        m[pre + "_bmodT"] = np.ascontiguousarray(np.asarray(inp[pre + "_b_mod"][0], np.float32).reshape(48, 128).T)
        m[pre + "_b_mod"] = f(inp[pre + "_b_mod"][0]).reshape(1, -1)
        m[pre + "_w_in"] = f(inp[pre + "_w_in"][0])
        m[pre + "_w_out"] = f(inp[pre + "_w_out"][0])
    m["e_g_q"] = f(inp["e_g_q"][0]).reshape(1, 64)
    m["e_g_k"] = f(inp["e_g_k"][0]).reshape(1, 64)
    wc = np.asarray(inp["e_w_conv"][0], np.float32)
    m["e_w_convT"] = np.ascontiguousarray(wc.reshape(3, 4, 128).transpose(2, 1, 0).reshape(128, 12))
    m["e_w_gate"] = f(inp["e_w_gate"][0])
    m["e_w_up"] = f(inp["e_w_up"][0])
    m["e_w_down"] = f(inp["e_w_down"][0])
    m["o_g_vT"] = _fm(inp["o_g_v"][0])
    m["o_b_vT"] = _fm(inp["o_b_v"][0])
    m["o_w_s"] = f(inp["o_w_s"][0])
    m["o_b_s"] = f(inp["o_b_s"][0])
    m["o_w_router"] = f(inp["o_w_router"][0])
    m["o_b_router"] = f(inp["o_b_router"][0]).reshape(1, NEXP)
    m["o_w_gate"] = f(inp["o_w_gate"][0])
    m["o_w_up"] = f(inp["o_w_up"][0])
    m["o_w_down"] = f(inp["o_w_down"][0])
    return m


def kernel(**inputs):
    nc = _build({})
    rope_tab = _rope_tables()
    in_maps = [_core_inputs(inputs, r, rope_tab) for r in range(8)]
    res = run_bass_kernel_spmd(nc, in_maps, core_ids=list(range(8)))
    out = np.empty((NB, SEQ, D), np.float32)
    for r in range(8):
        b, h = r // 2, r % 2
        out[b, h * NOWN:(h + 1) * NOWN] = res.results[r]["out"]
    return out
```

```python
import numpy as np
from contextlib import ExitStack
import concourse.bass as bass
import concourse.mybir as mybir
from concourse.bass_utils import run_bass_kernel_spmd

F32 = mybir.dt.float32
BF16 = mybir.dt.bfloat16
AF = mybir.ActivationFunctionType
ALU = mybir.AluOpType
AX = mybir.AxisListType

D = 1024
SEQ = 8192
NB = 4
CTX = 256
NOWN = 4096
NT_OWN = 32
EPS = 1e-6
IN_W = 2304
D_FF = 2816
E_FF = 3584
NEXP = 8


class Buf:
    __slots__ = ("name", "w", "r")

    def __init__(self, name):
        self.name = name
        self.w = None
        self.r = {}


class Eng:
    def __init__(self, nc, name, eng):
        self.name = name
        self.e = eng
        self.sem = nc.alloc_semaphore("sem_" + name)
        self.cnt = 0
        self.waited = {}

    def wait(self, tok):
        if tok is None:
            return
        key, sem, val, _ = tok
        if self.waited.get(key, 0) >= val:
            return
        self.e.wait_ge(sem, val)
        self.waited[key] = val


class Prog:
    def __init__(self, nc):
        self.nc = nc
        self.PE = Eng(nc, "pe", nc.tensor)
        self.ACT = Eng(nc, "act", nc.scalar)
        self.DVE = Eng(nc, "dve", nc.vector)
        self.POOL = Eng(nc, "pool", nc.gpsimd)
        self.SP = Eng(nc, "sp", nc.sync)
        self.dsem = {}
        for q, n in (("sp", 12), ("pool", 8)):
            self.dsem[q] = [[nc.alloc_semaphore(f"dq_{q}{i}"), 0, f"dq_{q}{i}"] for i in range(n)]
        self.dnext = {"sp": 0, "pool": 0}

    def _deps(self, E, reads, writes):
        for b in reads:
            E.wait(b.w)
        for b in writes:
            if b.w is not None and not (E.name == "pe" and b.w[3] == "pe"):
                E.wait(b.w)
            for t in b.r.values():
                if not (t[3] == E.name and E.name == "pe"):
                    E.wait(t)

    def _record(self, tok, ename, reads, writes):
        for b in reads:
            b.r[ename] = tok
        for b in writes:
            b.w = tok
            b.r = {}

    def op(self, E, fn, reads=(), writes=(), inc=True):
        self._deps(E, reads, writes)
        ins = fn()
        if inc:
            E.cnt += 1
            ins.then_inc(E.sem, 1)
            tok = (E.name, E.sem, E.cnt, E.name)
        else:
            tok = (E.name, E.sem, E.cnt + 1, E.name)
        self._record(tok, E.name, reads, writes)
        return ins

    def dma(self, Q, out, in_, reads=(), writes=(), slow=False):
        q = Q.name
        slot = self.dsem[q][self.dnext[q]]
        self.dnext[q] = (self.dnext[q] + 1) % len(self.dsem[q])
        if slot[1] > 0:
            Q.wait((slot[2], slot[0], slot[1], "dma"))
        self._deps(Q, reads, writes)
        if slow:
            ins = Q.e.dma_start(out=out, in_=in_, allow_slow_non_contiguous=True)
        else:
            ins = Q.e.dma_start(out=out, in_=in_)
        slot[1] += 16
        ins.then_inc(slot[0], 16)
        tok = (slot[2], slot[0], slot[1], "dma")
        for b in reads:
            b.r["dma_" + slot[2]] = tok
        for b in writes:
            b.w = tok
            b.r = {}
        return tok

    def dma_ind(self, out, out_offset, in_, in_offset, reads=(), writes=()):
        Q = self.POOL
        q = "pool"
        slot = self.dsem[q][self.dnext[q]]
        self.dnext[q] = (self.dnext[q] + 1) % len(self.dsem[q])
        if slot[1] > 0:
            Q.wait((slot[2], slot[0], slot[1], "dma"))
        self._deps(Q, reads, writes)
        ins = Q.e.indirect_dma_start(out=out, out_offset=out_offset, in_=in_, in_offset=in_offset)
        slot[1] += 16
        ins.then_inc(slot[0], 16)
        tok = (slot[2], slot[0], slot[1], "dma")
        for b in reads:
            b.r["dma_" + slot[2]] = tok
        for b in writes:
            b.w = tok
            b.r = {}
        return tok

    def predicated(self, regs, thresh, body):
        nc = self.nc
        engs = (self.PE, self.ACT, self.DVE, self.POOL, self.SP)
        cnt0 = {E.name: E.cnt for E in engs}
        waited0 = {E.name: dict(E.waited) for E in engs}
        d0 = {q: [sl[1] for sl in slots] for q, slots in self.dsem.items()}
        with nc.If_cmp(regs, thresh, "IS_GT"):
            body()
        for E in engs:
            E.waited = waited0[E.name]
        with nc.Else():
            for E in engs:
                delta = E.cnt - cnt0[E.name]
                if delta > 0:
                    E.e.drain()
                    E.e.sem_inc(E.sem, delta)
            for q, slots in self.dsem.items():
                Q = self.SP if q == "sp" else self.POOL
                for i, sl in enumerate(slots):
                    delta = sl[1] - d0[q][i]
                    if delta > 0:
                        if d0[q][i] > 0:
                            Q.e.wait_ge(sl[0], d0[q][i])
                        Q.e.sem_inc(sl[0], delta)
        for E in engs:
            E.waited = waited0[E.name]
        if getattr(self, "_dbgreg", False):
            self._nreg = getattr(self, "_nreg", 0) + 1
            for E in engs:
                got = []
                try:
                    while True:
                        got.append(E.e.alloc_register(f"probe_{E.name}_{self._nreg}_{len(got)}"))
                except Exception:
                    pass
                for r in got:
                    E.e.free_register(r)
                if self._nreg <= 3 or self._nreg % 20 == 0:
                    print("region", self._nreg, E.name, "free regs", len(got), flush=True)

    def barrier(self):
        engs = (self.PE, self.ACT, self.DVE, self.POOL, self.SP)
        for E in engs:
            for O in engs:
                if O is not E and O.cnt > 0:
                    E.wait((O.name, O.sem, O.cnt, O.name))
            self.drain_all(E)

    def drain_all(self, E):
        for q in self.dsem.values():
            for slot in q:
                if slot[1] > 0:
                    E.wait((slot[2], slot[0], slot[1], "dma"))


def _build(cfg):
    dbg = cfg.get("dbg", False)
    phases = cfg.get("phases", "0ABCD")
    nblk_a = cfg.get("nblk_a", 8)
    nc = bass.Bass("TRN2", target_bir_lowering=False)
    es = ExitStack()
    P = Prog(nc)
    PE, ACT, DVE, POOL, SP = P.PE, P.ACT, P.DVE, P.POOL, P.SP

    def din(name, shape, dt=F32):
        return nc.dram_tensor(name, list(shape), dt, kind="ExternalInput").ap()

    def dscr(name, shape, dt=F32, out=False):
        kind = "ExternalOutput" if out else "Internal"
        return nc.dram_tensor(name, list(shape), dt, kind=kind).ap()

    def sb(name, shape, dt=F32):
        return es.enter_context(nc.sbuf_tensor(name, list(shape), dt))

    xown = din("xown", [NOWN, D])
    xoth = din("xoth", [NOWN, D])
    ctx = din("ctx", [CTX, D])
    cvecT = din("cvecT", [128, 16])
    rope = din("rope", [8192, 64])
    ident_in = din("ident", [128, 128])
    halo_mask = din("halo_mask", [128, 2])
    W = {}
    for pre, win_w in (("e", IN_W), ("o", 2048)):
        W[pre + "_w_mod"] = din(pre + "_w_mod", [D, 6 * D])
        W[pre + "_bmodT"] = din(pre + "_bmodT", [128, 48])
        W[pre + "_b_mod"] = din(pre + "_b_mod", [1, 6 * D])
        for v in ("g_pre_mix", "g_pre_ffn"):
            W[pre + "_" + v + "T"] = din(pre + "_" + v + "T", [128, 8])
        for v in ("g_post_mix", "g_post_ffn"):
            W[pre + "_" + v] = din(pre + "_" + v, [1, D])
        W[pre + "_w_in"] = din(pre + "_w_in", [D, win_w])
        W[pre + "_w_out"] = din(pre + "_w_out", [D, D])
    W["e_g_q"] = din("e_g_q", [1, 64])
    W["e_g_k"] = din("e_g_k", [1, 64])
    W["e_w_convT"] = din("e_w_convT", [128, 12])
    W["e_w_gate"] = din("e_w_gate", [D, D_FF])
    W["e_w_up"] = din("e_w_up", [D, D_FF])
    W["e_w_down"] = din("e_w_down", [D_FF, D])
    W["o_g_vT"] = din("o_g_vT", [128, 8])
    W["o_b_vT"] = din("o_b_vT", [128, 8])
    W["o_w_s"] = din("o_w_s", [8, 128, 128])
    W["o_b_s"] = din("o_b_s", [8, 128])
    W["o_w_router"] = din("o_w_router", [D, NEXP])
    W["o_b_router"] = din("o_b_router", [1, NEXP])
    W["o_w_gate"] = din("o_w_gate", [NEXP, D, E_FF])
    W["o_w_up"] = din("o_w_up", [NEXP, D, E_FF])
    W["o_w_down"] = din("o_w_down", [NEXP, E_FF, D])

    out_d = nc.dram_tensor("out", [NOWN, D], F32, kind="ExternalOutput").ap()
    xA = dscr("xA", [NOWN, D], out=dbg)
    xB = dscr("xB", [NOWN, D], out=dbg)
    xC = dscr("xC", [NOWN, D], out=dbg)
    hT_scr = dscr("hT_scr", [8, 128, NOWN + 2], BF16, out=dbg)

    ident_f = sb("ident_f", [128, 128])
    ident_b = sb("ident_b", [128, 128], BF16)
    eps_t = sb("eps_t", [128, 1])
    ones_f = sb("ones_f", [128, 64])
    ones_f_full = sb("ones_f_full", [128, 128])
    hmask = sb("hmask", [128, 2])
    AS = {}
    for L in ("e", "o"):
        for k in ("A_mix", "S_mix", "A_ffn", "S_ffn"):
            AS[L + k] = sb(f"{L}{k}", [128, 8, 2])
    Gt = {k: sb("G_" + k, [128, D]) for k in ("e_mix", "e_ffn", "o_mix", "o_ffn")}
    B_consts = Buf("consts")
    B_mod = Buf("mod")

    PS = [es.enter_context(nc.psum_tensor(f"ps{i}", [128, 1024], F32)) for i in range(4)]
    PSB = [[Buf(f"ps{i}a"), Buf(f"ps{i}b")] for i in range(4)]

    def psb16(i, half):
        return PS[i][:, half * 512:(half + 1) * 512].bitcast(BF16)

    P.dma(SP, ident_f[:], ident_in[:], writes=[B_consts])
    P.dma(SP, hmask[:], halo_mask[:], writes=[B_consts])
    P.op(DVE, lambda: nc.vector.tensor_copy(out=ident_b[:], in_=ident_f[:]), reads=[B_consts], writes=[B_consts])
    P.op(DVE, lambda: nc.vector.memset(eps_t[:], EPS), writes=[B_consts])
    P.op(DVE, lambda: nc.vector.memset(ones_f[:], 1.0), writes=[B_consts])
    P.op(DVE, lambda: nc.vector.memset(ones_f_full[:], 1.0), writes=[B_consts])

    def rstd_from_ss(ss_ap, n, tmp_ap, out_ap, bufs_r, bufs_w, width=1):
        P.op(ACT, lambda: nc.scalar.activation(out=tmp_ap, in_=ss_ap, func=AF.Sqrt, bias=eps_t[:], scale=1.0 / n),
             reads=bufs_r + [B_consts], writes=bufs_w)
        P.op(DVE, lambda: nc.vector.reciprocal(out=out_ap, in_=tmp_ap), reads=bufs_w, writes=bufs_w)

    def phase0():
        st = ExitStack()

        def sbt(name, shape, dt=F32):
            return st.enter_context(nc.sbuf_tensor(name, list(shape), dt))
        cs_raw = sbt("cs_raw", [128, 16])
        cs = sbt("cs", [128, 16], BF16)
        csb = sbt("csb", [128, 8, 128], BF16)
        wm = [sbt(f"wm{i}", [128, 8, 512], BF16) for i in range(2)]
        Bwm = [Buf("wm0"), Buf("wm1")]
        modt = sbt("modt", [128, 48, 2])
        bmodT = sbt("bmodT", [128, 48])
        gpreT = sbt("gpreT", [128, 16])
        brow = sbt("brow", [128, 512])
        grow = sbt("grow", [128, 512])
        Bc = Buf("cs")
        Bm = Buf("modt")
        Bv = Buf("vecs")
        Brow = Buf("rows")
        P.dma(SP, cs_raw[:], cvecT[:], writes=[Bc])
        P.op(ACT, lambda: nc.scalar.activation(out=cs[:], in_=cs_raw[:], func=AF.Silu), reads=[Bc], writes=[Bc])
        for kc in range(8):
            P.op(DVE, lambda kc=kc: nc.vector.tensor_copy(out=csb[:, kc, :], in_=cs[:, 2 * kc:2 * kc + 1].to_broadcast([128, 128])),
                 reads=[Bc], writes=[Bc])
        pi = 0
        for L in ("e", "o"):
            wmod = W[L + "_w_mod"]
            P.dma(SP, bmodT[:], W[L + "_bmodT"][:], writes=[Bv])
            P.dma(SP, gpreT[:, 0:8], W[L + "_g_pre_mixT"][:], writes=[Bv])
            P.dma(SP, gpreT[:, 8:16], W[L + "_g_pre_ffnT"][:], writes=[Bv])
            for piece in range(12):
                s, half = piece // 2, piece % 2
                wb, Bw = wm[pi % 2], Bwm[pi % 2]
                pi += 1
                src = wmod[:, piece * 512:(piece + 1) * 512].rearrange("(kc p) n -> p kc n", p=128)
                P.dma(POOL, wb[:], src, writes=[Bw])
                if s in (0, 1, 3, 4):
                    for j in range(4):
                        idx = s * 8 + half * 4 + j
                        for kc in range(8):
                            P.op(PE, lambda kc=kc, j=j, idx=idx, wb=wb: nc.tensor.matmul(
                                PS[3][:, 2 * idx:2 * idx + 2], lhsT=wb[:, kc, j * 128:(j + 1) * 128],
                                rhs=cs[:, 2 * kc:2 * kc + 2], start=(kc == 0), stop=(kc == 7)),
                                reads=[Bw, Bc], writes=[PSB[3][0]], inc=(kc == 7))
                else:
                    for kc in range(8):
                        P.op(PE, lambda kc=kc, wb=wb: nc.tensor.matmul(
                            PS[3][:, 512:1024], lhsT=csb[:, kc, :], rhs=wb[:, kc, :],
                            start=(kc == 0), stop=(kc == 7)),
                            reads=[Bw, Bc], writes=[PSB[3][1]], inc=(kc == 7))
                    key = L + ("_mix" if s == 2 else "_ffn")
                    col0 = s * D + half * 512
                    P.dma(SP, brow[:], W[L + "_b_mod"][:, col0:col0 + 512].partition_broadcast(128), writes=[Brow])
                    gp = W[L + ("_g_post_mix" if s == 2 else "_g_post_ffn")]
                    P.dma(SP, grow[:], gp[:, half * 512:(half + 1) * 512].partition_broadcast(128), writes=[Brow])
                    gdst = Gt[key][:, half * 512:(half + 1) * 512]
                    P.op(DVE, lambda gdst=gdst: nc.vector.tensor_tensor(out=gdst, in0=PS[3][:, 512:1024], in1=brow[:], op=ALU.add),
                         reads=[PSB[3][1], Brow], writes=[B_mod])
                    P.op(DVE, lambda gdst=gdst: nc.vector.tensor_tensor(out=gdst, in0=gdst, in1=grow[:], op=ALU.mult),
                         reads=[B_mod, Brow], writes=[B_mod, Brow])
            for j0 in (0, 24):
                P.op(DVE, lambda j0=j0: nc.vector.tensor_tensor(
                    out=modt[:, j0:j0 + 16, :], in0=PS[3][:, 2 * j0:2 * j0 + 32].rearrange("p (j r) -> p j r", r=2),
                    in1=bmodT[:, j0:j0 + 16].unsqueeze(2).to_broadcast([128, 16, 2]), op=ALU.add),
                    reads=[PSB[3][0], Bv], writes=[Bm])
            for nm, s_shift, s_scale, goff in (("mix", 0, 1, 0), ("ffn", 3, 4, 8)):
                A = AS[L + "A_" + nm]
                S = AS[L + "S_" + nm]
                P.op(DVE, lambda S=S, s_shift=s_shift: nc.vector.tensor_copy(out=S[:], in_=modt[:, s_shift * 8:s_shift * 8 + 8, :]),
                     reads=[Bm], writes=[B_mod])
                P.op(DVE, lambda A=A, s_scale=s_scale: nc.vector.tensor_scalar(
                    out=A[:], in0=modt[:, s_scale * 8:s_scale * 8 + 8, :], scalar1=1.0, scalar2=None, op0=ALU.add),
                    reads=[Bm], writes=[B_mod])
                P.op(DVE, lambda A=A, goff=goff: nc.vector.tensor_tensor(
                    out=A[:], in0=A[:], in1=gpreT[:, goff:goff + 8].unsqueeze(2).to_broadcast([128, 8, 2]), op=ALU.mult),
                    reads=[B_mod, Bv], writes=[B_mod, Bv, Bm])
        P.barrier()
        st.close()

    def prenorm_a(x_t, Bx, wk):
        P.op(ACT, lambda: nc.scalar.activation(out=wk["junk"][:], in_=x_t, func=AF.Square, accum_out=wk["ss"][:]),
             reads=[Bx], writes=[wk["Bs"]])
        rstd_from_ss(wk["ss"][:], D, wk["sd"][:], wk["rstd"][:], [wk["Bs"]], [wk["Bs"]])
        P.op(DVE, lambda: nc.vector.tensor_scalar(out=wk["xn"][:], in0=x_t, scalar1=wk["rstd"][:], scalar2=None, op0=ALU.mult),
             reads=[Bx, wk["Bs"]], writes=[wk["Bxn"]])

    def prenorm_b(A, S, r, hT_dst, B_h, wk, psi):
        pi_, ph = psi
        pv = psb16(pi_, ph)
        for c in range(8):
            P.op(PE, lambda c=c: nc.tensor.transpose(out=pv[:, c * 128:(c + 1) * 128], in_=wk["xn"][:, c * 128:(c + 1) * 128], identity=ident_b[:]),
                 reads=[wk["Bxn"], B_consts], writes=[PSB[pi_][ph]], inc=(c == 7))
        for c in range(8):
            if c % 2 == 0:
                P.op(DVE, lambda c=c: nc.vector.tensor_scalar(
                    out=hT_dst[:, c, :], in0=pv[:, c * 128:(c + 1) * 128], scalar1=A[:, c, r:r + 1], scalar2=S[:, c, r:r + 1],
                    op0=ALU.mult, op1=ALU.add), reads=[PSB[pi_][ph], B_mod], writes=[B_h])
            else:
                P.op(ACT, lambda c=c: nc.scalar.activation(
                    out=hT_dst[:, c, :], in_=pv[:, c * 128:(c + 1) * 128], func=AF.Identity,
                    bias=S[:, c, r:r + 1], scale=A[:, c, r:r + 1]), reads=[PSB[pi_][ph], B_mod], writes=[B_h])

    def prenorm_tile(x_t, Bx, A, S, r, hT_dst, B_h, wk, psi, fp32=False):
        prenorm_a(x_t, Bx, wk)
        prenorm_b(A, S, r, hT_dst, B_h, wk, psi)

    def epilogue(y_ap, By, x_src_dram, G, out_dram, wk, add_on_dve=False):
        P.dma(SP, wk["x"][:], x_src_dram, writes=[wk["Bx"]])
        P.op(ACT, lambda: nc.scalar.activation(out=wk["junk"][:], in_=y_ap, func=AF.Square, accum_out=wk["ss"][:]),
             reads=By, writes=[wk["Bs"]])
        rstd_from_ss(wk["ss"][:], D, wk["sd"][:], wk["rstd"][:], [wk["Bs"]], [wk["Bs"]])
        P.op(DVE, lambda: nc.vector.scalar_tensor_tensor(out=wk["t"][:], in0=y_ap, scalar=wk["rstd"][:], in1=G[:], op0=ALU.mult, op1=ALU.mult),
             reads=By + [wk["Bs"], B_mod], writes=[wk["Bt"]])
        if add_on_dve:
            P.op(DVE, lambda: nc.vector.tensor_tensor(out=wk["x"][:], in0=wk["x"][:], in1=wk["t"][:], op=ALU.add),
                 reads=[wk["Bt"]], writes=[wk["Bx"]])
        else:
            P.op(POOL, lambda: nc.gpsimd.tensor_tensor(out=wk["x"][:], in0=wk["x"][:], in1=wk["t"][:], op=ALU.add),
                 reads=[wk["Bt"]], writes=[wk["Bx"]])
        P.dma(SP, out_dram, wk["x"][:], reads=[wk["Bx"]])

    def mk_wk(sbt, tag, n=2, with_x=True, with_xn=True):
        res = []
        for i in range(n):
            wk = {}
            wk["ss"] = sbt(f"{tag}ss{i}", [128, 1])
            wk["sd"] = sbt(f"{tag}sd{i}", [128, 1])
            wk["rstd"] = sbt(f"{tag}rstd{i}", [128, 1])
            wk["junk"] = sbt(f"{tag}junk{i}", [128, D], BF16)
            if with_xn:
                wk["xn"] = sbt(f"{tag}xn{i}", [128, D], BF16)
            wk["Bs"] = Buf("Bs")
            wk["Bxn"] = Buf("Bxn")
            if with_x:
                wk["x"] = sbt(f"{tag}x{i}", [128, D])
                wk["t"] = sbt(f"{tag}t{i}", [128, D])
                wk["Bx"] = Buf("Bx")
                wk["Bt"] = Buf("Bt")
            res.append(wk)
        return res

    def phaseA():
        st = ExitStack()

        def sbt(name, shape, dt=F32):
            return st.enter_context(nc.sbuf_tensor(name, list(shape), dt))
        NKT = 66
        A, S = AS["eA_mix"], AS["eS_mix"]
        w_in = W["e_w_in"]
        wqs = sbt("wqs", [128, 8, 2048], BF16)
        woA = sbt("woA", [64, 8, D], BF16)
        woC = sbt("woC", [128, 4, D], BF16)
        Bw = Buf("wA")
        w_in_v = w_in.rearrange("(kc p) n -> p kc n", p=128)
        for kc in range(8):
            P.dma(POOL, wqs[:, kc, 0:512], w_in_v[:, kc, 0:512], writes=[Bw])
            P.dma(POOL, wqs[:, kc, 512:2048], w_in_v[:, kc, 768:2304], writes=[Bw])
        wo = W["e_w_out"]
        P.dma(POOL, woA[:], wo[0:512, :].rearrange("(h p) n -> p h n", p=64), writes=[Bw])
        P.dma(POOL, woC[:], wo[512:1024, :].rearrange("(c p) n -> p c n", p=128), writes=[Bw])
        KT = sbt("KT", [128, 2, NKT * 128], BF16)
        VA = sbt("VA", [128, NKT, 2, 66], BF16)
        B_KV = Buf("KV")
        P.op(DVE, lambda: nc.vector.memset(VA[:, :, :, 64:66], 1.0), writes=[B_KV])
        gqB = sbt("gqB", [128, 64])
        gkB = sbt("gkB", [128, 64])
        wconv = sbt("wconv", [128, 12])
        Bg = Buf("g")
        P.dma(SP, gqB[:], W["e_g_q"][:].partition_broadcast(128), writes=[Bg])
        P.dma(SP, gkB[:], W["e_g_k"][:].partition_broadcast(128), writes=[Bg])
        P.dma(SP, wconv[:], W["e_w_convT"][:], writes=[Bg])

        NS = 2
        st_outer = st
        st = ExitStack()
        wkv = sbt("wkv", [128, 8, 256], BF16)
        P.dma(POOL, wkv[:], w_in_v[:, :, 512:768], writes=[Bw])
        wks = mk_wk(sbt, "a1", NS, with_x=True)
        hTt = [sbt(f"a1hT{i}", [128, 8, 128], BF16) for i in range(NS)]
        Bh = [Buf("hTt") for _ in range(NS)]
        ropet = [sbt(f"a1rope{i}", [128, 64]) for i in range(NS)]
        ksq = [sbt(f"a1ksq{i}", [128, 128]) for i in range(NS)]
        kss = [sbt(f"a1kss{i}", [128, 2]) for i in range(NS)]
        ksd = [sbt(f"a1ksd{i}", [128, 2]) for i in range(NS)]
        krs = [sbt(f"a1krs{i}", [128, 2]) for i in range(NS)]
        kn = [sbt(f"a1kn{i}", [128, 2, 64]) for i in range(NS)]
        ktmp = [sbt(f"a1ktmp{i}", [128, 4, 2, 32]) for i in range(NS)]
        kd = [sbt(f"a1kd{i}", [128, 2, 2, 64], BF16) for i in range(NS)]
        Bk = [Buf("k") for _ in range(NS)]
        Bkd = [Buf("kd") for _ in range(NS)]
        Brope = [Buf("rope") for _ in range(NS)]
        def a1_stage1(kt):
            sl = kt % NS
            wk = wks[sl]
            if kt < 2:
                src, r = ctx[kt * 128:(kt + 1) * 128, :], 1
            elif kt < 34:
                src, r = xown[(kt - 2) * 128:(kt - 1) * 128, :], 0
            else:
                src, r = xoth[(kt - 34) * 128:(kt - 33) * 128, :], 0
            P.dma(SP, wk["x"][:], src, writes=[wk["Bx"]])
            if kt >= 2:
                P.dma(SP, ropet[sl][:], rope[(kt - 2) * 128:(kt - 1) * 128, :], writes=[Brope[sl]])
            prenorm_tile(wk["x"][:], wk["Bx"], A, S, r, hTt[sl], Bh[sl], wk, (kt % 2, 0))
        def a1_stage2(kt):
            sl = kt % NS
            kb = kt % 2
            for kc in range(8):
                P.op(PE, lambda kc=kc, sl=sl: nc.tensor.matmul(PS[2][:, kb * 512:kb * 512 + 256], lhsT=hTt[sl][:, kc, :], rhs=wkv[:, kc, :],
                                                             start=(kc == 0), stop=(kc == 7)),
                     reads=[Bh[sl], Bw], writes=[PSB[2][kb]], inc=(kc == 7))
            kps = PS[2][:, kb * 512:kb * 512 + 128]
            vps = PS[2][:, kb * 512 + 128:kb * 512 + 256]
            P.op(ACT, lambda sl=sl: nc.scalar.activation(out=VA[:, kt, :, 0:64], in_=vps.rearrange("p (g d) -> p g d", g=2), func=AF.Copy),
                 reads=[PSB[2][kb]], writes=[B_KV])
            P.op(ACT, lambda sl=sl: nc.scalar.activation(out=ksq[sl][:], in_=kps, func=AF.Square), reads=[PSB[2][kb]], writes=[Bk[sl]])
            P.op(DVE, lambda sl=sl: nc.vector.tensor_reduce(out=kss[sl][:], in_=ksq[sl][:].rearrange("p (g d) -> p g d", g=2), axis=AX.X, op=ALU.add),
                 reads=[Bk[sl]], writes=[Bk[sl]])
            rstd_from_ss(kss[sl][:], 64, ksd[sl][:], krs[sl][:], [Bk[sl]], [Bk[sl]])
            P.op(DVE, lambda sl=sl: nc.vector.tensor_tensor(out=kn[sl][:], in0=kps.rearrange("p (g d) -> p g d", g=2),
                                                          in1=krs[sl][:].unsqueeze(2).to_broadcast([128, 2, 64]), op=ALU.mult),
                 reads=[PSB[2][kb], Bk[sl]], writes=[Bk[sl]])
            kdv = kd[sl]
            if kt < 2:
                P.op(DVE, lambda sl=sl, kdv=kdv: nc.vector.tensor_tensor(out=kdv[:, :, 0, :], in0=kn[sl][:],
                                                                       in1=gkB[:].unsqueeze(1).to_broadcast([128, 2, 64]), op=ALU.mult),
                     reads=[Bk[sl], Bg], writes=[Bkd[sl]])
            else:
                P.op(DVE, lambda sl=sl: nc.vector.tensor_tensor(out=kn[sl][:], in0=kn[sl][:],
                                                              in1=gkB[:].unsqueeze(1).to_broadcast([128, 2, 64]), op=ALU.mult),
                     reads=[Bk[sl], Bg], writes=[Bk[sl]])
                cosb = ropet[sl][:, 0:32].unsqueeze(1).to_broadcast([128, 2, 32])
                sinb = ropet[sl][:, 32:64].unsqueeze(1).to_broadcast([128, 2, 32])
                k1, k2 = kn[sl][:, :, 0:32], kn[sl][:, :, 32:64]
                tt = ktmp[sl]
                for j, (a, b_) in enumerate(((k1, cosb), (k2, sinb), (k2, cosb), (k1, sinb))):
                    P.op(DVE, lambda j=j, a=a, b_=b_, tt=tt: nc.vector.tensor_tensor(out=tt[:, j], in0=a, in1=b_, op=ALU.mult),
                         reads=[Bk[sl], Brope[sl]], writes=[Bk[sl]])
                P.op(DVE, lambda tt=tt, kdv=kdv: nc.vector.tensor_tensor(out=kdv[:, :, 0, 0:32], in0=tt[:, 0], in1=tt[:, 1], op=ALU.subtract),
                     reads=[Bk[sl]], writes=[Bkd[sl]])
                P.op(DVE, lambda tt=tt, kdv=kdv: nc.vector.tensor_tensor(out=kdv[:, :, 0, 32:64], in0=tt[:, 2], in1=tt[:, 3], op=ALU.add),
                     reads=[Bk[sl]], writes=[Bkd[sl]])
            P.op(DVE, lambda kdv=kdv: nc.vector.tensor_copy(out=kdv[:, :, 1, :], in_=kdv[:, :, 0, :]), reads=[Bkd[sl]], writes=[Bkd[sl]])
            pv = psb16(3, kb)
            for g in range(2):
                P.op(PE, lambda g=g, kdv=kdv: nc.tensor.transpose(out=pv[:, g * 128:(g + 1) * 128],
                                                                 in_=kdv[:, g].rearrange("p a d -> p (a d)"), identity=ident_b[:]),
                     reads=[Bkd[sl], B_consts], writes=[PSB[3][kb]], inc=(g == 1))
            P.op(DVE, lambda: nc.vector.tensor_copy(out=KT[:, :, kt * 128:(kt + 1) * 128], in_=pv[:, 0:256].rearrange("p (g t) -> p g t", g=2)),
                 reads=[PSB[3][kb]], writes=[B_KV])
            if 2 <= kt < 34:
                c0 = 1 + (kt - 2) * 128
                P.dma(SP, hT_scr[:, :, c0:c0 + 128].rearrange("c p t -> p c t"), hTt[sl][:], reads=[Bh[sl]])
            if kt == 34:
                P.dma(SP, hT_scr[:, :, NOWN + 1:NOWN + 2].rearrange("c p t -> p c t"), hTt[sl][:, :, 0:1], reads=[Bh[sl]], slow=True)
            if kt == 65:
                P.dma(SP, hT_scr[:, :, 0:1].rearrange("c p t -> p c t"), hTt[sl][:, :, 127:128], reads=[Bh[sl]], slow=True)
        a1_stage1(0)
        for kt in range(NKT):
            if kt + 1 < NKT:
                a1_stage1(kt + 1)
            a1_stage2(kt)
        B_scr = Buf("scr")
        P.barrier()
        st.close()
        st = st_outer

        hTb = [sbt(f"a2hT{i}", [128, 8, 514], BF16) for i in range(2)]
        BhT = [Buf("hTb") for _ in range(2)]
        qT = sbt("qT", [128, 4, 512], BF16)
        BqT = Buf("qT")
        qsq = sbt("qsq", [128, 512])
        qss = sbt("qss", [128, 8])
        qsd = sbt("qsd", [128, 8])
        qrs = sbt("qrs", [128, 8])
        qn = sbt("qn", [128, 8, 64])
        qtmp = sbt("qtmp", [128, 4, 8, 32])
        qr = sbt("qr", [128, 8, 64], BF16)
        ropeq = sbt("ropeq", [128, 64])
        Bq = Buf("q")
        Bqr = Buf("qr")
        Bropeq = Buf("ropeq")
        Zc = sbt("Zc", [128, 514])
        Z = sbt("Z", [128, 514])
        cv = sbt("cv", [128, 512])
        convT = sbt("convT", [128, 4, 512], BF16)
        Bz = Buf("Z")
        Bconv = Buf("convT")
        PT = [sbt(f"PT{i}", [128, 1024], BF16) for i in range(3)]
        BPT = [Buf("PT") for _ in range(3)]
        attnT = sbt("attnT", [64, 8, 512], BF16)
        Battn = Buf("attnT")
        oT = [sbt(f"oT{i}", [128, 512]) for i in range(2)]
        BoT = [Buf("oT") for _ in range(2)]
        bcb = [sbt(f"bcb{i}", [64, 512]) for i in range(2)]
        Bbcb = [Buf("bcb") for _ in range(2)]
        rcd = dscr("rcd", [4, 512])
        Brcd = [Buf("rcd") for _ in range(4)]
        rci = 0
        ewk = mk_wk(sbt, "a2e", 2, with_x=True, with_xn=False)
        pti = 0
        def a2_load(blk):
            hb, Bhb = hTb[blk % 2], BhT[blk % 2]
            P.dma(SP, hb[:], hT_scr[:, :, blk * 512:blk * 512 + 514].rearrange("c p t -> p c t"), writes=[Bhb])

        def a2_qproc_tile(blk, t):
            hb, Bhb = hTb[blk % 2], BhT[blk % 2]
            tok0 = blk * 512 + t * 128
            P.dma(SP, ropeq[:], rope[tok0:tok0 + 128, :], writes=[Bropeq])
            for kc in range(8):
                P.op(PE, lambda kc=kc, t=t: nc.tensor.matmul(PS[3][:, 0:512], lhsT=hb[:, kc, 1 + t * 128:1 + (t + 1) * 128], rhs=wqs[:, kc, 0:512],
                                                             start=(kc == 0), stop=(kc == 7)),
                     reads=[Bhb, Bw], writes=[PSB[3][0]], inc=(kc == 7))
            qps = PS[3][:, 0:512]
            P.op(ACT, lambda: nc.scalar.activation(out=qsq[:], in_=qps, func=AF.Square), reads=[PSB[3][0]], writes=[Bq])
            P.op(DVE, lambda: nc.vector.tensor_reduce(out=qss[:], in_=qsq[:].rearrange("p (h d) -> p h d", h=8), axis=AX.X, op=ALU.add),
                 reads=[Bq], writes=[Bq])
            rstd_from_ss(qss[:], 64, qsd[:], qrs[:], [Bq], [Bq])
            P.op(DVE, lambda: nc.vector.tensor_tensor(out=qn[:], in0=qps.rearrange("p (h d) -> p h d", h=8),
                                                      in1=qrs[:].unsqueeze(2).to_broadcast([128, 8, 64]), op=ALU.mult),
                 reads=[PSB[3][0], Bq], writes=[Bq])
            P.op(POOL, lambda: nc.gpsimd.tensor_tensor(out=qn[:], in0=qn[:], in1=gqB[:].unsqueeze(1).to_broadcast([128, 8, 64]), op=ALU.mult),
                 reads=[Bq, Bg], writes=[Bq])
            cosb = ropeq[:, 0:32].unsqueeze(1).to_broadcast([128, 8, 32])
            sinb = ropeq[:, 32:64].unsqueeze(1).to_broadcast([128, 8, 32])
            q1, q2 = qn[:, :, 0:32], qn[:, :, 32:64]
            for j, (a, b_) in enumerate(((q1, cosb), (q2, sinb), (q2, cosb), (q1, sinb))):
                if j < 2:
                    P.op(DVE, lambda j=j, a=a, b_=b_: nc.vector.tensor_tensor(out=qtmp[:, j], in0=a, in1=b_, op=ALU.mult),
                         reads=[Bq, Bropeq], writes=[Bq])
                else:
                    P.op(POOL, lambda j=j, a=a, b_=b_: nc.gpsimd.tensor_tensor(out=qtmp[:, j], in0=a, in1=b_, op=ALU.mult),
                         reads=[Bq, Bropeq], writes=[Bq])
            P.op(DVE, lambda: nc.vector.tensor_tensor(out=qr[:, :, 0:32], in0=qtmp[:, 0], in1=qtmp[:, 1], op=ALU.subtract),
                 reads=[Bq], writes=[Bqr])
            P.op(POOL, lambda: nc.gpsimd.tensor_tensor(out=qr[:, :, 32:64], in0=qtmp[:, 2], in1=qtmp[:, 3], op=ALU.add),
                 reads=[Bq], writes=[Bqr])
            pv = psb16(3, 1)
            for pr in range(4):
                P.op(PE, lambda pr=pr: nc.tensor.transpose(out=pv[:, pr * 128:(pr + 1) * 128],
                                                          in_=qr[:, 2 * pr:2 * pr + 2, :].rearrange("p h d -> p (h d)"), identity=ident_b[:]),
                     reads=[Bqr, B_consts], writes=[PSB[3][1]], inc=(pr == 3))
            P.op(DVE, lambda t=t: nc.vector.tensor_copy(out=qT[:, :, t * 128:(t + 1) * 128], in_=pv[:, 0:512].rearrange("p (a t) -> p a t", a=4)),
                 reads=[PSB[3][1]], writes=[BqT])

        def a2_conv(blk):
            hb, Bhb = hTb[blk % 2], BhT[blk % 2]
            first, last = (blk == 0), (blk == 7)
            for c in range(4):
                def proj(colbase, ps_ap_main, ps_ap_halo, Bps, halo):
                    for kc in range(8):
                        P.op(PE, lambda kc=kc: nc.tensor.matmul(ps_ap_main, lhsT=wqs[:, kc, colbase:colbase + 128], rhs=hb[:, kc, 1:513],
                                                                start=(kc == 0), stop=(kc == 7)),
                             reads=[Bhb, Bw], writes=[Bps], inc=(kc == 7 and not halo))
                    if halo:
                        for kc in range(8):
                            P.op(PE, lambda kc=kc: nc.tensor.matmul(ps_ap_halo, lhsT=wqs[:, kc, colbase:colbase + 128], rhs=hb[:, kc, 0:514:513],
                                                                    start=(kc == 0), stop=(kc == 7)),
                                 reads=[Bhb, Bw], writes=[halo], inc=(kc == 7))
                proj(512 + 512 + c * 128, PS[3][:, 0:512], PS[1][:, 0:2], PSB[3][0], PSB[1][0])
                P.op(ACT, lambda: nc.scalar.activation(out=Zc[:, 1:513], in_=PS[3][:, 0:512], func=AF.Copy), reads=[PSB[3][0]], writes=[Bz])
                P.op(ACT, lambda: nc.scalar.activation(out=Zc[:, 0:514:513], in_=PS[1][:, 0:2], func=AF.Copy), reads=[PSB[1][0]], writes=[Bz])
                proj(512 + 1024 + c * 128, PS[3][:, 512:1024], PS[1][:, 512:514], PSB[3][1], PSB[1][1])
                P.op(DVE, lambda: nc.vector.tensor_tensor(out=Z[:, 1:513], in0=PS[3][:, 512:1024], in1=Zc[:, 1:513], op=ALU.mult),
                     reads=[PSB[3][1], Bz], writes=[Bz])
                P.op(DVE, lambda: nc.vector.tensor_tensor(out=Z[:, 0:514:513], in0=PS[1][:, 512:514], in1=Zc[:, 0:514:513], op=ALU.mult),
                     reads=[PSB[1][1], Bz], writes=[Bz])
                if first:
                    P.op(DVE, lambda: nc.vector.tensor_scalar(out=Z[:, 0:1], in0=Z[:, 0:1], scalar1=hmask[:, 0:1], scalar2=None, op0=ALU.mult),
                         reads=[Bz, B_consts], writes=[Bz])
                if last:
                    P.op(DVE, lambda: nc.vector.tensor_scalar(out=Z[:, 513:514], in0=Z[:, 513:514], scalar1=hmask[:, 1:2], scalar2=None, op0=ALU.mult),
                         reads=[Bz, B_consts], writes=[Bz])
                proj(512 + c * 128, PS[3][:, 0:512], None, PSB[3][0], None)
                P.op(DVE, lambda c=c: nc.vector.tensor_scalar(out=cv[:], in0=Z[:, 0:512], scalar1=wconv[:, 3 * c:3 * c + 1], scalar2=None, op0=ALU.mult),
                     reads=[Bz, Bg], writes=[Bz])
                P.op(DVE, lambda c=c: nc.vector.scalar_tensor_tensor(out=cv[:], in0=Z[:, 1:513], scalar=wconv[:, 3 * c + 1:3 * c + 2], in1=cv[:],
                                                                      op0=ALU.mult, op1=ALU.add), reads=[Bz, Bg], writes=[Bz])
                P.op(DVE, lambda c=c: nc.vector.scalar_tensor_tensor(out=cv[:], in0=Z[:, 2:514], scalar=wconv[:, 3 * c + 2:3 * c + 3], in1=cv[:],
                                                                      op0=ALU.mult, op1=ALU.add), reads=[Bz, Bg], writes=[Bz])
                P.op(DVE, lambda c=c: nc.vector.tensor_tensor(out=convT[:, c, :], in0=PS[3][:, 0:512], in1=cv[:], op=ALU.mult),
                     reads=[PSB[3][0], Bz], writes=[Bconv])

        def a2_attn(blk):
            nonlocal pti, rci
            Sbuf = [(PS[0], PSB[0]), (PS[1], PSB[1]), (PS[3], PSB[3])]
            seq = [(hp_, kt_) for hp_ in range(4) for kt_ in range(NKT)]

            def s_mm(i):
                hp, kt = seq[i]
                g = hp // 2
                ps_, pb_ = Sbuf[i % 3]
                P.op(PE, lambda: nc.tensor.matmul(ps_[:, 0:512], lhsT=KT[0:64, g, kt * 128:(kt + 1) * 128], rhs=qT[0:64, hp, :], start=True, stop=True),
                     reads=[B_KV, BqT], writes=[pb_[0]], inc=False)
                P.op(PE, lambda: nc.tensor.matmul(ps_[:, 512:1024], lhsT=KT[64:128, g, kt * 128:(kt + 1) * 128], rhs=qT[64:128, hp, :], start=True, stop=True),
                     reads=[B_KV, BqT], writes=[pb_[1]], inc=True)
            s_mm(0)
            s_mm(1)
            for i, (hp, kt) in enumerate(seq):
                g = hp // 2
                if i + 2 < len(seq):
                    s_mm(i + 2)
                ps_, pb_ = Sbuf[i % 3]
                pt, Bpt = PT[pti % 3], BPT[pti % 3]
                pti += 1
                P.op(ACT, lambda ps_=ps_, pt=pt: nc.scalar.activation(out=pt[:], in_=ps_[:], func=AF.Exp, scale=0.125),
                     reads=[pb_[0], pb_[1]], writes=[Bpt])
                for hh in range(2):
                    P.op(PE, lambda hh=hh, pt=pt: nc.tensor.matmul(PS[2][0:65, hh * 512:(hh + 1) * 512], lhsT=VA[:, kt, g, 0:65],
                                                                  rhs=pt[:, hh * 512:(hh + 1) * 512], start=(kt == 0), stop=(kt == NKT - 1)),
                         reads=[B_KV, Bpt], writes=[PSB[2][hh]], inc=(kt == NKT - 1 or hh == 1))
                if kt == NKT - 1:
                    for hh in range(2):
                        o = oT[hh]
                        P.op(DVE, lambda hh=hh, o=o: nc.vector.tensor_copy(out=o[0:64, :], in_=PS[2][0:64, hh * 512:(hh + 1) * 512]),
                             reads=[PSB[2][hh]], writes=[BoT[hh]])
                        P.op(DVE, lambda hh=hh, o=o: nc.vector.reciprocal(out=o[64:65, :], in_=PS[2][64:65, hh * 512:(hh + 1) * 512]),
                             reads=[PSB[2][hh]], writes=[BoT[hh]])
                        slot = rci % 4
                        rci += 1
                        P.dma(SP, rcd[slot:slot + 1, :], o[64:65, :], reads=[BoT[hh]], writes=[Brcd[slot]])
                        P.dma(SP, bcb[hh][:, :], rcd[slot:slot + 1, :].partition_broadcast(64), reads=[Brcd[slot]], writes=[Bbcb[hh]])
                    for hh in range(2):
                        h = 2 * hp + hh
                        P.op(DVE, lambda h=h, hh=hh: nc.vector.tensor_tensor(out=attnT[:, h, :], in0=oT[hh][0:64, :], in1=bcb[hh][:, :], op=ALU.mult),
                             reads=[BoT[hh], Bbcb[hh]], writes=[Battn])

        def a2_wout_tile(blk, t):
            PSy, PSBy = (PS[0], PSB[0]) if t % 2 == 0 else (PS[1], PSB[1])
            wk = ewk[t % 2]
            for half in range(2):
                n0 = half * 512
                for h in range(8):
                    P.op(PE, lambda h=h, n0=n0, t=t: nc.tensor.matmul(PSy[:, n0:n0 + 512], lhsT=attnT[:, h, t * 128:(t + 1) * 128],
                                                                       rhs=woA[:, h, n0:n0 + 512], start=(h == 0), stop=False),
                         reads=[Battn, Bw], writes=[PSBy[half]], inc=False)
                for c in range(4):
                    P.op(PE, lambda c=c, n0=n0, t=t: nc.tensor.matmul(PSy[:, n0:n0 + 512], lhsT=convT[:, c, t * 128:(t + 1) * 128],
                                                                       rhs=woC[:, c, n0:n0 + 512], start=False, stop=(c == 3)),
                         reads=[Bconv, Bw], writes=[PSBy[half]], inc=(c == 3))
            r0 = blk * 512 + t * 128
            epilogue(PSy[:], [PSBy[0], PSBy[1]], xown[r0:r0 + 128, :], Gt["e_mix"], xA[r0:r0 + 128, :], wk)

        a2_load(0)
        for t in range(4):
            a2_qproc_tile(0, t)
        for blk in range(nblk_a):
            a2_conv(blk)
            if blk + 1 < nblk_a:
                a2_load(blk + 1)
            a2_attn(blk)
            for t in range(4):
                if blk + 1 < nblk_a:
                    a2_qproc_tile(blk + 1, t)
                a2_wout_tile(blk, t)
        P.barrier()
        st.close()

    def phase_ffn(L, x_in, x_out, n_exp, ff, gch, wg_d, wu_d, wd_d, fp32_router):
        st = ExitStack()

        def sbt(name, shape, dt=F32):
            return st.enter_context(nc.sbuf_tensor(L + name, list(shape), dt))
        A, S = AS[L + "A_ffn"], AS[L + "S_ffn"]
        G = Gt[L + "_ffn"]
        TB = 2048
        NTB = TB // 128
        gw = gch * 128
        ngrp = ff // gw
        nhb = 1
        hT_l = [sbt(f"f_hT{i}", [128, 8, TB], BF16) for i in range(nhb)]
        BhT_l = [[Buf("f_hT") for _ in range(NTB)] for _ in range(nhb)]
        hT, BhT = hT_l[0], BhT_l[0]
        acc = sbt("f_acc", [128, NTB, D])
        Bacc = [Buf("f_acc") for _ in range(NTB)]
        wgb = [sbt(f"f_wg{i}", [128, 8, gw], BF16) for i in range(2)]
        wub = [sbt(f"f_wu{i}", [128, 8, gw], BF16) for i in range(2)]
        wdb = [sbt(f"f_wd{i}", [128, gch, D], BF16) for i in range(2)]
        Bwt = [Buf("f_w") for _ in range(2)]
        act = [sbt(f"f_act{i}", [128, gch, 512], BF16) for i in range(2)]
        Bact = [Buf("f_act") for _ in range(2)]
        sil = [sbt(f"f_sil{i}", [128, 512]) for i in range(2)]
        Bsil = [Buf("f_sil") for _ in range(2)]
        wks = mk_wk(sbt, "f", 2, with_x=True, with_xn=not fp32_router)
        ewks = wks if fp32_router else mk_wk(sbt, "fe", 2, with_x=True, with_xn=False)
        if fp32_router:
            gates = sbt("f_gates", [128, NTB, NEXP])
            Bgates = [Buf("gates") for _ in range(NTB)]
            wr = sbt("f_wr", [128, 8, NEXP])
            brB = sbt("f_brB", [128, NEXP])
            Bwr = Buf("wr")
            P.dma(SP, wr[:], W["o_w_router"].rearrange("(kc p) n -> p kc n", p=128), writes=[Bwr])
            P.dma(SP, brB[:], W["o_b_router"][:].partition_broadcast(128), writes=[Bwr])
            xn32 = [sbt(f"f_xn32{i}", [128, D]) for i in range(2)]
            h32 = sbt("f_h32", [128, 8, 128])
            Bx32 = [Buf("xn32") for _ in range(2)]
            Bh32 = Buf("h32")
            rt = {k: sbt("f_rt_" + k, [128, 8]) for k in ("lg", "m1", "l2", "m2", "g")}
            rs = {k: sbt("f_rs_" + k, [128, 1]) for k in ("mx1", "mx2", "d", "e", "den", "w1", "w2")}
            Brt = Buf("rt")
        wi = 0
        si = 0
        for tb in range(NOWN // TB):
            def f_pa(t, tbx=None):
                tbx = tb if tbx is None else tbx
                wk = wks[t % 2]
                r0 = tbx * TB + t * 128
                P.dma(SP, wk["x"][:], x_in[r0:r0 + 128, :], writes=[wk["Bx"]])
                if not fp32_router:
                    prenorm_a(wk["x"][:], wk["Bx"], wk)
                else:
                    xs = xn32[t % 2]
                    P.op(ACT, lambda wk=wk: nc.scalar.activation(out=wk["junk"][:], in_=wk["x"][:], func=AF.Square, accum_out=wk["ss"][:]),
                         reads=[wk["Bx"]], writes=[wk["Bs"]])
                    rstd_from_ss(wk["ss"][:], D, wk["sd"][:], wk["rstd"][:], [wk["Bs"]], [wk["Bs"]])
                    P.op(DVE, lambda wk=wk: nc.vector.tensor_scalar(out=xs[:], in0=wk["x"][:], scalar1=wk["rstd"][:], scalar2=None, op0=ALU.mult),
                         reads=[wk["Bx"], wk["Bs"]], writes=[Bx32[t % 2]])

            def f_pb(t, tbx=None):
                tbx = tb if tbx is None else tbx
                wk = wks[t % 2]
                if not fp32_router:
                    prenorm_b(A, S, 0, hT_l[tbx % nhb][:, :, t * 128:(t + 1) * 128], BhT_l[tbx % nhb][t], wk, (3, t % 2))
                    return
                hdst = hT[:, :, t * 128:(t + 1) * 128]
                xs = xn32[t % 2]
                pp = 3 if t % 2 == 0 else 1
                for c in range(8):
                    half = c // 4
                    P.op(PE, lambda c=c: nc.tensor.transpose(out=PS[pp][:, c * 128:(c + 1) * 128], in_=xs[:, c * 128:(c + 1) * 128], identity=ident_f[:]),
                         reads=[Bx32[t % 2], B_consts], writes=[PSB[pp][half]], inc=(c % 4 == 3))
                for c in range(8):
                    half = c // 4
                    P.op(DVE, lambda c=c: nc.vector.tensor_scalar(out=h32[:, c, :], in0=PS[pp][:, c * 128:(c + 1) * 128], scalar1=A[:, c, 0:1],
                                                                  scalar2=S[:, c, 0:1], op0=ALU.mult, op1=ALU.add),
                         reads=[PSB[pp][half], B_mod], writes=[Bh32])
                P.op(POOL, lambda hdst=hdst: nc.gpsimd.tensor_copy(out=hdst, in_=h32[:]), reads=[Bh32], writes=[BhT[t]])
                for kc in range(8):
                    P.op(PE, lambda kc=kc: nc.tensor.matmul(PS[2][:, 0:NEXP], lhsT=h32[:, kc, :], rhs=wr[:, kc, :], start=(kc == 0), stop=(kc == 7)),
                         reads=[Bh32, Bwr], writes=[PSB[2][0]], inc=(kc == 7))
                V = nc.vector
                P.op(DVE, lambda: V.tensor_tensor(out=rt["lg"][:], in0=PS[2][:, 0:NEXP], in1=brB[:], op=ALU.add), reads=[PSB[2][0], Bwr], writes=[Brt])
                P.op(DVE, lambda: V.tensor_reduce(out=rs["mx1"][:], in_=rt["lg"][:], axis=AX.X, op=ALU.max), reads=[Brt], writes=[Brt])
                P.op(DVE, lambda: V.tensor_scalar(out=rt["m1"][:], in0=rt["lg"][:], scalar1=rs["mx1"][:], scalar2=None, op0=ALU.is_equal), reads=[Brt], writes=[Brt])
                P.op(DVE, lambda: V.scalar_tensor_tensor(out=rt["l2"][:], in0=rt["m1"][:], scalar=-1e30, in1=rt["lg"][:], op0=ALU.mult, op1=ALU.add), reads=[Brt], writes=[Brt])
                P.op(DVE, lambda: V.tensor_reduce(out=rs["mx2"][:], in_=rt["l2"][:], axis=AX.X, op=ALU.max), reads=[Brt], writes=[Brt])
                P.op(DVE, lambda: V.tensor_scalar(out=rt["m2"][:], in0=rt["l2"][:], scalar1=rs["mx2"][:], scalar2=None, op0=ALU.is_equal), reads=[Brt], writes=[Brt])
                P.op(DVE, lambda: V.tensor_tensor(out=rs["d"][:], in0=rs["mx2"][:], in1=rs["mx1"][:], op=ALU.subtract), reads=[Brt], writes=[Brt])
                P.op(ACT, lambda: nc.scalar.activation(out=rs["e"][:], in_=rs["d"][:], func=AF.Exp), reads=[Brt], writes=[Brt])
                P.op(DVE, lambda: V.tensor_scalar(out=rs["den"][:], in0=rs["e"][:], scalar1=1.0, scalar2=None, op0=ALU.add), reads=[Brt], writes=[Brt])
                P.op(DVE, lambda: V.reciprocal(out=rs["w1"][:], in_=rs["den"][:]), reads=[Brt], writes=[Brt])
                P.op(DVE, lambda: V.tensor_tensor(out=rs["w2"][:], in0=rs["e"][:], in1=rs["w1"][:], op=ALU.mult), reads=[Brt], writes=[Brt])
                P.op(DVE, lambda: V.tensor_scalar(out=rt["g"][:], in0=rt["m1"][:], scalar1=rs["w1"][:], scalar2=None, op0=ALU.mult), reads=[Brt], writes=[Brt])
                P.op(DVE, lambda t=t: V.scalar_tensor_tensor(out=gates[:, t, :], in0=rt["m2"][:], scalar=rs["w2"][:], in1=rt["g"][:], op0=ALU.mult, op1=ALU.add),
                     reads=[Brt], writes=[Bgates[t]])
            if True:
                f_pa(0)
                for t in range(NTB):
                    if t + 1 < NTB:
                        f_pa(t + 1)
                    f_pb(t)
            hT, BhT = hT_l[tb % nhb], BhT_l[tb % nhb]
            if dbg and fp32_router and tb == 0:
                dg = dscr("dbg_gates", [128, NTB, NEXP], out=True)
                dh = dscr("dbg_hT", [128, 8, TB], BF16, out=True)
                P.dma(SP, dg[:], gates[:], reads=Bgates)
                P.dma(SP, dh[:], hT[:], reads=BhT)
            elist = cfg.get("exp_list", list(range(n_exp))) if n_exp > 1 else [0]
            items = [(e, grp, sbk) for e in elist for grp in range(ngrp) for sbk in range(TB // 512)]
            state = {}

            def load_w(e, grp):
                nonlocal wi
                wsl = wi % 2
                wi += 1
                f0 = grp * gw
                if n_exp == 1:
                    gsrc, usrc, dsrc = wg_d[:, f0:f0 + gw], wu_d[:, f0:f0 + gw], wd_d[f0:f0 + gw, :]
                else:
                    gsrc, usrc, dsrc = wg_d[e, :, f0:f0 + gw], wu_d[e, :, f0:f0 + gw], wd_d[e, f0:f0 + gw, :]
                P.dma(POOL, wgb[wsl][:], gsrc.rearrange("(kc p) n -> p kc n", p=128), writes=[Bwt[wsl]])
                P.dma(POOL, wub[wsl][:], usrc.rearrange("(kc p) n -> p kc n", p=128), writes=[Bwt[wsl]])
                P.dma(POOL, wdb[wsl][:], dsrc.rearrange("(j p) n -> p j n", p=128), writes=[Bwt[wsl]])
                state[(e, grp)] = wsl

            def gateup(idx):
                nonlocal si
                e, grp, sbk = items[idx]
                if (e, grp) not in state:
                    load_w(e, grp)
                wsl = state[(e, grp)]
                asl = idx % 2
                hTr = [BhT[sbk * 4 + i] for i in range(4)]
                for j in range(gch):
                    for (wbuf, half) in ((wgb[wsl], 0), (wub[wsl], 1)):
                        for kc in range(8):
                            P.op(PE, lambda kc=kc, j=j, wbuf=wbuf, half=half: nc.tensor.matmul(
                                PS[j % 2][:, half * 512:(half + 1) * 512],
                                lhsT=wbuf[:, kc, j * 128:(j + 1) * 128], rhs=hT[:, kc, sbk * 512:(sbk + 1) * 512],
                                start=(kc == 0), stop=(kc == 7)),
                                reads=hTr + [Bwt[wsl]], writes=[PSB[j % 2][half]], inc=(kc == 7))
                    psg = PS[j % 2]
                    ssl = si % 2
                    si += 1
                    P.op(ACT, lambda psg=psg, ssl=ssl: nc.scalar.activation(out=sil[ssl][:], in_=psg[:, 0:512], func=AF.Silu),
                         reads=[PSB[j % 2][0]], writes=[Bsil[ssl]])
                    P.op(DVE, lambda psg=psg, ssl=ssl, j=j, asl=asl: nc.vector.tensor_tensor(out=act[asl][:, j, :], in0=psg[:, 512:1024], in1=sil[ssl][:], op=ALU.mult),
                         reads=[PSB[j % 2][1], Bsil[ssl]], writes=[Bact[asl]])

            def down(idx):
                e, grp, sbk = items[idx]
                wsl = state[(e, grp)]
                asl = idx % 2
                firstacc = (e == elist[0] and grp == 0)
                for t4 in range(4):
                    t = sbk * 4 + t4
                    pso = 2 + (t4 % 2)
                    for half in range(2):
                        for j in range(gch):
                            P.op(PE, lambda j=j, half=half, t4=t4, pso=pso, asl=asl: nc.tensor.matmul(
                                PS[pso][:, half * 512:(half + 1) * 512], lhsT=act[asl][:, j, t4 * 128:(t4 + 1) * 128],
                                rhs=wdb[wsl][:, j, half * 512:(half + 1) * 512], start=(j == 0), stop=(j == gch - 1)),
                                reads=[Bact[asl], Bwt[wsl]], writes=[PSB[pso][half]], inc=(j == gch - 1))
                    rd = [PSB[pso][0], PSB[pso][1]]
                    if n_exp == 1:
                        if firstacc:
                            P.op(DVE, lambda t=t, pso=pso: nc.vector.tensor_copy(out=acc[:, t, :], in_=PS[pso][:]), reads=rd, writes=[Bacc[t]])
                        else:
                            P.op(DVE, lambda t=t, pso=pso: nc.vector.tensor_tensor(out=acc[:, t, :], in0=PS[pso][:], in1=acc[:, t, :], op=ALU.add),
                                 reads=rd, writes=[Bacc[t]])
                    else:
                        if firstacc:
                            P.op(DVE, lambda t=t, pso=pso, e=e: nc.vector.tensor_scalar(out=acc[:, t, :], in0=PS[pso][:], scalar1=gates[:, t, e:e + 1],
                                                                                     scalar2=None, op0=ALU.mult), reads=rd + [Bgates[t]], writes=[Bacc[t]])
                        else:
                            P.op(DVE, lambda t=t, pso=pso, e=e: nc.vector.scalar_tensor_tensor(out=acc[:, t, :], in0=PS[pso][:], scalar=gates[:, t, e:e + 1],
                                                                                            in1=acc[:, t, :], op0=ALU.mult, op1=ALU.add),
                                 reads=rd + [Bgates[t]], writes=[Bacc[t]])
            gateup(0)
            nxt_t = 0
            overlap_next = False
            for idx in range(len(items)):
                if idx + 1 < len(items):
                    gateup(idx + 1)
                down(idx)
                if overlap_next and idx >= 2 and idx % 2 == 0 and nxt_t < NTB:
                    f_pa(nxt_t, tb + 1)
                    f_pb(nxt_t, tb + 1)
                    nxt_t += 1
            if overlap_next:
                while nxt_t < NTB:
                    f_pa(nxt_t, tb + 1)
                    f_pb(nxt_t, tb + 1)
                    nxt_t += 1
            for t in range(NTB):
                r0 = tb * TB + t * 128
                epilogue(acc[:, t, :], [Bacc[t]], x_in[r0:r0 + 128, :], G, x_out[r0:r0 + 128, :], ewks[t % 2])
        P.barrier()
        st.close()

    def phase_moe(x_in, x_out):
        I32 = mybir.dt.int32
        A, S = AS["oA_ffn"], AS["oS_ffn"]
        G = Gt["o_ffn"]
        CAP = 4608
        NSUB = 9
        hsorted = dscr("hsorted", [NEXP * CAP, D], BF16)
        ysorted = dscr("ysorted", [NEXP * CAP, D], F32)
        flags_d = dscr("flags_d", [1, NEXP * 16], I32, out=dbg)
        st0 = ExitStack()

        def sb0(name, shape, dt=F32):
            return st0.enter_context(nc.sbuf_tensor("m_" + name, list(shape), dt))
        dest_i = sb0("dest_i", [128, NT_OWN, 2], I32)
        wts = sb0("wts", [128, NT_OWN, 2])
        Bdest = [Buf("dest") for _ in range(NT_OWN)]
        st = ExitStack()

        def sbt(name, shape, dt=F32):
            return st.enter_context(nc.sbuf_tensor("mr_" + name, list(shape), dt))
        wks = mk_wk(sbt, "mr", 2, with_x=True, with_xn=False)
        wr = sbt("wr", [128, 8, NEXP])
        brB = sbt("brB", [128, NEXP])
        Bwr = Buf("wr")
        P.dma(SP, wr[:], W["o_w_router"].rearrange("(kc p) n -> p kc n", p=128), writes=[Bwr])
        P.dma(SP, brB[:], W["o_b_router"][:].partition_broadcast(128), writes=[Bwr])
        xn32 = [sbt(f"xn32{i}", [128, D]) for i in range(2)]
        xnb = [sbt(f"xnb{i}", [128, D], BF16) for i in range(2)]
        Bx32 = [Buf("xn32") for _ in range(2)]
        Bxnb = [Buf("xnb") for _ in range(2)]
        h32 = sbt("h32", [128, 8, 128])
        Bh32 = Buf("h32")
        rt = {k: sbt("rt_" + k, [128, 8]) for k in ("lg", "m1", "l2", "m2", "pos", "t1", "t2", "macc", "ebase")}
        rs = {k: sbt("rs_" + k, [128, 1]) for k in ("mx1", "mx2", "d", "e", "den", "d1", "d2")}
        maskb = sbt("maskb", [128, 8], BF16)
        maccb = sbt("maccb", [128, 8], BF16)
        ustrict = sbt("ustrict", [128, 128], BF16)
        ones_b = sbt("ones_b", [128, 128], BF16)
        fl = sbt("fl", [128, NSUB, NEXP])
        fl_i = sbt("fl_i", [128, NEXP, 16], I32)
        nsub_f = sbt("nsub_f", [128, NEXP])
        Brt = Buf("rt")
        Bmacc = Buf("macc")
        Bc2 = Buf("c2")
        V = nc.vector
        ustr_f = sbt("ustr_f", [128, 128])
        P.op(DVE, lambda: V.memset(ones_b[:], 1.0), writes=[Bc2])
        P.op(DVE, lambda: V.memset(rt["macc"][:], 0.0), writes=[Bmacc])
        P.op(DVE, lambda: V.memset(maccb[:], 0.0), writes=[Bmacc])
        for e in range(NEXP):
            P.op(DVE, lambda e=e: V.memset(rt["ebase"][:, e:e + 1], float(e * CAP)), writes=[Bc2])
        P.op(DVE, lambda: V.tensor_tensor_scan(out=ustr_f[:], data0=ones_f_full[:], data1=ident_f[:], initial=0.0, op0=ALU.mult, op1=ALU.add),
             reads=[B_consts], writes=[Bc2])
        P.op(DVE, lambda: V.tensor_tensor(out=ustrict[:], in0=ustr_f[:], in1=ident_f[:], op=ALU.subtract), reads=[Bc2, B_consts], writes=[Bc2])

        def r_pa(t):
            wk = wks[t % 2]
            r0 = t * 128
            P.dma(SP, wk["x"][:], x_in[r0:r0 + 128, :], writes=[wk["Bx"]])
            xs = xn32[t % 2]
            P.op(ACT, lambda: nc.scalar.activation(out=wk["junk"][:], in_=wk["x"][:], func=AF.Square, accum_out=wk["ss"][:]),
                 reads=[wk["Bx"]], writes=[wk["Bs"]])
            rstd_from_ss(wk["ss"][:], D, wk["sd"][:], wk["rstd"][:], [wk["Bs"]], [wk["Bs"]])
            P.op(DVE, lambda: V.tensor_scalar(out=xs[:], in0=wk["x"][:], scalar1=wk["rstd"][:], scalar2=None, op0=ALU.mult),
                 reads=[wk["Bx"], wk["Bs"]], writes=[Bx32[t % 2]])
            P.op(ACT, lambda: nc.scalar.activation(out=xnb[t % 2][:], in_=xs[:], func=AF.Copy), reads=[Bx32[t % 2]], writes=[Bxnb[t % 2]])

        def r_pb(t):
            xs = xn32[t % 2]
            pp = 3 if t % 2 == 0 else 1
            for c in range(8):
                half = c // 4
                P.op(PE, lambda c=c: nc.tensor.transpose(out=PS[pp][:, c * 128:(c + 1) * 128], in_=xs[:, c * 128:(c + 1) * 128], identity=ident_f[:]),
                     reads=[Bx32[t % 2], B_consts], writes=[PSB[pp][half]], inc=(c % 4 == 3))
            for c in range(8):
                half = c // 4
                P.op(DVE, lambda c=c: V.tensor_scalar(out=h32[:, c, :], in0=PS[pp][:, c * 128:(c + 1) * 128], scalar1=A[:, c, 0:1],
                                                      scalar2=S[:, c, 0:1], op0=ALU.mult, op1=ALU.add),
                     reads=[PSB[pp][half], B_mod], writes=[Bh32])
            for kc in range(8):
                P.op(PE, lambda kc=kc: nc.tensor.matmul(PS[2][:, 0:NEXP], lhsT=h32[:, kc, :], rhs=wr[:, kc, :], start=(kc == 0), stop=(kc == 7)),
                     reads=[Bh32, Bwr], writes=[PSB[2][0]], inc=(kc == 7))
            P.op(DVE, lambda: V.tensor_tensor(out=rt["lg"][:], in0=PS[2][:, 0:NEXP], in1=brB[:], op=ALU.add), reads=[PSB[2][0], Bwr], writes=[Brt])
            P.op(DVE, lambda: V.tensor_reduce(out=rs["mx1"][:], in_=rt["lg"][:], axis=AX.X, op=ALU.max), reads=[Brt], writes=[Brt])
            P.op(DVE, lambda: V.tensor_scalar(out=rt["m1"][:], in0=rt["lg"][:], scalar1=rs["mx1"][:], scalar2=None, op0=ALU.is_equal), reads=[Brt], writes=[Brt])
            P.op(DVE, lambda: V.scalar_tensor_tensor(out=rt["l2"][:], in0=rt["m1"][:], scalar=-1e30, in1=rt["lg"][:], op0=ALU.mult, op1=ALU.add), reads=[Brt], writes=[Brt])
            P.op(DVE, lambda: V.tensor_reduce(out=rs["mx2"][:], in_=rt["l2"][:], axis=AX.X, op=ALU.max), reads=[Brt], writes=[Brt])
            P.op(DVE, lambda: V.tensor_scalar(out=rt["m2"][:], in0=rt["l2"][:], scalar1=rs["mx2"][:], scalar2=None, op0=ALU.is_equal), reads=[Brt], writes=[Brt])
            P.op(DVE, lambda: V.tensor_tensor(out=rs["d"][:], in0=rs["mx2"][:], in1=rs["mx1"][:], op=ALU.subtract), reads=[Brt], writes=[Brt])
            P.op(ACT, lambda: nc.scalar.activation(out=rs["e"][:], in_=rs["d"][:], func=AF.Exp), reads=[Brt], writes=[Brt])
            P.op(DVE, lambda: V.tensor_scalar(out=rs["den"][:], in0=rs["e"][:], scalar1=1.0, scalar2=None, op0=ALU.add), reads=[Brt], writes=[Brt])
            P.op(DVE, lambda: V.reciprocal(out=wts[:, t, 0:1], in_=rs["den"][:]), reads=[Brt], writes=[Bdest[t]])
            P.op(DVE, lambda: V.tensor_tensor(out=wts[:, t, 1:2], in0=rs["e"][:], in1=wts[:, t, 0:1], op=ALU.mult), reads=[Brt, Bdest[t]], writes=[Bdest[t]])
            P.op(DVE, lambda: V.tensor_tensor(out=maskb[:], in0=rt["m1"][:], in1=rt["m2"][:], op=ALU.add), reads=[Brt], writes=[Brt])
            P.op(PE, lambda: nc.tensor.matmul(PS[2][:, 512:512 + NEXP], lhsT=ustrict[:], rhs=maskb[:], start=True, stop=False),
                 reads=[Brt, Bc2], writes=[PSB[2][1]], inc=False)
            P.op(PE, lambda: nc.tensor.matmul(PS[2][:, 512:512 + NEXP], lhsT=ones_b[:], rhs=maccb[:], start=False, stop=True),
                 reads=[Bmacc, Bc2], writes=[PSB[2][1]], inc=True)
            P.op(DVE, lambda: V.tensor_tensor(out=rt["pos"][:], in0=PS[2][:, 512:512 + NEXP], in1=rt["ebase"][:], op=ALU.add), reads=[PSB[2][1], Bc2], writes=[Brt])
            P.op(DVE, lambda: V.tensor_tensor(out=rt["t1"][:], in0=rt["pos"][:], in1=rt["m1"][:], op=ALU.mult), reads=[Brt], writes=[Brt])
            P.op(DVE, lambda: V.tensor_reduce(out=rs["d1"][:], in_=rt["t1"][:], axis=AX.X, op=ALU.add), reads=[Brt], writes=[Brt])
            P.op(DVE, lambda: V.tensor_tensor(out=rt["t2"][:], in0=rt["pos"][:], in1=rt["m2"][:], op=ALU.mult), reads=[Brt], writes=[Brt])
            P.op(DVE, lambda: V.tensor_reduce(out=rs["d2"][:], in_=rt["t2"][:], axis=AX.X, op=ALU.add), reads=[Brt], writes=[Brt])
            P.op(DVE, lambda: V.tensor_copy(out=dest_i[:, t, 0:1], in_=rs["d1"][:]), reads=[Brt], writes=[Bdest[t]])
            P.op(DVE, lambda: V.tensor_copy(out=dest_i[:, t, 1:2], in_=rs["d2"][:]), reads=[Brt], writes=[Bdest[t]])
            P.op(DVE, lambda: V.tensor_tensor(out=rt["macc"][:], in0=rt["macc"][:], in1=maskb[:], op=ALU.add), reads=[Brt, Bmacc], writes=[Bmacc])
            P.op(DVE, lambda: V.tensor_copy(out=maccb[:], in_=rt["macc"][:]), reads=[Bmacc], writes=[Bmacc])
            for k in range(2):
                P.dma_ind(hsorted[:, :], bass.IndirectOffsetOnAxis(ap=dest_i[:, t, k:k + 1], axis=0), xnb[t % 2][:, :], None,
                          reads=[Bxnb[t % 2], Bdest[t]])
        r_pa(0)
        for t in range(NT_OWN):
            if t + 1 < NT_OWN:
                r_pa(t + 1)
            r_pb(t)
        P.op(PE, lambda: nc.tensor.matmul(PS[2][:, 0:NEXP], lhsT=ones_b[:], rhs=maccb[:], start=True, stop=True), reads=[Bmacc, Bc2], writes=[PSB[2][0]], inc=True)
        Bfl = Buf("fl")
        for j in range(NSUB):
            P.op(DVE, lambda j=j: V.tensor_scalar(out=fl[:, j, :], in0=PS[2][:, 0:NEXP], scalar1=float(j * 512) + 0.5, scalar2=None, op0=ALU.is_gt),
                 reads=[PSB[2][0]], writes=[Bfl])
        P.op(DVE, lambda: V.memset(fl_i[:], 0), writes=[Bfl])
        P.op(DVE, lambda: V.tensor_reduce(out=nsub_f[:], in_=fl[:].rearrange("p j e -> p e j"), axis=AX.X, op=ALU.add), reads=[Bfl], writes=[Bfl])
        P.op(DVE, lambda: V.tensor_copy(out=fl_i[:, :, 0:1], in_=nsub_f[:].unsqueeze(2)), reads=[Bfl], writes=[Bfl])
        tok_flags = P.dma(SP, flags_d[:, :], fl_i[0:1, :, :].rearrange("p a b -> p (a b)"), reads=[Bfl])
        if dbg:
            ddst = dscr("dbg_dest", [128, NT_OWN, 2], I32, out=True)
            P.dma(SP, ddst[:], dest_i[:], reads=Bdest)
            dw = dscr("dbg_wts", [128, NT_OWN, 2], out=True)
            P.dma(SP, dw[:], wts[:], reads=Bdest)
        P.barrier()
        st.close()

        st = ExitStack()

        def sbt(name, shape, dt=F32):
            return st.enter_context(nc.sbuf_tensor("me_" + name, list(shape), dt))
        gch, gw, ngrp = 4, 512, E_FF // 512
        wgb = [sbt(f"wg{i}", [128, 8, gw], BF16) for i in range(2)]
        wub = [sbt(f"wu{i}", [128, 8, gw], BF16) for i in range(2)]
        wdb = [sbt(f"wd{i}", [128, gch, D], BF16) for i in range(2)]
        Bwt = [Buf("w") for _ in range(2)]
        act = [sbt(f"act{i}", [128, gch, 512], BF16) for i in range(2)]
        Bact = [[Buf("act") for _ in range(4)] for _ in range(2)]
        sil = [sbt(f"sil{i}", [128, 512]) for i in range(2)]
        Bsil = [Buf("sil") for _ in range(2)]
        hTc = sbt("hTc", [128, 8, 1536], BF16)
        BhTc = [Buf("hTc") for _ in range(12)]
        acc = sbt("acc", [128, 12, D])
        Bacc = [Buf("acc") for _ in range(12)]
        xr = [sbt(f"xr{i}", [128, D], BF16) for i in range(2)]
        Bxr = [Buf("xr") for _ in range(2)]
        all_eng = [mybir.EngineType.PE, mybir.EngineType.Activation, mybir.EngineType.DVE, mybir.EngineType.Pool, mybir.EngineType.SP]
        Rn = nc.alloc_registers("nsub", all_eng)
        for E in (PE, ACT, DVE, POOL, SP):
            E.wait(tok_flags)
        wi = 0
        si = 0
        xi = 0
        for e in range(NEXP):
            for reg in Rn:
                nc.reg_load(reg, flags_d[0:1, e * 16:e * 16 + 1])
            for c in range(3):
                for s_ in range(3):
                    j = 3 * c + s_

                    def prep(j=j, s_=s_):
                        nonlocal xi
                        for t4 in range(4):
                            k = xi % 2
                            xi += 1
                            row0 = e * CAP + j * 512 + t4 * 128
                            P.dma(SP, xr[k][:], hsorted[row0:row0 + 128, :], writes=[Bxr[k]])
                            ti = s_ * 4 + t4
                            wkx = {"xn": xr[k], "Bxn": Bxr[k]}
                            prenorm_b(A, S, 0, hTc[:, :, ti * 128:(ti + 1) * 128], BhTc[ti], wkx, (3, t4 % 2))
                    P.predicated(Rn, j, prep)
                for grp in range(ngrp):
                    wsl = wi % 2
                    wi += 1
                    f0 = grp * gw

                    def loadw(wsl=wsl, f0=f0):
                        P.dma(POOL, wgb[wsl][:], W["o_w_gate"][e, :, f0:f0 + gw].rearrange("(kc p) n -> p kc n", p=128), writes=[Bwt[wsl]])
                        P.dma(POOL, wub[wsl][:], W["o_w_up"][e, :, f0:f0 + gw].rearrange("(kc p) n -> p kc n", p=128), writes=[Bwt[wsl]])
                        P.dma(POOL, wdb[wsl][:], W["o_w_down"][e, f0:f0 + gw, :].rearrange("(j p) n -> p j n", p=128), writes=[Bwt[wsl]])
                    P.predicated(Rn, 3 * c, loadw)
                    for s_ in range(3):
                        j = 3 * c + s_

                        def body(s_=s_, wsl=wsl, grp=grp):
                            nonlocal si
                            asl = si % 2
                            hTr = [BhTc[s_ * 4 + i] for i in range(4)]
                            for jj in range(gch):
                                for (wbuf, half) in ((wgb[wsl], 0), (wub[wsl], 1)):
                                    for kc in range(8):
                                        P.op(PE, lambda kc=kc, jj=jj, wbuf=wbuf, half=half: nc.tensor.matmul(
                                            PS[jj % 2][:, half * 512:(half + 1) * 512],
                                            lhsT=wbuf[:, kc, jj * 128:(jj + 1) * 128], rhs=hTc[:, kc, s_ * 512:(s_ + 1) * 512],
                                            start=(kc == 0), stop=(kc == 7)),
                                            reads=hTr + [Bwt[wsl]], writes=[PSB[jj % 2][half]], inc=(kc == 7))
                                psg = PS[jj % 2]
                                ssl = si % 2
                                si += 1
                                P.op(ACT, lambda psg=psg, ssl=ssl: nc.scalar.activation(out=sil[ssl][:], in_=psg[:, 0:512], func=AF.Silu),
                                     reads=[PSB[jj % 2][0]], writes=[Bsil[ssl]])
                                P.op(DVE, lambda psg=psg, ssl=ssl, jj=jj: nc.vector.tensor_tensor(out=act[asl][:, jj, :], in0=psg[:, 512:1024], in1=sil[ssl][:], op=ALU.mult),
                                     reads=[PSB[jj % 2][1], Bsil[ssl]], writes=[Bact[asl][jj]])
                            for tp in range(2):
                                for jj in range(gch):
                                    for t4 in (2 * tp, 2 * tp + 1):
                                        pso = 2 + (t4 % 2)
                                        for half in range(2):
                                            P.op(PE, lambda jj=jj, half=half, t4=t4, pso=pso: nc.tensor.matmul(
                                                PS[pso][:, half * 512:(half + 1) * 512], lhsT=act[asl][:, jj, t4 * 128:(t4 + 1) * 128],
                                                rhs=wdb[wsl][:, jj, half * 512:(half + 1) * 512], start=(jj == 0), stop=(jj == gch - 1)),
                                                reads=[Bact[asl][jj], Bwt[wsl]], writes=[PSB[pso][half]], inc=(jj == gch - 1))
                                for t4 in (2 * tp, 2 * tp + 1):
                                    ti = s_ * 4 + t4
                                    pso = 2 + (t4 % 2)
                                    rd = [PSB[pso][0], PSB[pso][1]]
                                    if grp == 0:
                                        P.op(DVE, lambda ti=ti, pso=pso: nc.vector.tensor_copy(out=acc[:, ti, :], in_=PS[pso][:]), reads=rd, writes=[Bacc[ti]])
                                    else:
                                        P.op(DVE, lambda ti=ti, pso=pso: nc.vector.tensor_tensor(out=acc[:, ti, :], in0=PS[pso][:], in1=acc[:, ti, :], op=ALU.add),
                                             reads=rd, writes=[Bacc[ti]])
                                    if grp == ngrp - 1:
                                        row0 = e * CAP + (3 * c + s_) * 512 + t4 * 128
                                        P.dma(SP, ysorted[row0:row0 + 128, :], acc[:, ti, :], reads=[Bacc[ti]])
                        P.predicated(Rn, j, body)
        P.barrier()
        st.close()

        st = ExitStack()

        def sbt(name, shape, dt=F32):
            return st.enter_context(nc.sbuf_tensor("mc_" + name, list(shape), dt))
        wks = mk_wk(sbt, "mc", 2, with_x=True, with_xn=False)
        NY = 6
        y1 = [sbt(f"y1{i}", [128, D]) for i in range(NY)]
        y2 = [sbt(f"y2{i}", [128, D]) for i in range(NY)]
        By = [Buf("y") for _ in range(NY)]

        def gather(t):
            k = t % NY
            P.dma_ind(y1[k][:, :], None, ysorted[:, :], bass.IndirectOffsetOnAxis(ap=dest_i[:, t, 0:1], axis=0), reads=[Bdest[t]], writes=[By[k]])
            P.dma_ind(y2[k][:, :], None, ysorted[:, :], bass.IndirectOffsetOnAxis(ap=dest_i[:, t, 1:2], axis=0), reads=[Bdest[t]], writes=[By[k]])
        for t0 in range(4):
            gather(t0)
        for t in range(NT_OWN):
            k = t % NY
            if t + 4 < NT_OWN:
                gather(t + 4)
            P.op(DVE, lambda: nc.vector.tensor_scalar(out=y1[k][:], in0=y1[k][:], scalar1=wts[:, t, 0:1], scalar2=None, op0=ALU.mult), reads=[By[k], Bdest[t]], writes=[By[k]])
            P.op(DVE, lambda: nc.vector.scalar_tensor_tensor(out=y1[k][:], in0=y2[k][:], scalar=wts[:, t, 1:2], in1=y1[k][:], op0=ALU.mult, op1=ALU.add),
                 reads=[By[k], Bdest[t]], writes=[By[k]])
            r0 = t * 128
            epilogue(y1[k][:], [By[k]], x_in[r0:r0 + 128, :], G, x_out[r0:r0 + 128, :], wks[t % 2], add_on_dve=True)
        P.barrier()
        st.close()
        st0.close()

    def phaseC():
        st = ExitStack()

        def sbt(name, shape, dt=F32):
            return st.enter_context(nc.sbuf_tensor(name, list(shape), dt))
        A, S = AS["oA_mix"], AS["oS_mix"]
        G = Gt["o_mix"]
        wi_ = sbt("c_win", [128, 8, 2048], BF16)
        wo_ = sbt("c_wout", [128, 8, D], BF16)
        Bw = Buf("c_w")
        P.dma(POOL, wi_[:, :, 0:1024], W["o_w_in"][:, 0:1024].rearrange("(kc p) n -> p kc n", p=128), writes=[Bw])
        P.dma(POOL, wi_[:, :, 1024:2048], W["o_w_in"][:, 1024:2048].rearrange("(kc p) n -> p kc n", p=128), writes=[Bw])
        P.dma(POOL, wo_[:], W["o_w_out"].rearrange("(kc p) n -> p kc n", p=128), writes=[Bw])
        ws_b = sbt("c_wsb", [128, 8, 128], BF16)
        WsT = sbt("c_WsT", [128, 8, 128], BF16)
        ones_b = sbt("c_ones", [128, 128], BF16)
        Rg = sbt("c_Rg", [128, 8, 128])
        bsB = sbt("c_bsB", [128, 8, 128])
        gvT = sbt("c_gvT", [128, 8])
        bvT = sbt("c_bvT", [128, 8])
        Bs_ = Buf("c_s")
        P.dma(POOL, ws_b[:], W["o_w_s"].rearrange("g p q -> p g q"), writes=[Bs_])
        P.dma(SP, gvT[:], W["o_g_vT"][:], writes=[Bs_])
        P.dma(SP, bvT[:], W["o_b_vT"][:], writes=[Bs_])
        for g in range(8):
            P.dma(SP, bsB[:, g, :], W["o_b_s"][g:g + 1, :].partition_broadcast(128), writes=[Bs_])
        P.op(DVE, lambda: nc.vector.memset(ones_b[:], 1.0), writes=[Bs_])
        pv = psb16(3, 0)
        for g in range(8):
            P.op(PE, lambda g=g: nc.tensor.transpose(out=pv[:, g * 128:(g + 1) * 128], in_=ws_b[:, g, :], identity=ident_b[:]),
                 reads=[Bs_, B_consts], writes=[PSB[3][0]], inc=(g == 7))
        P.op(DVE, lambda: nc.vector.tensor_copy(out=WsT[:], in_=pv[:, 0:1024].rearrange("p (g q) -> p g q", g=8)), reads=[PSB[3][0]], writes=[Bs_])
        for g in range(8):
            P.op(PE, lambda g=g: nc.tensor.matmul(PS[3][:, 512 + (g % 4) * 128:512 + (g % 4 + 1) * 128], lhsT=ones_b[:], rhs=WsT[:, g, :], start=True, stop=True),
                 reads=[Bs_], writes=[PSB[3][1]], inc=True)
            P.op(DVE, lambda g=g: nc.vector.scalar_tensor_tensor(out=Rg[:, g, :], in0=PS[3][:, 512 + (g % 4) * 128:512 + (g % 4 + 1) * 128],
                                                                  scalar=bvT[:, g:g + 1], in1=bsB[:, g, :], op0=ALU.mult, op1=ALU.add),
                 reads=[PSB[3][1], Bs_], writes=[Bs_])
        hTb = [sbt(f"c_hT{i}", [128, 8, 512], BF16) for i in range(2)]
        BhT = [Buf("c_hT") for _ in range(2)]
        uT = sbt("c_uT", [128, 8, 512], BF16)
        Bu = Buf("c_uT")
        usT = sbt("c_usT", [128, 8, 512], BF16)
        Bus = Buf("c_usT")
        sT = sbt("c_sT", [128, 512])
        BsT = Buf("c_sT")
        vraw = [sbt(f"c_vraw{i}", [128, D]) for i in range(2)]
        vn = [sbt(f"c_vn{i}", [128, D], BF16) for i in range(4)]
        Bvr = [Buf("c_vraw") for _ in range(2)]
        Bvn = [Buf("c_vn") for _ in range(4)]
        stats = [sbt(f"c_stats{i}", [128, 2, 6]) for i in range(2)]
        mv = [sbt(f"c_mv{i}", [128, 2]) for i in range(2)]
        vsd = [sbt(f"c_vsd{i}", [128, 1]) for i in range(2)]
        vrs = [sbt(f"c_vrs{i}", [128, 1]) for i in range(2)]
        wks = mk_wk(sbt, "c", 2, with_x=True)
        ewks = mk_wk(sbt, "ce", 2, with_x=True, with_xn=False)
        gi = 0

        def gelu_from_psum(ps_ap, Bps, out_ap, Bout):
            P.op(ACT, lambda: nc.scalar.activation(out=out_ap, in_=ps_ap, func=AF.Gelu_apprx_tanh), reads=Bps, writes=[Bout])

        def c_prenorm(blk):
            hb, Bhb = hTb[blk % 2], BhT[blk % 2]

            def c_pa(t):
                wk = wks[t % 2]
                r0 = blk * 512 + t * 128
                P.dma(SP, wk["x"][:], xB[r0:r0 + 128, :], writes=[wk["Bx"]])
                prenorm_a(wk["x"][:], wk["Bx"], wk)
            c_pa(0)
            for t in range(4):
                if t + 1 < 4:
                    c_pa(t + 1)
                prenorm_b(A, S, 0, hb[:, :, t * 128:(t + 1) * 128], Bhb, wks[t % 2], (3, t % 2))

        c_prenorm(0)
        for blk in range(8):
            hb, Bhb = hTb[blk % 2], BhT[blk % 2]
            for f in range(8):
                pi_ = f % 2
                for kc in range(8):
                    P.op(PE, lambda kc=kc, f=f, pi_=pi_: nc.tensor.matmul(PS[pi_][:, 0:512], lhsT=wi_[:, kc, f * 128:(f + 1) * 128], rhs=hb[:, kc, :],
                                                                            start=(kc == 0), stop=(kc == 7)),
                         reads=[Bhb, Bw], writes=[PSB[pi_][0]], inc=(kc == 7))
                gelu_from_psum(PS[pi_][:, 0:512], [PSB[pi_][0]], uT[:, f, :], Bu)
            for t in range(4):
                k2 = t % 2
                for half in range(2):
                    for kc in range(8):
                        P.op(PE, lambda kc=kc, half=half, t=t: nc.tensor.matmul(PS[2][:, half * 512:(half + 1) * 512], lhsT=hb[:, kc, t * 128:(t + 1) * 128],
                                                                                 rhs=wi_[:, kc, 1024 + half * 512:1024 + (half + 1) * 512], start=(kc == 0), stop=(kc == 7)),
                             reads=[Bhb, Bw], writes=[PSB[2][half]], inc=(kc == 7))
                    gelu_from_psum(PS[2][:, half * 512:(half + 1) * 512], [PSB[2][half]], vraw[k2][:, half * 512:(half + 1) * 512], Bvr[k2])
                for half in range(2):
                    P.op(DVE, lambda half=half, k2=k2: nc.vector.bn_stats(out=stats[k2][:, half, :], in_=vraw[k2][:, half * 512:(half + 1) * 512]),
                         reads=[Bvr[k2]], writes=[Bvr[k2]])
                P.op(DVE, lambda k2=k2: nc.vector.bn_aggr(out=mv[k2][:], in_=stats[k2][:].rearrange("p a s -> p (a s)")), reads=[Bvr[k2]], writes=[Bvr[k2]])
                P.op(ACT, lambda k2=k2: nc.scalar.activation(out=vsd[k2][:], in_=mv[k2][:, 1:2], func=AF.Sqrt, bias=eps_t[:], scale=1.0),
                     reads=[Bvr[k2], B_consts], writes=[Bvr[k2]])
                P.op(DVE, lambda k2=k2: nc.vector.reciprocal(out=vrs[k2][:], in_=vsd[k2][:]), reads=[Bvr[k2]], writes=[Bvr[k2]])
                P.op(DVE, lambda k2=k2, t=t: nc.vector.tensor_scalar(out=vn[t][:], in0=vraw[k2][:], scalar1=mv[k2][:, 0:1], scalar2=vrs[k2][:],
                                                                   op0=ALU.subtract, op1=ALU.mult), reads=[Bvr[k2]], writes=[Bvn[t]])
            for g in range(8):
                pi_ = g % 2
                for t in range(4):
                    P.op(PE, lambda g=g, t=t, pi_=pi_: nc.tensor.matmul(PS[pi_][:, 512 + t * 128:512 + (t + 1) * 128], lhsT=vn[t][:, g * 128:(g + 1) * 128],
                                                                          rhs=WsT[:, g, :], start=True, stop=True),
                         reads=[Bvn[t], Bs_], writes=[PSB[pi_][1]], inc=(t == 3))
                P.op(DVE, lambda g=g, pi_=pi_: nc.vector.scalar_tensor_tensor(
                    out=sT[:].rearrange("p (c q) -> p c q", c=4), in0=PS[pi_][:, 512:1024].rearrange("p (c q) -> p c q", c=4), scalar=gvT[:, g:g + 1],
                    in1=Rg[:, g, :].unsqueeze(1).to_broadcast([128, 4, 128]), op0=ALU.mult, op1=ALU.add),
                    reads=[PSB[pi_][1], Bs_], writes=[BsT])
                P.op(DVE, lambda g=g: nc.vector.tensor_tensor(out=usT[:, g, :], in0=uT[:, g, :], in1=sT[:], op=ALU.mult), reads=[Bu, BsT], writes=[Bus])
            if blk + 1 < 8:
                c_prenorm(blk + 1)
            for t in range(4):
                wk = ewks[t % 2]
                py = 3 if t % 2 == 0 else 2
                for half in range(2):
                    for c in range(8):
                        P.op(PE, lambda c=c, half=half, t=t: nc.tensor.matmul(PS[py][:, half * 512:(half + 1) * 512], lhsT=usT[:, c, t * 128:(t + 1) * 128],
                                                                               rhs=wo_[:, c, half * 512:(half + 1) * 512], start=(c == 0), stop=(c == 7)),
                             reads=[Bus, Bw], writes=[PSB[py][half]], inc=(c == 7))
                r0 = blk * 512 + t * 128
                epilogue(PS[py][:], [PSB[py][0], PSB[py][1]], xB[r0:r0 + 128, :], G, xC[r0:r0 + 128, :], wk)
        P.barrier()
        st.close()

    if "0" in phases:
        phase0()
    if "A" in phases:
        phaseA()
    if "B" in phases:
        phase_ffn("e", xA, xB, 1, D_FF, 2, W["e_w_gate"], W["e_w_up"], W["e_w_down"], False)
    if "C" in phases:
        phaseC()
    if "D" in phases:
        if cfg.get("dense_moe", False):
            phase_ffn("o", xown if cfg.get("d_in") == "xown" else xC, out_d, NEXP, E_FF, 4, W["o_w_gate"], W["o_w_up"], W["o_w_down"], True)
        else:
            phase_moe(xown if cfg.get("d_in") == "xown" else xC, out_d)
    if dbg:
        dump = dscr("dbg_mod", [128, 8 * 2 * 8 + 4 * 0], out=True)
        i = 0
        for L in ("e", "o"):
            for k in ("A_mix", "S_mix", "A_ffn", "S_ffn"):
                P.dma(SP, dump[:, i * 16:(i + 1) * 16], AS[L + k][:].rearrange("p c r -> p (c r)"), reads=[B_mod])
                i += 1
        dumpG = dscr("dbg_G", [4, 128, D], out=True)
        for i, k in enumerate(("e_mix", "e_ffn", "o_mix", "o_ffn")):
            P.dma(SP, dumpG[i], Gt[k][:], reads=[B_mod])
    P.drain_all(SP)
    P.drain_all(POOL)
    es.close()
    return nc


def _rope_tables():
    rows = SEQ // 64
    r, col = np.meshgrid(np.arange(rows, dtype=np.float32), np.arange(64, dtype=np.float32), indexing="ij")
    inv = (1.0 / (10000.0 ** (np.arange(0, 32, 2, dtype=np.float32) / 32.0))).astype(np.float32)
    ang = np.concatenate([r.reshape(-1, 1) * inv, col.reshape(-1, 1) * inv], axis=-1).astype(np.float32)
    return np.concatenate([np.cos(ang), np.sin(ang)], axis=-1).astype(np.float32)


def _fm(v):
    return np.ascontiguousarray(np.asarray(v, np.float32).reshape(8, 128).T)


def _core_inputs(inp, r, rope_tab):
    b, h = r // 2, r % 2
    f = lambda a: np.ascontiguousarray(np.asarray(a, np.float32))
    x = inp["x"]
    own = f(x[b, h * NOWN:(h + 1) * NOWN])
    oth = f(x[b, (1 - h) * NOWN:(2 - h) * NOWN])
    cv = np.stack([np.asarray(inp["c"][b], np.float32), np.asarray(inp["c_ctx"], np.float32)], axis=-1)
    cvecT = np.ascontiguousarray(cv.reshape(8, 128, 2).transpose(1, 0, 2).reshape(128, 16))
    rope_c = np.concatenate([rope_tab[h * NOWN:(h + 1) * NOWN], rope_tab[(1 - h) * NOWN:(2 - h) * NOWN]], axis=0)
    hm = np.zeros((128, 2), np.float32)
    hm[:, 0] = float(h)
    hm[:, 1] = float(1 - h)
    m = {"xown": own, "xoth": oth, "ctx": f(inp["ctx"][b]), "cvecT": cvecT, "rope": np.ascontiguousarray(rope_c),
         "ident": np.eye(128, dtype=np.float32), "halo_mask": hm}
    m["e_g_pre_mixT"] = _fm(inp["e_g_pre_mix"][0])
    m["e_g_pre_ffnT"] = _fm(inp["e_g_pre_ffn"][0])
    m["o_g_pre_mixT"] = _fm(inp["o_g_pre_mix"][0])
    m["o_g_pre_ffnT"] = _fm(inp["o_g_pre_ffn"][0])
    m["e_g_post_mix"] = f(inp["e_g_post_mix"][0]).reshape(1, -1)
    m["e_g_post_ffn"] = f(inp["e_g_post_ffn"][0]).reshape(1, -1)
    m["o_g_post_mix"] = f(inp["o_g_post_mix"][0]).reshape(1, -1)
    m["o_g_post_ffn"] = f(inp["o_g_post_ffn"][0]).reshape(1, -1)
    for pre in ("e", "o"):
        m[pre + "_w_mod"] = f(inp[pre + "_w_mod"][0])
        m[pre + "_bmodT"] = np.ascontiguousarray(np.asarray(inp[pre + "_b_mod"][0], np.float32).reshape(48, 128).T)
        m[pre + "_b_mod"] = f(inp[pre + "_b_mod"][0]).reshape(1, -1)
        m[pre + "_w_in"] = f(inp[pre + "_w_in"][0])
        m[pre + "_w_out"] = f(inp[pre + "_w_out"][0])
    m["e_g_q"] = f(inp["e_g_q"][0]).reshape(1, 64)
    m["e_g_k"] = f(inp["e_g_k"][0]).reshape(1, 64)
    wc = np.asarray(inp["e_w_conv"][0], np.float32)
    m["e_w_convT"] = np.ascontiguousarray(wc.reshape(3, 4, 128).transpose(2, 1, 0).reshape(128, 12))
    m["e_w_gate"] = f(inp["e_w_gate"][0])
    m["e_w_up"] = f(inp["e_w_up"][0])
    m["e_w_down"] = f(inp["e_w_down"][0])
    m["o_g_vT"] = _fm(inp["o_g_v"][0])
    m["o_b_vT"] = _fm(inp["o_b_v"][0])
    m["o_w_s"] = f(inp["o_w_s"][0])
    m["o_b_s"] = f(inp["o_b_s"][0])
    m["o_w_router"] = f(inp["o_w_router"][0])
    m["o_b_router"] = f(inp["o_b_router"][0]).reshape(1, NEXP)
    m["o_w_gate"] = f(inp["o_w_gate"][0])
    m["o_w_up"] = f(inp["o_w_up"][0])
    m["o_w_down"] = f(inp["o_w_down"][0])
    return m


def kernel(**inputs):
    nc = _build({})
    rope_tab = _rope_tables()
    in_maps = [_core_inputs(inputs, r, rope_tab) for r in range(8)]
    res = run_bass_kernel_spmd(nc, in_maps, core_ids=list(range(8)))
    out = np.empty((NB, SEQ, D), np.float32)
    for r in range(8):
        b, h = r // 2, r % 2
        out[b, h * NOWN:(h + 1) * NOWN] = res.results[r]["out"]
    return out
```
